# Optimizing a Trainium2 kernel written in Bass

```python
import math
import jax, jax.numpy as jnp
from jax import lax
import numpy as np

D_MODEL = 1024
BATCH = 32
SEQ = 2048
DEPTH = 2

HEAD_DIM = D_MODEL // 16
A_HEADS = 4
IDX_HEADS = 4
IDX_DIM = HEAD_DIM
TOPK_MAX = 256
B_HEADS = 4
DIFF_DH = HEAD_DIM // 2
C_HEADS = 8
C_CONFIGS = ((128, 1), (512, 4), (2048, 16))
C_BLOCK = 128
Q_BLOCK = 128
MIX_WIDTH = (A_HEADS + B_HEADS + C_HEADS) * HEAD_DIM
FF_DIM = -(-8 * D_MODEL // (3 * 256)) * 256
DEEPNORM_ALPHA = (2 * DEPTH) ** 0.25
DEEPNORM_BETA = (8 * DEPTH) ** -0.25
LN_EPS = 1e-5
IN_SIZES = (A_HEADS * HEAD_DIM, HEAD_DIM, HEAD_DIM,
            IDX_HEADS * IDX_DIM, IDX_DIM, IDX_HEADS,
            B_HEADS * 2 * DIFF_DH, B_HEADS * 2 * DIFF_DH, B_HEADS * HEAD_DIM,
            C_HEADS * HEAD_DIM, C_HEADS * HEAD_DIM, C_HEADS * HEAD_DIM)
IN_COLS = sum(IN_SIZES)

kernel_name = 'hybrid_dsa_diff_dilated_deepnorm'


def layer_norm(x, g, b):
    xf = x.astype(jnp.float32)
    mu = jnp.mean(xf, axis=-1, keepdims=True)
    var = jnp.mean(jnp.square(xf - mu), axis=-1, keepdims=True)
    y = (xf - mu) * lax.rsqrt(var + LN_EPS) * g.astype(jnp.float32) + b.astype(jnp.float32)
    return y.astype(x.dtype)


def alibi_slopes(n):
    return jnp.asarray([2.0 ** (-8.0 * (i + 1) / n) for i in range(n)], dtype=jnp.float32)


def to_blocks(a):
    b, s = a.shape[:2]
    a = a.reshape((b, s // Q_BLOCK, Q_BLOCK) + a.shape[2:])
    return jnp.moveaxis(a, 1, 0)


def from_blocks(a):
    nb, b, q = a.shape[:3]
    return jnp.moveaxis(a, 0, 1).reshape((b, nb * q) + a.shape[3:])


def split_columns(z):
    parts, start = [], 0
    for size in IN_SIZES:
        parts.append(z[..., start:start + size])
        start += size
    return parts


def dsa_attention(q, k, v, qi, ki, wi):
    s = q.shape[1]
    topk = min(TOPK_MAX, s // 4)
    slopes = alibi_slopes(A_HEADS)
    key_pos = jnp.arange(s)
    wi = wi.astype(jnp.float32) * IDX_HEADS ** -0.5

    def block(args):
        blk, q_b, qi_b, wi_b = args
        t = blk * Q_BLOCK + jnp.arange(Q_BLOCK)
        rel = jax.nn.relu(jnp.einsum('bqhd,bsd->bqhs', qi_b, ki).astype(jnp.float32) * IDX_DIM ** -0.5)
        score = jnp.einsum('bqhs,bqh->bqs', rel, wi_b)
        score = jnp.where(key_pos[None, None, :] <= t[None, :, None], score, -jnp.inf)
        _, sel = lax.top_k(score, topk)
        k_sel = jax.vmap(lambda kk, ii: kk[ii])(k, sel)
        v_sel = jax.vmap(lambda vv, ii: vv[ii])(v, sel)
        dist = (t[None, :, None] - sel).astype(jnp.float32)
        logits = jnp.einsum('bqhd,bqkd->bqhk', q_b, k_sel).astype(jnp.float32) * HEAD_DIM ** -0.5
        logits = logits - slopes[None, None, :, None] * dist[:, :, None, :]
        logits = jnp.where((dist >= 0)[:, :, None, :], logits, -jnp.inf)
        p = jax.nn.softmax(logits, axis=-1)
        return jnp.einsum('bqhk,bqkd->bqhd', p, v_sel.astype(jnp.float32)).astype(v.dtype)

    nb = s // Q_BLOCK
    out = lax.map(block, (jnp.arange(nb), to_blocks(q), to_blocks(qi), to_blocks(wi)))
    return from_blocks(out)


def diff_attention(q, k, v, lam, subln_g, lambda_init):
    s = q.shape[1]
    slopes = alibi_slopes(B_HEADS)
    lam = lam.astype(jnp.float32)
    lam_full = jnp.exp(jnp.sum(lam[0] * lam[1])) - jnp.exp(jnp.sum(lam[2] * lam[3])) + lambda_init
    key_pos = jnp.arange(s)
    vf = v.astype(jnp.float32)

    def block(args):
        blk, q_b = args
        t = blk * Q_BLOCK + jnp.arange(Q_BLOCK)
        dist = (t[:, None] - key_pos[None, :]).astype(jnp.float32)
        logits = jnp.einsum('bqhmd,bshmd->bhmqs', q_b, k).astype(jnp.float32) * DIFF_DH ** -0.5
        logits = logits - slopes[None, :, None, None, None] * dist
        logits = jnp.where(dist >= 0, logits, -jnp.inf)
        p = jax.nn.softmax(logits, axis=-1)
        w = p[:, :, 0] - lam_full * p[:, :, 1]
        return jnp.einsum('bhqs,bshd->bqhd', w, vf)

    nb = s // Q_BLOCK
    o = from_blocks(lax.map(block, (jnp.arange(nb), to_blocks(q))))
    o = o * lax.rsqrt(jnp.mean(o * o, axis=-1, keepdims=True) + LN_EPS) * subln_g.astype(jnp.float32)
    return (o * (1.0 - lambda_init)).astype(v.dtype)


def dilated_branch(q, k, v, dilation, steps, slopes):
    b, s, h, d = q.shape
    ls = s // dilation
    nb = -(-ls // C_BLOCK)
    lp = nb * C_BLOCK

    def sub(a):
        a = a.reshape(b, ls, dilation, h, d).transpose(0, 2, 1, 3, 4)
        a = jnp.pad(a, ((0, 0), (0, 0), (0, lp - ls), (0, 0), (0, 0)))
        return a.reshape(b, dilation, nb, C_BLOCK, h, d)

    def band(a):
        prev = jnp.pad(a, ((0, 0), (0, 0), (1, 0), (0, 0), (0, 0), (0, 0)))[:, :, :-1]
        return jnp.concatenate([prev, a], axis=3)

    qs = sub(q)
    kb, vb = band(sub(k)), band(sub(v))
    qi = jnp.arange(C_BLOCK) + C_BLOCK
    kj = jnp.arange(2 * C_BLOCK)
    delta = qi[:, None] - kj[None, :]
    key_idx = jnp.arange(nb)[:, None] * C_BLOCK + kj[None, :] - C_BLOCK
    valid = (delta >= 0)[None] & (delta <= steps)[None] & (key_idx >= 0)[:, None, :]
    dist = (delta * dilation).astype(jnp.float32)
    logits = jnp.einsum('bonqhd,bonkhd->bonhqk', qs, kb).astype(jnp.float32) * HEAD_DIM ** -0.5
    logits = logits - slopes[:, None, None] * dist
    logits = jnp.where(valid[:, None], logits, -jnp.inf)
    lse = jax.nn.logsumexp(logits, axis=-1)
    p = jnp.exp(logits - lse[..., None])
    o = jnp.einsum('bonhqk,bonkhd->bonqhd', p, vb.astype(jnp.float32))
    o = o.reshape(b, dilation, lp, h, d)[:, :, :ls].transpose(0, 2, 1, 3, 4).reshape(b, s, h, d)
    lse = lse.transpose(0, 1, 2, 4, 3).reshape(b, dilation, lp, h)[:, :, :ls]
    lse = lse.transpose(0, 2, 1, 3).reshape(b, s, h)
    return o, lse


def dilated_attention(q, k, v):
    slopes = alibi_slopes(C_HEADS)
    outs, lses = [], []
    for window, dil in C_CONFIGS:
        o, l = dilated_branch(q, k, v, dil, window // dil, slopes)
        outs.append(o)
        lses.append(l)
    wts = jax.nn.softmax(jnp.stack(lses), axis=0)
    o = jnp.sum(wts[..., None] * jnp.stack(outs), axis=0)
    return o.astype(v.dtype)


def hybrid_mixer(h, w_in, w_o, lam, subln_g, lambda_init):
    b, s, _ = h.shape
    p = split_columns(h @ w_in)
    qa = p[0].reshape(b, s, A_HEADS, HEAD_DIM)
    qi = p[3].reshape(b, s, IDX_HEADS, IDX_DIM)
    oa = dsa_attention(qa, p[1], p[2], qi, p[4], p[5]).reshape(b, s, -1)
    qb = p[6].reshape(b, s, B_HEADS, 2, DIFF_DH)
    kb = p[7].reshape(b, s, B_HEADS, 2, DIFF_DH)
    vb = p[8].reshape(b, s, B_HEADS, HEAD_DIM)
    ob = diff_attention(qb, kb, vb, lam, subln_g, lambda_init).reshape(b, s, -1)
    qc = p[9].reshape(b, s, C_HEADS, HEAD_DIM)
    kc = p[10].reshape(b, s, C_HEADS, HEAD_DIM)
    vc = p[11].reshape(b, s, C_HEADS, HEAD_DIM)
    oc = dilated_attention(qc, kc, vc).reshape(b, s, -1)
    return jnp.concatenate([oa, ob, oc], axis=-1) @ w_o


def setup_inputs(seed: int = 0) -> dict:
    key = jax.random.key(seed)
    ks = jax.random.split(key, 12)
    nrm = jax.random.normal
    x = nrm(ks[0], (BATCH, SEQ, D_MODEL), jnp.float32)
    w_in = nrm(ks[1], (DEPTH, D_MODEL, IN_COLS), jnp.float32) * D_MODEL ** -0.5
    w_o = nrm(ks[2], (DEPTH, MIX_WIDTH, D_MODEL), jnp.float32) * (MIX_WIDTH ** -0.5 * DEEPNORM_BETA)
    lam = 0.1 * nrm(ks[3], (DEPTH, 4, DIFF_DH), jnp.float32)
    subln_g = 1.0 + 0.01 * nrm(ks[4], (DEPTH, HEAD_DIM), jnp.float32)
    ln1_g = 1.0 + 0.01 * nrm(ks[5], (DEPTH, D_MODEL), jnp.float32)
    ln1_b = 0.01 * nrm(ks[6], (DEPTH, D_MODEL), jnp.float32)
    w_gate = nrm(ks[7], (DEPTH, D_MODEL, FF_DIM), jnp.float32) * D_MODEL ** -0.5
    w_up = nrm(ks[8], (DEPTH, D_MODEL, FF_DIM), jnp.float32) * D_MODEL ** -0.5
    w_down = nrm(ks[9], (DEPTH, FF_DIM, D_MODEL), jnp.float32) * (FF_DIM ** -0.5 * DEEPNORM_BETA)
    ln2_g = 1.0 + 0.01 * nrm(ks[10], (DEPTH, D_MODEL), jnp.float32)
    ln2_b = 0.01 * nrm(ks[11], (DEPTH, D_MODEL), jnp.float32)
    return {'x': x, 'w_in': w_in, 'w_o': w_o, 'lam': lam, 'subln_g': subln_g,
            'ln1_g': ln1_g, 'ln1_b': ln1_b, 'w_gate': w_gate, 'w_up': w_up,
            'w_down': w_down, 'ln2_g': ln2_g, 'ln2_b': ln2_b}


def reference(x, w_in, w_o, lam, subln_g, ln1_g, ln1_b, w_gate, w_up, w_down, ln2_g, ln2_b):
    for l in range(DEPTH):
        lambda_init = 0.8 - 0.6 * math.exp(-0.3 * l)
        mix = hybrid_mixer(x, w_in[l], w_o[l], lam[l], subln_g[l], lambda_init)
        x = layer_norm(DEEPNORM_ALPHA * x + mix, ln1_g[l], ln1_b[l])
        f = (jax.nn.silu(x @ w_gate[l]) * (x @ w_up[l])) @ w_down[l]
        x = layer_norm(DEEPNORM_ALPHA * x + f, ln2_g[l], ln2_b[l])
    return x
```

```python
import math
from contextlib import ExitStack

import numpy as np
import concourse.bass as bass
import concourse.mybir as mybir
from concourse.bass_utils import run_bass_kernel_spmd

F32 = mybir.dt.float32
BF16 = mybir.dt.bfloat16
AF = mybir.ActivationFunctionType
ALU = mybir.AluOpType
AX = mybir.AxisListType

S = 2048
D = 1024
NT = 16
FF = 2816
NFF = 22
INC = 3012
ALPHA = 4.0 ** 0.25
LN_EPS = 1e-5
NIT = 14
NEG = -30000.0
N_CORES = 8
SEQ_PER_CORE = 4

C_QA, C_KA, C_VA, C_QI, C_KI, C_WI, C_QB, C_KB, C_VB, C_QC, C_KC, C_VC = (
    0, 256, 320, 384, 640, 704, 708, 964, 1220, 1476, 1988, 2500)


class Sem:
    def __init__(self, h):
        self.h = h
        self.n = 0


class Eng:
    def __init__(self, k, eng, name):
        self.e = eng
        self.sem = Sem(k.new_sem("e_" + name))
        self.seen = {}
        self.last = None

    def wait(self, *toks):
        for t in toks:
            if t is None:
                continue
            if isinstance(t, list):
                self.wait(*t)
                continue
            s, v = t
            if self.seen.get(s, 0) >= v:
                continue
            self.e.wait_ge(s.h, v)
            self.seen[s] = v

    def mark(self, inst):
        self.sem.n += 1
        inst.then_inc(self.sem.h, 1)
        self.last = (self.sem, self.sem.n)
        return self.last


class K:
    def __init__(self, nl, nseq, lam_inits, debug=False):
        self.nl, self.nseq, self.lam_inits, self.debug = nl, nseq, lam_inits, debug
        self.nc = bass.Bass("TRN2", target_bir_lowering=False)
        self.ctx = ExitStack()
        self.dsems = []
        self.sem_pool = []
        self.in_use = []

    def new_sem(self, name):
        return self.ctx.enter_context(self.nc.semaphore(name))

    def dsem(self, name):
        if self.sem_pool:
            s = self.sem_pool.pop()
        else:
            s = Sem(self.new_sem("d%d" % len(self.dsems)))
            self.dsems.append(s)
        self.in_use.append(s)
        return s

    def dram(self, name, shape, dt, kind="Internal"):
        return self.nc.dram_tensor(name, list(shape), dt, kind=kind).ap()

    def dma(self, q, out, in_, sem, waits=(), **kw):
        q.wait(*waits)
        inst = q.e.dma_start(out=out, in_=in_, **kw)
        sem.n += 16
        inst.then_inc(sem.h, 16)
        return (sem, sem.n)

    def barrier(self):
        sp = self.SP
        for e in self.engs:
            if e is not sp:
                sp.wait(e.last)
        for s in self.dsems:
            if s.n:
                sp.wait((s, s.n))
        tok = self.dma(sp, self.bar_b[:], self.bar_a[:], self.bar_sem)
        for e in self.engs:
            e.wait(tok)
        self.sem_pool.extend(self.in_use)
        self.in_use = []

    def build(self):
        nc, nl, nseq = self.nc, self.nl, self.nseq
        ctx = self.ctx
        dbg = "ExternalOutput" if self.debug else "Internal"
        ein = lambda n, s: self.dram(n, s, F32, "ExternalInput")
        self.x_in = ein("x", [nseq, S, D])
        self.w_in = ein("w_in", [nl, D, INC])
        self.w_o = ein("w_o", [nl, D, D])
        self.w_g = ein("w_gate", [nl, D, FF])
        self.w_u = ein("w_up", [nl, D, FF])
        self.w_d = ein("w_down", [nl, FF, D])
        self.lam_r = ein("lam_r", [nl, 128, 128])
        self.sg_r = ein("sg_r", [nl, 128, 64])
        self.ln_r = ein("ln_r", [nl, 4, 128, D])
        self.c_lc = ein("c_lc", [16, 128, 128])
        self.c_kaug = ein("c_kaug", [4, S])
        self.c_qaug = ein("c_qaug", [8, 4, S])
        self.c_ident = ein("c_ident", [128, 128])
        self.c_causf = ein("c_causf", [128, 128])
        self.c_lcaus = ein("c_lcaus", [128, 128])
        self.c_pow2 = ein("c_pow2", [128, NIT])
        self.y_out = self.dram("y", [nseq, S, D], F32, "ExternalOutput")
        self.w1b = self.dram("w1b", [nl, D, INC], BF16)
        self.wob = self.dram("wob", [nl, D, D], BF16)
        self.wgub = self.dram("wgub", [nl, NFF, 128, 2, 1024], BF16)
        self.wdb = self.dram("wdb", [nl, FF, D], BF16)
        self.kaug_b = self.dram("kaug_b", [4, S], BF16)
        self.qaug_b = self.dram("qaug_b", [8, 4, S], BF16)
        self.zq = self.dram("zq", [nl, nseq, INC, S], BF16, dbg)
        self.zv = self.dram("zv", [nl, nseq, S, 13 * 65], BF16, dbg)
        self.zw = self.dram("zw", [nl, nseq, S, 4], F32, dbg)
        self.zqf = self.dram("zqf", [nl, nseq, 320, S], F32, dbg)
        self.om = self.dram("om", [nl, nseq, S, D], BF16, dbg)
        self.xmid = [self.dram("xmid%d" % i, [nseq, S, D], F32, dbg) for i in range(nl - 1)]

        with ctx:
            self.PE = Eng(self, nc.tensor, "pe")
            self.ACT = Eng(self, nc.scalar, "act")
            self.DVE = Eng(self, nc.vector, "dve")
            self.POOL = Eng(self, nc.gpsimd, "pool")
            self.SP = Eng(self, nc.sync, "sp")
            self.engs = [self.PE, self.ACT, self.DVE, self.POOL, self.SP]
            self.bar_sem = Sem(self.new_sem("bar"))
            sb = lambda n, s, d: ctx.enter_context(nc.sbuf_tensor(n, list(s), d))
            self.bar_a = sb("bar_a", [1, 8], F32)
            self.bar_b = sb("bar_b", [1, 8], F32)
            self.ident = sb("ident", [128, 128], BF16)
            self.identf = sb("identf", [128, 128], F32)
            self.lcaus = sb("lcaus", [128, 128], BF16)
            self.causf = sb("causf", [128, 128], F32)
            self.pow2 = sb("pow2", [128, NIT], F32)
            self.lc = sb("lc", [128, 16, 128], BF16)
            self.phase0()
            self.barrier()
            for li in range(nl):
                xsrc = self.x_in if li == 0 else self.xmid[li - 1]
                ydst = self.y_out if li == nl - 1 else self.xmid[li]
                self.phase1(li, xsrc)
                self.barrier()
                self.phase2(li)
                self.barrier()
                self.phase3(li, xsrc, ydst)
                self.barrier()
        return nc

    def phase0(self):
        nc, nl = self.nc, self.nl
        P, SP = self.POOL, self.SP
        sw = self.dsem("w0")
        ncd = [0]

        def cd(o, i):
            ncd[0] += 1
            t = self.dma(P, o, i, sw, max_dma_last_dim=4096)
            if ncd[0] % 6 == 0:
                P.wait(t)
            return t
        POOLm = lambda inst: P.mark(inst)
        POOLm(nc.gpsimd.memset(self.bar_a[:], 0.0))
        cd(self.ident[:], self.c_ident)
        cd(self.lcaus[:], self.c_lcaus)
        cd(self.lc[:], self.c_lc.rearrange("d s t -> s d t"))
        cd(self.kaug_b, self.c_kaug)
        cd(self.qaug_b, self.c_qaug)
        self.dma(SP, self.identf[:], self.c_ident, sw)
        self.dma(SP, self.causf[:], self.c_causf, sw)
        self.dma(SP, self.pow2[:], self.c_pow2, sw)
        for l in range(nl):
            for kc in range(8):
                cd(self.w1b[l, kc * 128:(kc + 1) * 128, :], self.w_in[l, kc * 128:(kc + 1) * 128, :])
            for h in range(2):
                cd(self.wob[l, h * 512:(h + 1) * 512, :], self.w_o[l, h * 512:(h + 1) * 512, :])
            for c in range(NFF):
                for j, w in enumerate((self.w_g, self.w_u)):
                    cd(self.wgub[l, c, :, j, :].rearrange("p (k f) -> p k f", k=8),
                       w[l, :, c * 128:(c + 1) * 128].rearrange("(k p) f -> p k f", p=128))
            for h in range(NFF):
                cd(self.wdb[l, h * 128:(h + 1) * 128, :], self.w_d[l, h * 128:(h + 1) * 128, :])

    def phase1(self, li, xsrc):
        nc = self.nc
        PE, ACT, DVE, POOL, SP = self.PE, self.ACT, self.DVE, self.POOL, self.SP
        with ExitStack() as c:
            sb = lambda n, s, d: c.enter_context(nc.sbuf_tensor("L%d_%s" % (li, n), list(s), d))
            ps = lambda n, s, d: c.enter_context(nc.psum_tensor("L%d_%s" % (li, n), list(s), d))
            w1 = sb("p1_w1", [128, 8, INC], BF16)
            xT = sb("p1_xT", [128, 8, S], BF16)
            xt = [sb("p1_xt%d" % i, [128, D], F32) for i in range(2)]
            zst = [sb("p1_zst%d" % i, [128, S], BF16) for i in range(2)]
            vst = [sb("p1_vst%d" % i, [128, 13, 65], BF16) for i in range(2)]
            wst4 = [sb("p1_wst4_%d" % i, [128, 4, 4], F32) for i in range(2)]
            xTf = [sb("p1_xTf%d" % i, [128, 8, 512], F32) for i in range(2)]
            w1f = sb("p1_w1f", [128, 8, 388], F32)
            zstf = [sb("p1_zstf%d" % i, [128, 512], F32) for i in range(3)]
            pX = [ps("p1_pX%d" % i, [128, 1024], F32) for i in range(2)]
            pz = [ps("p1_pz%d" % i, [128, 512], F32) for i in range(4)]
            s_w = self.dsem("p1w%d" % li)
            s_x = [self.dsem("p1x%d_%d" % (li, i)) for i in range(2)]
            s_z = [self.dsem("p1z%d_%d" % (li, i)) for i in range(2)]
            s_v = [self.dsem("p1v%d_%d" % (li, i)) for i in range(2)]
            s_f = [self.dsem("p1f%d_%d" % (li, i)) for i in range(3)]
            s_w4 = [self.dsem("p1w4%d_%d" % (li, i)) for i in range(2)]
            t_w = self.dma(SP, w1[:], self.w1b[li].rearrange("(k p) n -> p k n", p=128), s_w)
            t_pad = POOL.mark(nc.gpsimd.memset(w1f[:, :, 320:384], 0.0))
            wsrc = self.w_in[li].rearrange("(k p) n -> p k n", p=128)
            self.dma(SP, w1f[:, :, 0:320], wsrc[:, :, C_QI:C_QI + 320], s_w)
            t_wf = self.dma(SP, w1f[:, :, 384:388], wsrc[:, :, C_WI:C_WI + 4], s_w)
            xTf_free = [None, None]
            zstf_free = [None] * 3
            wst4_free = [None, None]
            fi = 0
            t_ones = [POOL.mark(nc.gpsimd.memset(vst[i][:, :, 64:65], 1.0)) for i in range(2)]
            xt_free = [None, None]
            pX_free = [None, None]
            pz_free = [None] * 4
            zst_free = [None, None]
            vst_free = [None, None]
            xT_free = None
            pzi = 0
            evi = 0

            def ev_copy(out, in_, scale, waits):
                nonlocal evi
                evi += 1
                if evi % 2 == 0:
                    ACT.wait(*waits)
                    if scale == 1.0:
                        return ACT.mark(nc.scalar.copy(out=out, in_=in_))
                    return ACT.mark(nc.scalar.activation(out=out, in_=in_, func=AF.Copy, scale=float(scale)))
                DVE.wait(*waits)
                if scale == 1.0:
                    return DVE.mark(nc.vector.tensor_copy(out=out, in_=in_))
                return DVE.mark(nc.vector.tensor_scalar(out, in_, float(scale), None, ALU.mult))

            fm_groups = ([(C_QA + 128 * i, 128, 0.125) for i in range(2)] + [(C_KA, 64, 1.0)]
                         + [(C_QB + 128 * i, 128, 32.0 ** -0.5) for i in range(2)]
                         + [(C_KB + 128 * i, 128, 1.0) for i in range(2)]
                         + [(C_QC + 128 * i, 128, 0.125) for i in range(4)]
                         + [(C_KC + 128 * i, 128, 1.0) for i in range(4)])
            for sq in range(self.nseq):
                tokX = []
                for i in range(NT):
                    sl = i % 2
                    t_ld = self.dma(SP, xt[sl][:], xsrc[sq, i * 128:(i + 1) * 128, :], s_x[sl], [xt_free[sl]])
                    PE.wait(t_ld, pX_free[sl])
                    for k in range(8):
                        ins = nc.tensor.transpose(pX[sl][:, k * 128:(k + 1) * 128], xt[sl][:, k * 128:(k + 1) * 128],
                                                  self.identf[:])
                    tT = PE.mark(ins)
                    xt_free[sl] = tT
                    ch, jj = i // 4, i % 4
                    xs = ch % 2
                    E1, E2 = (ACT, DVE) if i % 2 == 0 else (DVE, ACT)
                    cpy = lambda E, o, i_: E.mark(nc.scalar.copy(out=o, in_=i_) if E is ACT
                                                  else nc.vector.tensor_copy(out=o, in_=i_))
                    E1.wait(tT, xTf_free[xs])
                    tXf = cpy(E1, xTf[xs][:, :, jj * 128:(jj + 1) * 128], pX[sl][:].rearrange("p (k t) -> p k t", k=8))
                    pX_free[sl] = tXf
                    E2.wait(tXf, xT_free)
                    tX = cpy(E2, xT[:, :, i * 128:(i + 1) * 128], xTf[xs][:, :, jj * 128:(jj + 1) * 128])
                    tokX.append(tX)
                    if jj == 3:
                        PE.wait(t_wf, t_pad, ACT.last, DVE.last)
                        for (c0f, M, scale) in ((0, 128, 0.125), (128, 128, 0.125), (256, 128, 1.0)):
                            Mo = 64 if c0f == 256 else 128
                            b = pzi % 4
                            pzi += 1
                            PE.wait(pz_free[b])
                            for k in range(8):
                                ins = nc.tensor.matmul(pz[b][0:M, :], lhsT=w1f[:, k, c0f:c0f + M], rhs=xTf[xs][:, k, :],
                                                       start=(k == 0), stop=(k == 7))
                            tM = PE.mark(ins)
                            fs = fi % 3
                            fi += 1
                            te = ev_copy(zstf[fs][0:Mo, :], pz[b][0:Mo, :], scale, [tM, zstf_free[fs]])
                            pz_free[b] = te
                            zstf_free[fs] = self.dma(SP, self.zqf[li, sq, c0f:c0f + Mo, ch * 512:(ch + 1) * 512],
                                                     zstf[fs][0:Mo, :], s_f[fs], [te])
                        b = pzi % 4
                        pzi += 1
                        PE.wait(pz_free[b])
                        for j4 in range(4):
                            for k in range(8):
                                ins = nc.tensor.matmul(pz[b][:, j4 * 4:(j4 + 1) * 4],
                                                       lhsT=xTf[xs][:, k, j4 * 128:(j4 + 1) * 128],
                                                       rhs=w1f[:, k, 384:388], start=(k == 0), stop=(k == 7))
                        tM = PE.mark(ins)
                        xTf_free[xs] = tM
                        ws = ch % 2
                        te = ev_copy(wst4[ws][:].rearrange("p j h -> p (j h)"), pz[b][:, 0:16], 0.5,
                                     [tM, wst4_free[ws]])
                        pz_free[b] = te
                        wst4_free[ws] = self.dma(SP, self.zw[li, sq, ch * 512:(ch + 1) * 512, :].rearrange(
                            "(j p) h -> p j h", p=128), wst4[ws][:], s_w4[ws], [te])
                zi = 0
                for (c0, M, scale) in fm_groups:
                    zs = zi % 2
                    zi += 1
                    tE = []
                    for tc in range(4):
                        b = pzi % 4
                        pzi += 1
                        PE.wait(t_w, pz_free[b], tokX[tc * 4:(tc + 1) * 4])
                        for k in range(8):
                            ins = nc.tensor.matmul(pz[b][0:M, :], lhsT=w1[:, k, c0:c0 + M],
                                                   rhs=xT[:, k, tc * 512:(tc + 1) * 512], start=(k == 0), stop=(k == 7))
                        tM = PE.mark(ins)
                        te = ev_copy(zst[zs][0:M, tc * 512:(tc + 1) * 512], pz[b][0:M, :], scale, [tM, zst_free[zs]])
                        pz_free[b] = te
                        tE.append(te)
                    zst_free[zs] = self.dma(SP, self.zq[li, sq, c0:c0 + M, :], zst[zs][0:M, :], s_z[zs], tE)
                for i in range(NT):
                    sl = i % 2
                    bA = pzi % 4
                    bB = (pzi + 1) % 4
                    pzi += 2
                    PE.wait(t_w, pz_free[bA], pz_free[bB], tokX)
                    lhs = lambda k: xT[:, k, i * 128:(i + 1) * 128]
                    for (bank, o0, n, c0) in ((bA, 0, 512, C_VC), (bB, 0, 64, C_VA), (bB, 68, 256, C_VB)):
                        for k in range(8):
                            ins = nc.tensor.matmul(pz[bank][:, o0:o0 + n], lhsT=lhs(k), rhs=w1[:, k, c0:c0 + n],
                                                   start=(k == 0), stop=(k == 7))
                    tM = PE.mark(ins)
                    E = ACT if i % 2 == 0 else DVE
                    E.wait(tM, vst_free[sl], t_ones[sl])
                    if E is ACT:
                        cp = lambda o, i_: ACT.mark(nc.scalar.copy(out=o, in_=i_))
                        wsc = lambda o, i_: ACT.mark(nc.scalar.activation(out=o, in_=i_, func=AF.Copy, scale=0.5))
                    else:
                        cp = lambda o, i_: DVE.mark(nc.vector.tensor_copy(out=o, in_=i_))
                        wsc = lambda o, i_: DVE.mark(nc.vector.tensor_scalar(o, i_, 0.5, None, ALU.mult))
                    cp(vst[sl][:, 5:13, 0:64], pz[bA][:, :].rearrange("p (h d) -> p h d", h=8))
                    cp(vst[sl][:, 0, 0:64], pz[bB][:, 0:64])
                    te = cp(vst[sl][:, 1:5, 0:64], pz[bB][:, 68:324].rearrange("p (h d) -> p h d", h=4))
                    pz_free[bA] = te
                    pz_free[bB] = te
                    vst_free[sl] = self.dma(SP, self.zv[li, sq, i * 128:(i + 1) * 128, :],
                                            vst[sl][:].rearrange("p h d -> p (h d)"), s_v[sl], [te])
                xT_free = PE.last

    def phase2(self, li):
        nc = self.nc
        PE, ACT, DVE, POOL, SP = self.PE, self.ACT, self.DVE, self.POOL, self.SP
        lam_init = self.lam_inits[li]
        V = nc.vector
        with ExitStack() as c:
            sb = lambda n, s, d: c.enter_context(nc.sbuf_tensor("L%d_%s" % (li, n), list(s), d))
            ps = lambda n, s, d: c.enter_context(nc.psum_tensor("L%d_%s" % (li, n), list(s), d))
            aq = [sb("p2_aq%d" % i, [68, S], BF16) for i in range(4)]
            ak = sb("p2_ak", [68, S], BF16)
            aqi = [sb("p2_aqi%d" % i, [68, S], F32) for i in range(4)]
            aki = sb("p2_aki", [68, S], F32)
            av = sb("p2_av", [128, NT, 65], BF16)
            awi = sb("p2_awi", [128, NT, 4], F32)
            bc = [[sb("p2_bc%d_%d" % (s_, i), [68, S], BF16) for i in range(4)] for s_ in range(2)]
            bcv = [sb("p2_bcv%d" % s_, [128, NT, 65], BF16) for s_ in range(2)]
            acc4 = sb("p2_acc4", [128, 4, S], F32)
            mneg = [sb("p2_mneg0", [128, 4, S], BF16)] * 2
            junk = sb("p2_junk", [128, S], BF16)
            ones_t = sb("p2_ones", [128, S], BF16)
            zr = sb("p2_zr", [128, S], F32)
            identN = sb("p2_identN", [128, 128], BF16)
            R = [sb("p2_R%d" % i, [128, 512], F32) for i in range(2)]
            Pt = [sb("p2_P%d" % i, [128, 512], BF16) for i in range(3)]
            oTs = [sb("p2_oTs%d" % i, [65, 512], F32) for i in range(2)]
            ostg = [sb("p2_ostg%d" % i, [128, 4, 64], BF16) for i in range(2)]
            sm = sb("p2_sm", [128, 96], F32)
            stp = sb("p2_stp", [128, NIT, 4], F32)
            lamt = sb("p2_lam", [128, 128], F32)
            lamp = sb("p2_lamp", [128, 64], F32)
            gsc = sb("p2_gsc", [128, 64], F32)
            t1 = sb("p2_t1", [128, 4, 64], F32)
            osb = sb("p2_osb", [128, 4, 64], F32)
            sqj = sb("p2_sqj", [128, 64], F32)
            epst = sb("p2_eps", [128, 1], F32)
            pS = [ps("p2_pS%d" % i, [128, 512], F32) for i in range(3)]
            pO = [ps("p2_pO%d" % i, [128, 512], F32) for i in range(2)]
            pT = [ps("p2_pT%d" % i, [128, 512], F32) for i in range(3)]
            s_c = self.dsem("p2c%d" % li)
            s_a = self.dsem("p2a%d" % li)
            s_bc = [self.dsem("p2bc%d_%d" % (li, i)) for i in range(2)]
            s_o = [self.dsem("p2o%d_%d" % (li, i)) for i in range(2)]

            t_eps = POOL.mark(nc.gpsimd.memset(epst[:], LN_EPS))
            ACT.wait(t_eps)
            t_on = POOL.mark(nc.gpsimd.memset(ones_t[:], 1.0))
            DVE.wait(t_on)
            t_idn = DVE.mark(V.tensor_scalar(identN[:], self.ident[:], NEG, None, ALU.mult))
            PE.wait(t_idn)
            for t_ in aqi + [aki]:
                POOL.mark(nc.gpsimd.memset(t_[64:68, :], 0.0))
            for s_ in range(2):
                for t_ in bc[s_]:
                    t_zero = POOL.mark(nc.gpsimd.memset(t_[:, :], 0.0))
            SP.wait(t_zero)
            t_l = self.dma(SP, lamt[:], self.lam_r[li], s_c)
            t_g = self.dma(SP, gsc[:], self.sg_r[li], s_c)
            DVE.wait(t_l, t_g)
            a = DVE.mark(V.tensor_tensor(out=lamp[:, 0:32], in0=lamt[:, 0:32], in1=lamt[:, 32:64], op=ALU.mult))
            a = DVE.mark(V.tensor_tensor(out=lamp[:, 32:64], in0=lamt[:, 64:96], in1=lamt[:, 96:128], op=ALU.mult))
            DVE.wait(a)
            a = DVE.mark(V.tensor_reduce(out=sm[:, 0:2], in_=lamp[:].rearrange("p (a d) -> p a d", a=2),
                                         axis=AX.X, op=ALU.add))
            ACT.wait(a)
            a = ACT.mark(nc.scalar.activation(out=sm[:, 2:4], in_=sm[:, 0:2], func=AF.Exp))
            DVE.wait(a)
            a = DVE.mark(V.tensor_tensor(out=sm[:, 4:5], in0=sm[:, 3:4], in1=sm[:, 2:3], op=ALU.subtract))
            DVE.wait(a)
            t_nlam = DVE.mark(V.tensor_scalar(sm[:, 5:6], sm[:, 4:5], float(lam_init), None, ALU.subtract))
            t_gsc = DVE.mark(V.tensor_scalar(gsc[:], gsc[:], float(1.0 - lam_init), None, ALU.mult))
            nlam = sm[:, 5:6]

            st = dict(si=0, pi=0, oi=0, ti=0, ei=0, gi=0, ri=0)
            pS_free = [None] * 3
            P_free = [None] * 3
            pO_free = [None] * 2
            pT_free = [None] * 3
            oTs_free = [None] * 2
            ostg_free = [None] * 2
            R_free = [None, None]
            mneg_free = [None]
            a_free = None
            bc_free = [None, None]
            pend = []

            def flush_pv(keep):
                while len(pend) > keep:
                    pend.pop(0)()

            def dv(inst):
                t = DVE.mark(inst)
                DVE.wait(t)
                return t

            def emit_unit(G, qT, kT, Kr, vt, mkind, marg, final_fn):
                bt0 = 4 * G
                nb = bt0 + 4
                ob = st["oi"] % 2
                st["oi"] += 1
                for bs in range(nb):
                    j0 = max(0, bs - bt0)
                    c0 = j0 * 128
                    sbk = st["si"] % 3
                    st["si"] += 1
                    PE.wait(pS_free[sbk])
                    ins = nc.tensor.matmul(pS[sbk][:, c0:512], lhsT=kT[0:Kr, bs * 128:(bs + 1) * 128],
                                           rhs=qT[0:Kr, bt0 * 128 + c0:bt0 * 128 + 512], start=True,
                                           stop=(mkind == "B" and bs < bt0))
                    if mkind == "C":
                        ins = nc.tensor.matmul(pS[sbk][:, c0:512], lhsT=self.ident[:],
                                               rhs=self.lc[:, bt0 + j0 - bs:bt0 + 4 - bs, :].rearrange("p d t -> p (d t)"),
                                               start=False, stop=True)
                    elif mkind == "B":
                        if bs >= bt0:
                            j = bs - bt0
                            ins = nc.tensor.matmul(pS[sbk][:, j * 128:(j + 1) * 128], lhsT=self.ident[:],
                                                   rhs=self.lcaus[:], start=False, stop=True)
                    else:
                        for j in range(j0, 4):
                            ins = nc.tensor.matmul(pS[sbk][:, j * 128:(j + 1) * 128],
                                                   lhsT=marg[:, j, bs * 128:(bs + 1) * 128], rhs=identN[:],
                                                   start=False, stop=True)
                    tS = PE.mark(ins)
                    pi = st["pi"] % 3
                    st["pi"] += 1
                    ACT.wait(tS, P_free[pi])
                    tP = ACT.mark(nc.scalar.activation(out=Pt[pi][:, c0:512], in_=pS[sbk][:, c0:512], func=AF.Exp))
                    pS_free[sbk] = tP

                    def pv(bs=bs, c0=c0, pi=pi, tP=tP):
                        PE.wait(tP)
                        if bs == 0:
                            PE.wait(pO_free[ob])
                        tV = PE.mark(nc.tensor.matmul(pO[ob][0:65, c0:512], lhsT=vt[:, bs, :], rhs=Pt[pi][:, c0:512],
                                                      start=(bs == 0), stop=(bs == nb - 1)))
                        P_free[pi] = tV
                        if bs == nb - 1:
                            es = st["ei"] % 2
                            st["ei"] += 1
                            ACT.wait(tV, oTs_free[es])
                            tE = ACT.mark(nc.scalar.copy(out=oTs[es][:, :], in_=pO[ob][0:65, :]))
                            pO_free[ob] = tE

                            def tr():
                                tb = st["ti"] % 3
                                st["ti"] += 1
                                PE.wait(tE, pT_free[tb])
                                for j in range(4):
                                    ins2 = nc.tensor.transpose(pT[tb][:, j * 65:(j + 1) * 65],
                                                               oTs[es][0:65, j * 128:(j + 1) * 128],
                                                               self.identf[0:65, 0:65])
                                tT = PE.mark(ins2)
                                oTs_free[es] = tT
                                final_fn(tb, tT)
                            pend.append(tr)
                    pend.append(pv)
                    flush_pv(2)

            def store_out(G, col, osl, waits):
                dst = self.om[li, cur["sq"], G * 512:(G + 1) * 512, col:col + 64].rearrange("(j p) c -> p j c", p=128)
                with nc.allow_non_contiguous_dma(reason="64-col head slice"):
                    ostg_free[osl] = self.dma(SP, dst, ostg[osl][:], s_o[osl], waits)

            def norm_final(G, col):
                def f(tb, tT):
                    osl = st["gi"] % 2
                    st["gi"] += 1
                    rec = sm[:, 32 + 4 * tb:36 + 4 * tb]
                    DVE.wait(tT)
                    r = DVE.mark(V.reciprocal(out=rec, in_=pT[tb][:, 0:260].rearrange("p (j d) -> p j d", d=65)[:, :, 64]))
                    ACT.wait(r, ostg_free[osl])
                    for j in range(4):
                        tA = ACT.mark(nc.scalar.activation(out=ostg[osl][:, j, :], in_=pT[tb][:, j * 65:j * 65 + 64],
                                                           func=AF.Copy, scale=rec[:, j:j + 1]))
                    pT_free[tb] = tA
                    store_out(G, col, osl, [tA])
                return f

            bstate = {}

            def b_final(G, h, m):
                def f(tb, tT):
                    bstate[m] = (tb, tT)
                    if m == 0:
                        return
                    (b1, tv1), (b2, tv2) = bstate[0], bstate[1]
                    osl = st["gi"] % 2
                    st["gi"] += 1
                    rec1, rec2, nl2, ss, lnv, rstd = (sm[:, 48:52], sm[:, 52:56], sm[:, 56:60], sm[:, 60:64],
                                                      sm[:, 64:68], sm[:, 68:72])
                    v1 = pT[b1][:, 0:260].rearrange("p (j d) -> p j d", d=65)
                    v2 = pT[b2][:, 0:260].rearrange("p (j d) -> p j d", d=65)
                    DVE.wait(tv1, tv2, t_nlam, t_gsc)
                    DVE.mark(V.reciprocal(out=rec1, in_=v1[:, :, 64]))
                    dv(V.reciprocal(out=rec2, in_=v2[:, :, 64]))
                    dv(V.tensor_scalar(nl2, rec2, nlam, None, ALU.mult))
                    for j in range(4):
                        a_ = DVE.mark(V.tensor_scalar(t1[:, j, :], v1[:, j, 0:64], rec1[:, j:j + 1], None, ALU.mult))
                    DVE.wait(a_)
                    pT_free[b1] = a_
                    for j in range(4):
                        a_ = DVE.mark(V.scalar_tensor_tensor(out=osb[:, j, :], in0=v2[:, j, 0:64], scalar=nl2[:, j:j + 1],
                                                             in1=t1[:, j, :], op0=ALU.mult, op1=ALU.add))
                    DVE.wait(a_)
                    pT_free[b2] = a_
                    for j in range(4):
                        a_ = DVE.mark(V.scalar_tensor_tensor(out=sqj[:], in0=osb[:, j, :], scalar=1.0, in1=osb[:, j, :],
                                                             op0=ALU.mult, op1=ALU.mult, accum_out=ss[:, j:j + 1]))
                    ACT.wait(a_)
                    a6 = ACT.mark(nc.scalar.activation(out=lnv, in_=ss, func=AF.Ln, scale=1.0 / 64.0, bias=epst[:, 0:1]))
                    ACT.wait(a6)
                    a7 = ACT.mark(nc.scalar.activation(out=rstd, in_=lnv, func=AF.Exp, scale=-0.5))
                    DVE.wait(a7, ostg_free[osl])
                    for j in range(4):
                        a_ = DVE.mark(V.scalar_tensor_tensor(out=ostg[osl][:, j, :], in0=osb[:, j, :],
                                                             scalar=rstd[:, j:j + 1], in1=gsc[:], op0=ALU.mult,
                                                             op1=ALU.mult))
                    DVE.wait(a_)
                    store_out(G, 256 + 64 * h, osl, [a_])
                return f

            cur = dict(sq=0)

            def idx_and_bisect(G):
                sl = G % 2
                M_ = mneg[sl]
                last = None
                for j in range(4):
                    bt = 4 * G + j
                    nk = (bt + 1) * 128
                    for cidx in range((nk + 511) // 512):
                        n = min(512, nk - cidx * 512)
                        for h in range(4):
                            sbk = st["si"] % 3
                            st["si"] += 1
                            PE.wait(pS_free[sbk])
                            tS = PE.mark(nc.tensor.matmul(pS[sbk][:, 0:n], lhsT=aqi[h][0:68, bt * 128:(bt + 1) * 128],
                                                          rhs=aki[0:68, cidx * 512:cidx * 512 + n], start=True,
                                                          stop=True))
                            dst = acc4[:, j, cidx * 512:cidx * 512 + n]
                            if h == 0:
                                DVE.wait(tS)
                                last = DVE.mark(V.tensor_scalar(dst, pS[sbk][:, 0:n], 0.0, awi[:, bt, 0:1], ALU.max,
                                                                ALU.mult))
                                pS_free[sbk] = last
                            else:
                                rs = st["ri"] % 2
                                st["ri"] += 1
                                ACT.wait(tS, R_free[rs])
                                tR = ACT.mark(nc.scalar.activation(out=R[rs][:, 0:n], in_=pS[sbk][:, 0:n], func=AF.Relu))
                                pS_free[sbk] = tR
                                DVE.wait(tR, last)
                                last = DVE.mark(V.scalar_tensor_tensor(out=dst, in0=R[rs][:, 0:n],
                                                                       scalar=awi[:, bt, h:h + 1], in1=dst,
                                                                       op0=ALU.mult, op1=ALU.add))
                                R_free[rs] = last
                yield
                am, Aa, mid, cnt, gg, tt = (sm[:, 8:12], sm[:, 12:16], sm[:, 16:20], sm[:, 20:24], sm[:, 24:28],
                                            sm[:, 28:32])
                need = sm[:, 72:76]
                nks = [(4 * G + j + 1) * 128 for j in range(4)]
                DVE.wait(last)
                for j in range(4):
                    a_ = DVE.mark(V.tensor_reduce(out=am[:, j:j + 1], in_=acc4[:, j, 0:nks[j]], axis=AX.X, op=ALU.max,
                                                  apply_absolute_value=True))
                DVE.wait(a_)
                dv(V.tensor_scalar(Aa, am, 1.0001, 1e-30, ALU.mult, ALU.add))
                for j in range(4):
                    DVE.mark(V.tensor_scalar(stp[:, :, j], self.pow2[:], Aa[:, j:j + 1], None, ALU.mult))
                    DVE.mark(V.tensor_tensor(out=acc4[:, j, nks[j] - 128:nks[j]], in0=acc4[:, j, nks[j] - 128:nks[j]],
                                             in1=self.causf[:], op=ALU.add))
                dv(V.memset(mid, 0.0))
                yield
                for k in range(NIT):
                    if k:
                        yield
                    for j in range(4):
                        a_ = DVE.mark(V.tensor_scalar(junk[:, 0:nks[j]], acc4[:, j, 0:nks[j]], mid[:, j:j + 1], 0.0,
                                                      ALU.is_gt, ALU.add, accum_out=cnt[:, j:j + 1]))
                    DVE.wait(a_)
                    if k == 0:
                        dv(V.tensor_scalar(need, cnt, -1.0, 256.0, ALU.mult, ALU.add))
                        dv(V.tensor_scalar(need, need, 0.0, None, ALU.max))
                    dv(V.tensor_scalar(gg, cnt, 255.5, 0.5 if k < NIT - 1 else 1.0, ALU.is_ge, ALU.subtract))
                    dv(V.tensor_tensor(out=tt, in0=gg, in1=stp[:, k, :], op=ALU.mult))
                    dv(V.tensor_tensor(out=mid, in0=mid, in1=tt, op=ALU.add))
                DVE.wait(mneg_free[0])
                for j in range(4):
                    n_ = nks[j]
                    yield
                    dv(V.tensor_scalar(junk[:, 0:n_], acc4[:, j, 0:n_], 0.0, None, ALU.is_equal))
                    dv(V.tensor_tensor_scan(out=zr[:, 0:n_], data0=ones_t[:, 0:n_], data1=junk[:, 0:n_], initial=0.0,
                                            op0=ALU.mult, op1=ALU.add))
                    dv(V.scalar_tensor_tensor(out=junk[:, 0:n_], in0=zr[:, 0:n_], scalar=need[:, j:j + 1],
                                              in1=junk[:, 0:n_], op0=ALU.is_gt, op1=ALU.mult))
                    tM = dv(V.scalar_tensor_tensor(out=M_[:, j, 0:n_], in0=acc4[:, j, 0:n_], scalar=mid[:, j:j + 1],
                                                   in1=junk[:, 0:n_], op0=ALU.is_le, op1=ALU.max))
                tmn[G] = tM

            for sq in range(self.nseq):
                cur["sq"] = sq
                zq = self.zq[li, sq]
                zv = self.zv[li, sq].rearrange("(i p) (h d) -> p i h d", p=128, d=65)
                for h in range(4):
                    self.dma(SP, aq[h][0:64, :], zq[C_QA + 64 * h:C_QA + 64 * (h + 1), :], s_a, [a_free])
                    self.dma(SP, aq[h][64:68, :], self.qaug_b[2 * h + 1], s_a)
                    self.dma(SP, aqi[h][0:64, :], self.zqf[li, sq, 64 * h:64 * (h + 1), :], s_a)
                self.dma(SP, ak[0:64, :], zq[C_KA:C_KA + 64, :], s_a)
                self.dma(SP, ak[64:68, :], self.kaug_b, s_a)
                self.dma(SP, aki[0:64, :], self.zqf[li, sq, 256:320, :], s_a)
                self.dma(SP, av[:], zv[:, :, 0, :], s_a)
                t_la = self.dma(SP, awi[:], self.zw[li, sq].rearrange("(i p) h -> p i h", p=128), s_a)

                jobs = [("B", h) for h in range(4)] + [("C", h) for h in range(8)]
                job_tok = {}

                def load_job(ji):
                    kind, h = jobs[ji]
                    sl = ji % 2
                    T = bc[sl]
                    w = [bc_free[sl]]
                    if kind == "B":
                        for m in range(2):
                            c0 = C_QB + h * 64 + m * 32
                            self.dma(SP, T[2 * m][0:32, :], zq[c0:c0 + 32, :], s_bc[sl], w)
                            self.dma(SP, T[2 * m][32:36, :], self.qaug_b[2 * h + 1], s_bc[sl])
                            c0 = C_KB + h * 64 + m * 32
                            self.dma(SP, T[2 * m + 1][0:32, :], zq[c0:c0 + 32, :], s_bc[sl])
                            self.dma(SP, T[2 * m + 1][32:36, :], self.kaug_b, s_bc[sl])
                        job_tok[ji] = self.dma(SP, bcv[sl][:], zv[:, :, 1 + h, :], s_bc[sl])
                    else:
                        self.dma(SP, T[0][0:64, :], zq[C_QC + 64 * h:C_QC + 64 * (h + 1), :], s_bc[sl], w)
                        self.dma(SP, T[0][64:68, :], self.qaug_b[h], s_bc[sl])
                        self.dma(SP, T[2][0:64, :], zq[C_KC + 64 * h:C_KC + 64 * (h + 1), :], s_bc[sl])
                        self.dma(SP, T[2][64:68, :], self.kaug_b, s_bc[sl])
                        job_tok[ji] = self.dma(SP, bcv[sl][:], zv[:, :, 5 + h, :], s_bc[sl])

                load_job(0)
                load_job(1)

                PE.wait(t_la, t_zero)
                DVE.wait(t_la)
                tmn = {}

                def step(gen, n):
                    for _ in range(n):
                        try:
                            next(gen)
                        except StopIteration:
                            return

                for ji, (kind, h) in enumerate(jobs):
                    sl = ji % 2
                    T = bc[sl]
                    gen = idx_and_bisect(h) if kind == "B" else iter(())
                    step(gen, 1)
                    PE.wait(job_tok[ji])
                    for G in range(4):
                        if kind == "B":
                            for m in range(2):
                                emit_unit(G, T[2 * m], T[2 * m + 1], 68, bcv[sl], "B", None, b_final(G, h, m))
                                step(gen, 2)
                        else:
                            emit_unit(G, T[0], T[2], 68, bcv[sl], "C", None, norm_final(G, 512 + 64 * h))
                    step(gen, 1000)
                    flush_pv(0)
                    bc_free[sl] = PE.last
                    if ji + 2 < len(jobs):
                        load_job(ji + 2)
                    if kind == "B":
                        G = h
                        PE.wait(tmn[G])
                        for hh in range(4):
                            emit_unit(G, aq[hh], ak, 68, av, "A", mneg[G % 2], norm_final(G, 64 * hh))
                        flush_pv(0)
                        mneg_free[0] = PE.last
                        if G == 3:
                            a_free = PE.last

    def phase3(self, li, xsrc, ydst):
        nc = self.nc
        PE, ACT, DVE, POOL, SP = self.PE, self.ACT, self.DVE, self.POOL, self.SP
        with ExitStack() as c:
            sb = lambda n, s, d: c.enter_context(nc.sbuf_tensor("L%d_%s" % (li, n), list(s), d))
            ps = lambda n, s, d: c.enter_context(nc.psum_tensor("L%d_%s" % (li, n), list(s), d))
            wo = sb("p3_wo", [128, 8, D], BF16)
            wd = sb("p3_wd", [128, NFF, D], BF16)
            lnc = sb("p3_ln", [128, 4, D], F32)
            wgu = [sb("p3_wgu%d" % i, [128, 2, 1024], BF16) for i in range(3)]
            ot = [sb("p3_ot%d" % i, [128, D], BF16) for i in range(2)]
            xt = [sb("p3_xt%d" % i, [128, D], F32) for i in range(2)]
            oT = sb("p3_oT", [128, 8, 128], BF16)
            r = sb("p3_r", [128, D], F32)
            x1 = [sb("p3_x1_%d" % i, [128, D], F32) for i in range(4)]
            x1T = sb("p3_x1T", [128, 8, 512], BF16)
            hT = sb("p3_hT", [128, NFF, 512], BF16)
            sg = [sb("p3_sg%d" % i, [128, 512], F32) for i in range(2)]
            yt = [sb("p3_yt%d" % i, [128, D], F32) for i in range(2)]
            stt = sb("p3_stt", [128, 2, 6], F32)
            sm = sb("p3_sm", [128, 16], F32)
            eps = sb("p3_eps", [128, 1], F32)
            pOT = ps("p3_pOT", [128, 1024], BF16)
            pXT = ps("p3_pXT", [128, 1024], F32)
            pM = ps("p3_pM", [128, 1024], F32)
            pG = [ps("p3_pG%d" % i, [128, 512], F32) for i in range(3)]
            s_w = self.dsem("p3w%d" % li)
            s_o = [self.dsem("p3o%d_%d" % (li, i)) for i in range(2)]
            s_x = [self.dsem("p3x%d_%d" % (li, i)) for i in range(2)]
            s_g = [self.dsem("p3g%d_%d" % (li, i)) for i in range(3)]
            s_y = [self.dsem("p3y%d_%d" % (li, i)) for i in range(2)]
            t_w = [self.dma(SP, wo[:], self.wob[li].rearrange("(k p) n -> p k n", p=128), s_w),
                   self.dma(SP, wd[:], self.wdb[li].rearrange("(k p) n -> p k n", p=128), s_w),
                   self.dma(SP, lnc[:], self.ln_r[li].rearrange("a p n -> p a n"), s_w)]
            t_eps = POOL.mark(nc.gpsimd.memset(eps[:], LN_EPS))
            ACT.wait(t_eps)
            V = nc.vector

            def dv(inst):
                t = DVE.mark(inst)
                DVE.wait(t)
                return t

            def layer_norm(src, dst, gi):
                dv(V.bn_stats(out=stt[:, 0, :], in_=src[:, 0:512]))
                dv(V.bn_stats(out=stt[:, 1, :], in_=src[:, 512:1024]))
                a = dv(V.bn_aggr(out=sm[:, 0:2], in_=stt[:].rearrange("p a s -> p (a s)")))
                ACT.wait(a)
                a = ACT.mark(nc.scalar.activation(out=sm[:, 2:3], in_=sm[:, 1:2], func=AF.Sqrt, bias=eps[:, 0:1],
                                                  scale=1.0))
                DVE.wait(a)
                dv(V.reciprocal(out=sm[:, 3:4], in_=sm[:, 2:3]))
                dv(V.tensor_scalar(dst, src, sm[:, 0:1], sm[:, 3:4], ALU.subtract, ALU.mult))
                dv(V.tensor_tensor(out=dst, in0=dst, in1=lnc[:, gi, :], op=ALU.mult))
                return dv(V.tensor_tensor(out=dst, in0=dst, in1=lnc[:, gi + 1, :], op=ALU.add))

            ot_free = [None, None]
            xt_free = [None, None]
            wgu_free = [None] * 3
            pG_free = [None] * 3
            sg_free = [None, None]
            yt_free = [None, None]
            oT_free = None
            pOT_free = None
            pXT_free = None
            pM_free = None
            x1T_free = None
            hT_free = None
            r_free = None
            gi_ = 0
            ci_ = 0
            for sq in range(self.nseq):
                for grp in range(4):
                    x1_tok = []
                    for tl in range(4):
                        i = grp * 4 + tl
                        sl = i % 2
                        t_o = self.dma(SP, ot[sl][:], self.om[li, sq, i * 128:(i + 1) * 128, :], s_o[sl], [ot_free[sl]])
                        t_x = self.dma(SP, xt[sl][:], xsrc[sq, i * 128:(i + 1) * 128, :], s_x[sl], [xt_free[sl]])
                        PE.wait(t_o, pOT_free)
                        for k in range(8):
                            ins = nc.tensor.transpose(pOT[:, k * 128:(k + 1) * 128], ot[sl][:, k * 128:(k + 1) * 128],
                                                      self.ident[:])
                        tT = PE.mark(ins)
                        ot_free[sl] = tT
                        ACT.wait(tT, oT_free)
                        tC = ACT.mark(nc.scalar.copy(out=oT[:].rearrange("p k t -> p (k t)"), in_=pOT[:]))
                        pOT_free = tC
                        PE.wait(tC, pM_free, t_w)
                        for half in range(2):
                            for k in range(8):
                                ins = nc.tensor.matmul(pM[:, half * 512:(half + 1) * 512], lhsT=oT[:, k, :],
                                                       rhs=wo[:, k, half * 512:(half + 1) * 512], start=(k == 0),
                                                       stop=(k == 7))
                        tM = PE.mark(ins)
                        oT_free = tM
                        DVE.wait(tM, t_x, r_free)
                        a = dv(V.scalar_tensor_tensor(out=r[:], in0=xt[sl][:], scalar=float(ALPHA), in1=pM[:],
                                                      op0=ALU.mult, op1=ALU.add))
                        pM_free = a
                        xt_free[sl] = a
                        a = layer_norm(r[:], x1[tl][:], 0)
                        r_free = a
                        PE.wait(a, pXT_free)
                        for k in range(8):
                            ins = nc.tensor.transpose(pXT[:, k * 128:(k + 1) * 128], x1[tl][:, k * 128:(k + 1) * 128],
                                                      self.identf[:])
                        tT = PE.mark(ins)
                        ACT.wait(tT, x1T_free)
                        tC = ACT.mark(nc.scalar.copy(out=x1T[:, :, tl * 128:(tl + 1) * 128],
                                                     in_=pXT[:].rearrange("p (k t) -> p k t", k=8)))
                        pXT_free = tC
                        x1_tok.append(tC)
                    for cidx in range(NFF):
                        ws = ci_ % 3
                        ci_ += 1
                        t_g = self.dma(SP, wgu[ws][:], self.wgub[li, cidx], s_g[ws], [wgu_free[ws]])
                        bg = gi_ % 3
                        bu = (gi_ + 1) % 3
                        gi_ += 2
                        PE.wait(t_g, x1_tok, pG_free[bg], pG_free[bu])
                        for (bank, j) in ((bg, 0), (bu, 1)):
                            for k in range(8):
                                ins = nc.tensor.matmul(pG[bank][:, :], lhsT=wgu[ws][:, j, k * 128:(k + 1) * 128],
                                                       rhs=x1T[:, k, :], start=(k == 0), stop=(k == 7))
                        tM = PE.mark(ins)
                        wgu_free[ws] = tM
                        ss = cidx % 2
                        ACT.wait(tM, sg_free[ss])
                        tS = ACT.mark(nc.scalar.activation(out=sg[ss][:], in_=pG[bg][:, :], func=AF.Silu))
                        pG_free[bg] = tS
                        DVE.wait(tS, hT_free if cidx == 0 else None)
                        tH = DVE.mark(V.tensor_tensor(out=hT[:, cidx, :], in0=sg[ss][:], in1=pG[bu][:, :], op=ALU.mult))
                        pG_free[bu] = tH
                        sg_free[ss] = tH
                    x1T_free = PE.last
                    for tl in range(4):
                        i = grp * 4 + tl
                        ys = i % 2
                        PE.wait(tH, pM_free)
                        for cidx in range(NFF):
                            for half in range(2):
                                ins = nc.tensor.matmul(pM[:, half * 512:(half + 1) * 512],
                                                       lhsT=hT[:, cidx, tl * 128:(tl + 1) * 128],
                                                       rhs=wd[:, cidx, half * 512:(half + 1) * 512], start=(cidx == 0),
                                                       stop=(cidx == NFF - 1))
                        tM = PE.mark(ins)
                        DVE.wait(tM, r_free)
                        a = dv(V.scalar_tensor_tensor(out=r[:], in0=x1[tl][:], scalar=float(ALPHA), in1=pM[:],
                                                      op0=ALU.mult, op1=ALU.add))
                        pM_free = a
                        DVE.wait(yt_free[ys])
                        a = layer_norm(r[:], yt[ys][:], 2)
                        r_free = a
                        yt_free[ys] = self.dma(SP, ydst[sq, i * 128:(i + 1) * 128, :], yt[ys][:], s_y[ys], [a])
                    hT_free = PE.last


def make_consts():
    t = np.arange(128)
    lc = np.zeros((16, 128, 128), np.float32)
    for dl in range(16):
        dist = 128 * dl + t[None, :] - t[:, None]
        m = (((dist >= 0) & (dist <= 128)).astype(np.int64)
             + ((dist >= 0) & (dist <= 512) & (dist % 4 == 0))
             + ((dist >= 0) & (dist <= 2048) & (dist % 16 == 0)))
        lc[dl] = np.where(m > 0, np.log(np.maximum(m, 1)), NEG)
    pos = np.arange(S)
    rs, bs = (pos % 128).astype(np.float32), (pos // 128).astype(np.float32)
    kaug = np.stack([rs, np.ones(S, np.float32), bs, np.ones(S, np.float32)])
    qaug = np.zeros((8, 4, S), np.float32)
    for j in range(8):
        sl = 2.0 ** -(j + 1)
        qaug[j] = np.stack([np.full(S, sl, np.float32), -sl * rs, np.full(S, 128 * sl, np.float32), -128 * sl * bs])
    ident = np.eye(128, dtype=np.float32)
    causf = np.where(t[None, :] > t[:, None], -1e30, 0.0).astype(np.float32)
    lcaus = np.where(t[:, None] > t[None, :], NEG, 0.0).astype(np.float32)
    pow2 = np.tile((2.0 ** -np.arange(NIT)).astype(np.float32)[None, :], (128, 1))
    return dict(c_lc=lc, c_kaug=kaug, c_qaug=qaug, c_ident=ident, c_causf=causf, c_lcaus=lcaus, c_pow2=pow2)


_CACHE = {}


def get_nc(nl, nseq, lam_inits, debug=False):
    key = (nl, nseq, tuple(lam_inits), debug)
    if key not in _CACHE:
        _CACHE[key] = K(nl, nseq, lam_inits, debug).build()
    return _CACHE[key]


def layer_inputs(layers, w_in, w_o, lam, subln_g, ln1_g, ln1_b, w_gate, w_up, w_down, ln2_g, ln2_b):
    f = lambda a: np.ascontiguousarray(np.asarray(a, dtype=np.float32))
    L = list(layers)
    rep = lambda v: np.ascontiguousarray(np.broadcast_to(np.asarray(v, np.float32)[None, :], (128, v.shape[-1])))
    d = dict(w_in=f(w_in[L]), w_o=f(w_o[L]), w_gate=f(w_gate[L]), w_up=f(w_up[L]), w_down=f(w_down[L]))
    d["lam_r"] = np.stack([rep(np.asarray(lam[l]).reshape(-1)) for l in L])
    d["sg_r"] = np.stack([rep(np.asarray(subln_g[l])) for l in L])
    d["ln_r"] = np.stack([np.stack([rep(np.asarray(v[l])) for v in (ln1_g, ln1_b, ln2_g, ln2_b)]) for l in L])
    d.update(make_consts())
    return d


def lam_init_of(l):
    return 0.8 - 0.6 * math.exp(-0.3 * l)


def kernel(x, w_in, w_o, lam, subln_g, ln1_g, ln1_b, w_gate, w_up, w_down, ln2_g, ln2_b):
    x = np.ascontiguousarray(np.asarray(x, dtype=np.float32))
    args = (w_in, w_o, lam, subln_g, ln1_g, ln1_b, w_gate, w_up, w_down, ln2_g, ln2_b)
    args = tuple(np.asarray(a) for a in args)
    nl = 2
    nc = get_nc(nl, SEQ_PER_CORE, [lam_init_of(l) for l in range(nl)])
    shared = layer_inputs(range(nl), *args)
    in_maps = []
    for c in range(N_CORES):
        m = dict(shared)
        m["x"] = x[c * SEQ_PER_CORE:(c + 1) * SEQ_PER_CORE]
        in_maps.append(m)
    res = run_bass_kernel_spmd(nc, in_maps, core_ids=list(range(N_CORES)))
    return np.concatenate([np.asarray(r["y"]) for r in res.results], axis=0).astype(np.float32)
```

```python
import math
from contextlib import ExitStack

import numpy as np
import concourse.bass as bass
import concourse.mybir as mybir
from concourse.bass_utils import run_bass_kernel_spmd

F32 = mybir.dt.float32
BF16 = mybir.dt.bfloat16
AF = mybir.ActivationFunctionType
ALU = mybir.AluOpType
AX = mybir.AxisListType

S = 2048
D = 1024
NT = 16
FF = 2816
NFF = 22
INC = 3012
ALPHA = 4.0 ** 0.25
LN_EPS = 1e-5
NIT = 14
NEG = -30000.0
N_CORES = 8
SEQ_PER_CORE = 4

C_QA, C_KA, C_VA, C_QI, C_KI, C_WI, C_QB, C_KB, C_VB, C_QC, C_KC, C_VC = (
    0, 256, 320, 384, 640, 704, 708, 964, 1220, 1476, 1988, 2500)


class Sem:
    def __init__(self, h):
        self.h = h
        self.n = 0


class Eng:
    def __init__(self, k, eng, name):
        self.e = eng
        self.sem = Sem(k.new_sem("e_" + name))
        self.seen = {}
        self.last = None

    def wait(self, *toks):
        for t in toks:
            if t is None:
                continue
            if isinstance(t, list):
                self.wait(*t)
                continue
            s, v = t
            if self.seen.get(s, 0) >= v:
                continue
            self.e.wait_ge(s.h, v)
            self.seen[s] = v

    def mark(self, inst):
        self.sem.n += 1
        inst.then_inc(self.sem.h, 1)
        self.last = (self.sem, self.sem.n)
        return self.last


class K:
    def __init__(self, nl, nseq, lam_inits, debug=False):
        self.nl, self.nseq, self.lam_inits, self.debug = nl, nseq, lam_inits, debug
        self.nc = bass.Bass("TRN2", target_bir_lowering=False)
        self.ctx = ExitStack()
        self.dsems = []
        self.sem_pool = []
        self.in_use = []

    def new_sem(self, name):
        return self.ctx.enter_context(self.nc.semaphore(name))

    def dsem(self, name):
        if self.sem_pool:
            s = self.sem_pool.pop()
        else:
            s = Sem(self.new_sem("d%d" % len(self.dsems)))
            self.dsems.append(s)
        self.in_use.append(s)
        return s

    def dram(self, name, shape, dt, kind="Internal"):
        return self.nc.dram_tensor(name, list(shape), dt, kind=kind).ap()

    def dma(self, q, out, in_, sem, waits=(), **kw):
        q.wait(*waits)
        inst = q.e.dma_start(out=out, in_=in_, **kw)
        sem.n += 16
        inst.then_inc(sem.h, 16)
        return (sem, sem.n)

    def barrier(self):
        sp = self.SP
        for e in self.engs:
            if e is not sp:
                sp.wait(e.last)
        for s in self.dsems:
            if s.n:
                sp.wait((s, s.n))
        tok = self.dma(sp, self.bar_b[:], self.bar_a[:], self.bar_sem)
        for e in self.engs:
            e.wait(tok)
        self.sem_pool.extend(self.in_use)
        self.in_use = []

    def build(self):
        nc, nl, nseq = self.nc, self.nl, self.nseq
        ctx = self.ctx
        dbg = "ExternalOutput" if self.debug else "Internal"
        ein = lambda n, s: self.dram(n, s, F32, "ExternalInput")
        self.x_in = ein("x", [nseq, S, D])
        self.w_in = ein("w_in", [nl, D, INC])
        self.w_o = ein("w_o", [nl, D, D])
        self.w_g = ein("w_gate", [nl, D, FF])
        self.w_u = ein("w_up", [nl, D, FF])
        self.w_d = ein("w_down", [nl, FF, D])
        self.lam_r = ein("lam_r", [nl, 128, 128])
        self.sg_r = ein("sg_r", [nl, 128, 64])
        self.ln_r = ein("ln_r", [nl, 4, 128, D])
        self.c_lc = ein("c_lc", [16, 128, 128])
        self.c_kaug = ein("c_kaug", [4, S])
        self.c_qaug = ein("c_qaug", [8, 4, S])
        self.c_ident = ein("c_ident", [128, 128])
        self.c_causf = ein("c_causf", [128, 128])
        self.c_lcaus = ein("c_lcaus", [128, 128])
        self.c_pow2 = ein("c_pow2", [128, NIT])
        self.y_out = self.dram("y", [nseq, S, D], F32, "ExternalOutput")
        self.w1b = self.dram("w1b", [nl, D, INC], BF16)
        self.wob = self.dram("wob", [nl, D, D], BF16)
        self.wgub = self.dram("wgub", [nl, NFF, 128, 2, 1024], BF16)
        self.wdb = self.dram("wdb", [nl, FF, D], BF16)
        self.kaug_b = self.dram("kaug_b", [4, S], BF16)
        self.qaug_b = self.dram("qaug_b", [8, 4, S], BF16)
        self.zq = self.dram("zq", [nl, nseq, INC, S], BF16, dbg)
        self.zv = self.dram("zv", [nl, nseq, S, 13 * 65], BF16, dbg)
        self.zw = self.dram("zw", [nl, nseq, S, 4], F32, dbg)
        self.zqf = self.dram("zqf", [nl, nseq, 320, S], F32, dbg)
        self.om = self.dram("om", [nl, nseq, S, D], BF16, dbg)
        self.xmid = [self.dram("xmid%d" % i, [nseq, S, D], F32, dbg) for i in range(nl - 1)]

        with ctx:
            self.PE = Eng(self, nc.tensor, "pe")
            self.ACT = Eng(self, nc.scalar, "act")
            self.DVE = Eng(self, nc.vector, "dve")
            self.POOL = Eng(self, nc.gpsimd, "pool")
            self.SP = Eng(self, nc.sync, "sp")
            self.engs = [self.PE, self.ACT, self.DVE, self.POOL, self.SP]
            self.bar_sem = Sem(self.new_sem("bar"))
            sb = lambda n, s, d: ctx.enter_context(nc.sbuf_tensor(n, list(s), d))
            self.bar_a = sb("bar_a", [1, 8], F32)
            self.bar_b = sb("bar_b", [1, 8], F32)
            self.ident = sb("ident", [128, 128], BF16)
            self.identf = sb("identf", [128, 128], F32)
            self.lcaus = sb("lcaus", [128, 128], BF16)
            self.causf = sb("causf", [128, 128], F32)
            self.pow2 = sb("pow2", [128, NIT], F32)
            self.lc = sb("lc", [128, 16, 128], BF16)
            self.phase0()
            self.barrier()
            for li in range(nl):
                xsrc = self.x_in if li == 0 else self.xmid[li - 1]
                ydst = self.y_out if li == nl - 1 else self.xmid[li]
                self.phase1(li, xsrc)
                self.barrier()
                self.phase2(li)
                self.barrier()
                self.phase3(li, xsrc, ydst)
                self.barrier()
        return nc

    def phase0(self):
        nc, nl = self.nc, self.nl
        P, SP = self.POOL, self.SP
        sw = self.dsem("w0")
        ncd = [0]

        def cd(o, i):
            ncd[0] += 1
            t = self.dma(P, o, i, sw, max_dma_last_dim=4096)
            if ncd[0] % 6 == 0:
                P.wait(t)
            return t
        POOLm = lambda inst: P.mark(inst)
        POOLm(nc.gpsimd.memset(self.bar_a[:], 0.0))
        cd(self.ident[:], self.c_ident)
        cd(self.lcaus[:], self.c_lcaus)
        cd(self.lc[:], self.c_lc.rearrange("d s t -> s d t"))
        cd(self.kaug_b, self.c_kaug)
        cd(self.qaug_b, self.c_qaug)
        self.dma(SP, self.identf[:], self.c_ident, sw)
        self.dma(SP, self.causf[:], self.c_causf, sw)
        self.dma(SP, self.pow2[:], self.c_pow2, sw)
        for l in range(nl):
            for kc in range(8):
                cd(self.w1b[l, kc * 128:(kc + 1) * 128, :], self.w_in[l, kc * 128:(kc + 1) * 128, :])
            for h in range(2):
                cd(self.wob[l, h * 512:(h + 1) * 512, :], self.w_o[l, h * 512:(h + 1) * 512, :])
            for c in range(NFF):
                for j, w in enumerate((self.w_g, self.w_u)):
                    cd(self.wgub[l, c, :, j, :].rearrange("p (k f) -> p k f", k=8),
                       w[l, :, c * 128:(c + 1) * 128].rearrange("(k p) f -> p k f", p=128))
            for h in range(NFF):
                cd(self.wdb[l, h * 128:(h + 1) * 128, :], self.w_d[l, h * 128:(h + 1) * 128, :])

    def phase1(self, li, xsrc):
        nc = self.nc
        PE, ACT, DVE, POOL, SP = self.PE, self.ACT, self.DVE, self.POOL, self.SP
        with ExitStack() as c:
            sb = lambda n, s, d: c.enter_context(nc.sbuf_tensor("L%d_%s" % (li, n), list(s), d))
            ps = lambda n, s, d: c.enter_context(nc.psum_tensor("L%d_%s" % (li, n), list(s), d))
            w1 = sb("p1_w1", [128, 8, INC], BF16)
            xT = sb("p1_xT", [128, 8, S], BF16)
            xt = [sb("p1_xt%d" % i, [128, D], F32) for i in range(2)]
            zst = [sb("p1_zst%d" % i, [128, S], BF16) for i in range(2)]
            vst = [sb("p1_vst%d" % i, [128, 13, 65], BF16) for i in range(2)]
            wst4 = [sb("p1_wst4_%d" % i, [128, 4, 4], F32) for i in range(2)]
            xTf = [sb("p1_xTf%d" % i, [128, 8, 512], F32) for i in range(2)]
            w1f = sb("p1_w1f", [128, 8, 388], F32)
            zstf = [sb("p1_zstf%d" % i, [128, 512], F32) for i in range(3)]
            pX = [ps("p1_pX%d" % i, [128, 1024], F32) for i in range(2)]
            pz = [ps("p1_pz%d" % i, [128, 512], F32) for i in range(4)]
            s_w = self.dsem("p1w%d" % li)
            s_x = [self.dsem("p1x%d_%d" % (li, i)) for i in range(2)]
            s_z = [self.dsem("p1z%d_%d" % (li, i)) for i in range(2)]
            s_v = [self.dsem("p1v%d_%d" % (li, i)) for i in range(2)]
            s_f = [self.dsem("p1f%d_%d" % (li, i)) for i in range(3)]
            s_w4 = [self.dsem("p1w4%d_%d" % (li, i)) for i in range(2)]
            t_w = self.dma(SP, w1[:], self.w1b[li].rearrange("(k p) n -> p k n", p=128), s_w)
            t_pad = POOL.mark(nc.gpsimd.memset(w1f[:, :, 320:384], 0.0))
            wsrc = self.w_in[li].rearrange("(k p) n -> p k n", p=128)
            self.dma(SP, w1f[:, :, 0:320], wsrc[:, :, C_QI:C_QI + 320], s_w)
            t_wf = self.dma(SP, w1f[:, :, 384:388], wsrc[:, :, C_WI:C_WI + 4], s_w)
            xTf_free = [None, None]
            zstf_free = [None] * 3
            wst4_free = [None, None]
            fi = 0
            t_ones = [POOL.mark(nc.gpsimd.memset(vst[i][:, :, 64:65], 1.0)) for i in range(2)]
            xt_free = [None, None]
            pX_free = [None, None]
            pz_free = [None] * 4
            zst_free = [None, None]
            vst_free = [None, None]
            xT_free = None
            pzi = 0
            evi = 0

            def ev_copy(out, in_, scale, waits):
                nonlocal evi
                evi += 1
                if evi % 2 == 0:
                    ACT.wait(*waits)
                    if scale == 1.0:
                        return ACT.mark(nc.scalar.copy(out=out, in_=in_))
                    return ACT.mark(nc.scalar.activation(out=out, in_=in_, func=AF.Copy, scale=float(scale)))
                DVE.wait(*waits)
                if scale == 1.0:
                    return DVE.mark(nc.vector.tensor_copy(out=out, in_=in_))
                return DVE.mark(nc.vector.tensor_scalar(out, in_, float(scale), None, ALU.mult))

            fm_groups = ([(C_QA + 128 * i, 128, 0.125) for i in range(2)] + [(C_KA, 64, 1.0)]
                         + [(C_QB + 128 * i, 128, 32.0 ** -0.5) for i in range(2)]
                         + [(C_KB + 128 * i, 128, 1.0) for i in range(2)]
                         + [(C_QC + 128 * i, 128, 0.125) for i in range(4)]
                         + [(C_KC + 128 * i, 128, 1.0) for i in range(4)])
            for sq in range(self.nseq):
                tokX = []
                for i in range(NT):
                    sl = i % 2
                    t_ld = self.dma(SP, xt[sl][:], xsrc[sq, i * 128:(i + 1) * 128, :], s_x[sl], [xt_free[sl]])
                    PE.wait(t_ld, pX_free[sl])
                    for k in range(8):
                        ins = nc.tensor.transpose(pX[sl][:, k * 128:(k + 1) * 128], xt[sl][:, k * 128:(k + 1) * 128],
                                                  self.identf[:])
                    tT = PE.mark(ins)
                    xt_free[sl] = tT
                    ch, jj = i // 4, i % 4
                    xs = ch % 2
                    E1, E2 = (ACT, DVE) if i % 2 == 0 else (DVE, ACT)
                    cpy = lambda E, o, i_: E.mark(nc.scalar.copy(out=o, in_=i_) if E is ACT
                                                  else nc.vector.tensor_copy(out=o, in_=i_))
                    E1.wait(tT, xTf_free[xs])
                    tXf = cpy(E1, xTf[xs][:, :, jj * 128:(jj + 1) * 128], pX[sl][:].rearrange("p (k t) -> p k t", k=8))
                    pX_free[sl] = tXf
                    E2.wait(tXf, xT_free)
                    tX = cpy(E2, xT[:, :, i * 128:(i + 1) * 128], xTf[xs][:, :, jj * 128:(jj + 1) * 128])
                    tokX.append(tX)
                    if jj == 3:
                        PE.wait(t_wf, t_pad, ACT.last, DVE.last)
                        for (c0f, M, scale) in ((0, 128, 0.125), (128, 128, 0.125), (256, 128, 1.0)):
                            Mo = 64 if c0f == 256 else 128
                            b = pzi % 4
                            pzi += 1
                            PE.wait(pz_free[b])
                            for k in range(8):
                                ins = nc.tensor.matmul(pz[b][0:M, :], lhsT=w1f[:, k, c0f:c0f + M], rhs=xTf[xs][:, k, :],
                                                       start=(k == 0), stop=(k == 7))
                            tM = PE.mark(ins)
                            fs = fi % 3
                            fi += 1
                            te = ev_copy(zstf[fs][0:Mo, :], pz[b][0:Mo, :], scale, [tM, zstf_free[fs]])
                            pz_free[b] = te
                            zstf_free[fs] = self.dma(SP, self.zqf[li, sq, c0f:c0f + Mo, ch * 512:(ch + 1) * 512],
                                                     zstf[fs][0:Mo, :], s_f[fs], [te])
                        b = pzi % 4
                        pzi += 1
                        PE.wait(pz_free[b])
                        for j4 in range(4):
                            for k in range(8):
                                ins = nc.tensor.matmul(pz[b][:, j4 * 4:(j4 + 1) * 4],
                                                       lhsT=xTf[xs][:, k, j4 * 128:(j4 + 1) * 128],
                                                       rhs=w1f[:, k, 384:388], start=(k == 0), stop=(k == 7))
                        tM = PE.mark(ins)
                        xTf_free[xs] = tM
                        ws = ch % 2
                        te = ev_copy(wst4[ws][:].rearrange("p j h -> p (j h)"), pz[b][:, 0:16], 0.5,
                                     [tM, wst4_free[ws]])
                        pz_free[b] = te
                        wst4_free[ws] = self.dma(SP, self.zw[li, sq, ch * 512:(ch + 1) * 512, :].rearrange(
                            "(j p) h -> p j h", p=128), wst4[ws][:], s_w4[ws], [te])
                zi = 0
                for (c0, M, scale) in fm_groups:
                    zs = zi % 2
                    zi += 1
                    tE = []
                    for tc in range(4):
                        b = pzi % 4
                        pzi += 1
                        PE.wait(t_w, pz_free[b], tokX[tc * 4:(tc + 1) * 4])
                        for k in range(8):
                            ins = nc.tensor.matmul(pz[b][0:M, :], lhsT=w1[:, k, c0:c0 + M],
                                                   rhs=xT[:, k, tc * 512:(tc + 1) * 512], start=(k == 0), stop=(k == 7))
                        tM = PE.mark(ins)
                        te = ev_copy(zst[zs][0:M, tc * 512:(tc + 1) * 512], pz[b][0:M, :], scale, [tM, zst_free[zs]])
                        pz_free[b] = te
                        tE.append(te)
                    zst_free[zs] = self.dma(SP, self.zq[li, sq, c0:c0 + M, :], zst[zs][0:M, :], s_z[zs], tE)
                for i in range(NT):
                    sl = i % 2
                    bA = pzi % 4
                    bB = (pzi + 1) % 4
                    pzi += 2
                    PE.wait(t_w, pz_free[bA], pz_free[bB], tokX)
                    lhs = lambda k: xT[:, k, i * 128:(i + 1) * 128]
                    for (bank, o0, n, c0) in ((bA, 0, 512, C_VC), (bB, 0, 64, C_VA), (bB, 68, 256, C_VB)):
                        for k in range(8):
                            ins = nc.tensor.matmul(pz[bank][:, o0:o0 + n], lhsT=lhs(k), rhs=w1[:, k, c0:c0 + n],
                                                   start=(k == 0), stop=(k == 7))
                    tM = PE.mark(ins)
                    E = ACT if i % 2 == 0 else DVE
                    E.wait(tM, vst_free[sl], t_ones[sl])
                    if E is ACT:
                        cp = lambda o, i_: ACT.mark(nc.scalar.copy(out=o, in_=i_))
                        wsc = lambda o, i_: ACT.mark(nc.scalar.activation(out=o, in_=i_, func=AF.Copy, scale=0.5))
                    else:
                        cp = lambda o, i_: DVE.mark(nc.vector.tensor_copy(out=o, in_=i_))
                        wsc = lambda o, i_: DVE.mark(nc.vector.tensor_scalar(o, i_, 0.5, None, ALU.mult))
                    cp(vst[sl][:, 5:13, 0:64], pz[bA][:, :].rearrange("p (h d) -> p h d", h=8))
                    cp(vst[sl][:, 0, 0:64], pz[bB][:, 0:64])
                    te = cp(vst[sl][:, 1:5, 0:64], pz[bB][:, 68:324].rearrange("p (h d) -> p h d", h=4))
                    pz_free[bA] = te
                    pz_free[bB] = te
                    vst_free[sl] = self.dma(SP, self.zv[li, sq, i * 128:(i + 1) * 128, :],
                                            vst[sl][:].rearrange("p h d -> p (h d)"), s_v[sl], [te])
                xT_free = PE.last

    def phase2(self, li):
        nc = self.nc
        PE, ACT, DVE, POOL, SP = self.PE, self.ACT, self.DVE, self.POOL, self.SP
        lam_init = self.lam_inits[li]
        V = nc.vector
        with ExitStack() as c:
            sb = lambda n, s, d: c.enter_context(nc.sbuf_tensor("L%d_%s" % (li, n), list(s), d))
            ps = lambda n, s, d: c.enter_context(nc.psum_tensor("L%d_%s" % (li, n), list(s), d))
            aq = [sb("p2_aq%d" % i, [68, S], BF16) for i in range(4)]
            ak = sb("p2_ak", [68, S], BF16)
            aqi = [sb("p2_aqi%d" % i, [68, S], F32) for i in range(4)]
            aki = sb("p2_aki", [68, S], F32)
            av = sb("p2_av", [128, NT, 65], BF16)
            awi = sb("p2_awi", [128, NT, 4], F32)
            bc = [[sb("p2_bc%d_%d" % (s_, i), [68, S], BF16) for i in range(4)] for s_ in range(2)]
            bcv = [sb("p2_bcv%d" % s_, [128, NT, 65], BF16) for s_ in range(2)]
            acc4 = sb("p2_acc4", [128, 4, S], F32)
            mneg = [sb("p2_mneg0", [128, 4, S], BF16)] * 2
            junk = sb("p2_junk", [128, S], BF16)
            ones_t = sb("p2_ones", [128, S], BF16)
            zr = sb("p2_zr", [128, S], F32)
            identN = sb("p2_identN", [128, 128], BF16)
            R = [sb("p2_R%d" % i, [128, 512], F32) for i in range(2)]
            Pt = [sb("p2_P%d" % i, [128, 512], BF16) for i in range(3)]
            oTs = [sb("p2_oTs%d" % i, [65, 512], F32) for i in range(2)]
            ostg = [sb("p2_ostg%d" % i, [128, 4, 64], BF16) for i in range(2)]
            sm = sb("p2_sm", [128, 96], F32)
            stp = sb("p2_stp", [128, NIT, 4], F32)
            lamt = sb("p2_lam", [128, 128], F32)
            lamp = sb("p2_lamp", [128, 64], F32)
            gsc = sb("p2_gsc", [128, 64], F32)
            t1 = sb("p2_t1", [128, 4, 64], F32)
            osb = sb("p2_osb", [128, 4, 64], F32)
            sqj = sb("p2_sqj", [128, 64], F32)
            epst = sb("p2_eps", [128, 1], F32)
            pS = [ps("p2_pS%d" % i, [128, 512], F32) for i in range(3)]
            pO = [ps("p2_pO%d" % i, [128, 512], F32) for i in range(2)]
            pT = [ps("p2_pT%d" % i, [128, 512], F32) for i in range(3)]
            s_c = self.dsem("p2c%d" % li)
            s_a = self.dsem("p2a%d" % li)
            s_bc = [self.dsem("p2bc%d_%d" % (li, i)) for i in range(2)]
            s_o = [self.dsem("p2o%d_%d" % (li, i)) for i in range(2)]

            t_eps = POOL.mark(nc.gpsimd.memset(epst[:], LN_EPS))
            ACT.wait(t_eps)
            t_on = POOL.mark(nc.gpsimd.memset(ones_t[:], 1.0))
            DVE.wait(t_on)
            t_idn = DVE.mark(V.tensor_scalar(identN[:], self.ident[:], NEG, None, ALU.mult))
            PE.wait(t_idn)
            for t_ in aqi + [aki]:
                POOL.mark(nc.gpsimd.memset(t_[64:68, :], 0.0))
            for s_ in range(2):
                for t_ in bc[s_]:
                    t_zero = POOL.mark(nc.gpsimd.memset(t_[:, :], 0.0))
            SP.wait(t_zero)
            t_l = self.dma(SP, lamt[:], self.lam_r[li], s_c)
            t_g = self.dma(SP, gsc[:], self.sg_r[li], s_c)
            DVE.wait(t_l, t_g)
            a = DVE.mark(V.tensor_tensor(out=lamp[:, 0:32], in0=lamt[:, 0:32], in1=lamt[:, 32:64], op=ALU.mult))
            a = DVE.mark(V.tensor_tensor(out=lamp[:, 32:64], in0=lamt[:, 64:96], in1=lamt[:, 96:128], op=ALU.mult))
            DVE.wait(a)
            a = DVE.mark(V.tensor_reduce(out=sm[:, 0:2], in_=lamp[:].rearrange("p (a d) -> p a d", a=2),
                                         axis=AX.X, op=ALU.add))
            ACT.wait(a)
            a = ACT.mark(nc.scalar.activation(out=sm[:, 2:4], in_=sm[:, 0:2], func=AF.Exp))
            DVE.wait(a)
            a = DVE.mark(V.tensor_tensor(out=sm[:, 4:5], in0=sm[:, 3:4], in1=sm[:, 2:3], op=ALU.subtract))
            DVE.wait(a)
            t_nlam = DVE.mark(V.tensor_scalar(sm[:, 5:6], sm[:, 4:5], float(lam_init), None, ALU.subtract))
            t_gsc = DVE.mark(V.tensor_scalar(gsc[:], gsc[:], float(1.0 - lam_init), None, ALU.mult))
            nlam = sm[:, 5:6]

            st = dict(si=0, pi=0, oi=0, ti=0, ei=0, gi=0, ri=0)
            pS_free = [None] * 3
            P_free = [None] * 3
            pO_free = [None] * 2
            pT_free = [None] * 3
            oTs_free = [None] * 2
            ostg_free = [None] * 2
            R_free = [None, None]
            mneg_free = [None]
            a_free = None
            bc_free = [None, None]
            pend = []

            def flush_pv(keep):
                while len(pend) > keep:
                    pend.pop(0)()

            def dv(inst):
                t = DVE.mark(inst)
                DVE.wait(t)
                return t

            def emit_unit(G, qT, kT, Kr, vt, mkind, marg, final_fn):
                bt0 = 4 * G
                nb = bt0 + 4
                ob = st["oi"] % 2
                st["oi"] += 1
                for bs in range(nb):
                    j0 = max(0, bs - bt0)
                    c0 = j0 * 128
                    sbk = st["si"] % 3
                    st["si"] += 1
                    PE.wait(pS_free[sbk])
                    ins = nc.tensor.matmul(pS[sbk][:, c0:512], lhsT=kT[0:Kr, bs * 128:(bs + 1) * 128],
                                           rhs=qT[0:Kr, bt0 * 128 + c0:bt0 * 128 + 512], start=True,
                                           stop=(mkind == "B" and bs < bt0))
                    if mkind == "C":
                        ins = nc.tensor.matmul(pS[sbk][:, c0:512], lhsT=self.ident[:],
                                               rhs=self.lc[:, bt0 + j0 - bs:bt0 + 4 - bs, :].rearrange("p d t -> p (d t)"),
                                               start=False, stop=True)
                    elif mkind == "B":
                        if bs >= bt0:
                            j = bs - bt0
                            ins = nc.tensor.matmul(pS[sbk][:, j * 128:(j + 1) * 128], lhsT=self.ident[:],
                                                   rhs=self.lcaus[:], start=False, stop=True)
                    else:
                        for j in range(j0, 4):
                            ins = nc.tensor.matmul(pS[sbk][:, j * 128:(j + 1) * 128],
                                                   lhsT=marg[:, j, bs * 128:(bs + 1) * 128], rhs=identN[:],
                                                   start=False, stop=True)
                    tS = PE.mark(ins)
                    pi = st["pi"] % 3
                    st["pi"] += 1
                    ACT.wait(tS, P_free[pi])
                    tP = ACT.mark(nc.scalar.activation(out=Pt[pi][:, c0:512], in_=pS[sbk][:, c0:512], func=AF.Exp))
                    pS_free[sbk] = tP

                    def pv(bs=bs, c0=c0, pi=pi, tP=tP):
                        PE.wait(tP)
                        if bs == 0:
                            PE.wait(pO_free[ob])
                        tV = PE.mark(nc.tensor.matmul(pO[ob][0:65, c0:512], lhsT=vt[:, bs, :], rhs=Pt[pi][:, c0:512],
                                                      start=(bs == 0), stop=(bs == nb - 1)))
                        P_free[pi] = tV
                        if bs == nb - 1:
                            es = st["ei"] % 2
                            st["ei"] += 1
                            ACT.wait(tV, oTs_free[es])
                            tE = ACT.mark(nc.scalar.copy(out=oTs[es][:, :], in_=pO[ob][0:65, :]))
                            pO_free[ob] = tE

                            def tr():
                                tb = st["ti"] % 3
                                st["ti"] += 1
                                PE.wait(tE, pT_free[tb])
                                for j in range(4):
                                    ins2 = nc.tensor.transpose(pT[tb][:, j * 65:(j + 1) * 65],
                                                               oTs[es][0:65, j * 128:(j + 1) * 128],
                                                               self.identf[0:65, 0:65])
                                tT = PE.mark(ins2)
                                oTs_free[es] = tT
                                final_fn(tb, tT)
                            pend.append(tr)
                    pend.append(pv)
                    flush_pv(2)

            def store_out(G, col, osl, waits):
                dst = self.om[li, cur["sq"], G * 512:(G + 1) * 512, col:col + 64].rearrange("(j p) c -> p j c", p=128)
                with nc.allow_non_contiguous_dma(reason="64-col head slice"):
                    ostg_free[osl] = self.dma(SP, dst, ostg[osl][:], s_o[osl], waits)

            def norm_final(G, col):
                def f(tb, tT):
                    osl = st["gi"] % 2
                    st["gi"] += 1
                    rec = sm[:, 32 + 4 * tb:36 + 4 * tb]
                    DVE.wait(tT)
                    r = DVE.mark(V.reciprocal(out=rec, in_=pT[tb][:, 0:260].rearrange("p (j d) -> p j d", d=65)[:, :, 64]))
                    ACT.wait(r, ostg_free[osl])
                    for j in range(4):
                        tA = ACT.mark(nc.scalar.activation(out=ostg[osl][:, j, :], in_=pT[tb][:, j * 65:j * 65 + 64],
                                                           func=AF.Copy, scale=rec[:, j:j + 1]))
                    pT_free[tb] = tA
                    store_out(G, col, osl, [tA])
                return f

            bstate = {}

            def b_final(G, h, m):
                def f(tb, tT):
                    bstate[m] = (tb, tT)
                    if m == 0:
                        return
                    (b1, tv1), (b2, tv2) = bstate[0], bstate[1]
                    osl = st["gi"] % 2
                    st["gi"] += 1
                    rec1, rec2, nl2, ss, lnv, rstd = (sm[:, 48:52], sm[:, 52:56], sm[:, 56:60], sm[:, 60:64],
                                                      sm[:, 64:68], sm[:, 68:72])
                    v1 = pT[b1][:, 0:260].rearrange("p (j d) -> p j d", d=65)
                    v2 = pT[b2][:, 0:260].rearrange("p (j d) -> p j d", d=65)
                    DVE.wait(tv1, tv2, t_nlam, t_gsc)
                    DVE.mark(V.reciprocal(out=rec1, in_=v1[:, :, 64]))
                    dv(V.reciprocal(out=rec2, in_=v2[:, :, 64]))
                    dv(V.tensor_scalar(nl2, rec2, nlam, None, ALU.mult))
                    for j in range(4):
                        a_ = DVE.mark(V.tensor_scalar(t1[:, j, :], v1[:, j, 0:64], rec1[:, j:j + 1], None, ALU.mult))
                    DVE.wait(a_)
                    pT_free[b1] = a_
                    for j in range(4):
                        a_ = DVE.mark(V.scalar_tensor_tensor(out=osb[:, j, :], in0=v2[:, j, 0:64], scalar=nl2[:, j:j + 1],
                                                             in1=t1[:, j, :], op0=ALU.mult, op1=ALU.add))
                    DVE.wait(a_)
                    pT_free[b2] = a_
                    for j in range(4):
                        a_ = DVE.mark(V.scalar_tensor_tensor(out=sqj[:], in0=osb[:, j, :], scalar=1.0, in1=osb[:, j, :],
                                                             op0=ALU.mult, op1=ALU.mult, accum_out=ss[:, j:j + 1]))
                    ACT.wait(a_)
                    a6 = ACT.mark(nc.scalar.activation(out=lnv, in_=ss, func=AF.Ln, scale=1.0 / 64.0, bias=epst[:, 0:1]))
                    ACT.wait(a6)
                    a7 = ACT.mark(nc.scalar.activation(out=rstd, in_=lnv, func=AF.Exp, scale=-0.5))
                    DVE.wait(a7, ostg_free[osl])
                    for j in range(4):
                        a_ = DVE.mark(V.scalar_tensor_tensor(out=ostg[osl][:, j, :], in0=osb[:, j, :],
                                                             scalar=rstd[:, j:j + 1], in1=gsc[:], op0=ALU.mult,
                                                             op1=ALU.mult))
                    DVE.wait(a_)
                    store_out(G, 256 + 64 * h, osl, [a_])
                return f

            cur = dict(sq=0)

            def idx_and_bisect(G):
                sl = G % 2
                M_ = mneg[sl]
                last = None
                for j in range(4):
                    bt = 4 * G + j
                    nk = (bt + 1) * 128
                    for cidx in range((nk + 511) // 512):
                        n = min(512, nk - cidx * 512)
                        for h in range(4):
                            sbk = st["si"] % 3
                            st["si"] += 1
                            PE.wait(pS_free[sbk])
                            tS = PE.mark(nc.tensor.matmul(pS[sbk][:, 0:n], lhsT=aqi[h][0:68, bt * 128:(bt + 1) * 128],
                                                          rhs=aki[0:68, cidx * 512:cidx * 512 + n], start=True,
                                                          stop=True))
                            dst = acc4[:, j, cidx * 512:cidx * 512 + n]
                            if h == 0:
                                DVE.wait(tS)
                                last = DVE.mark(V.tensor_scalar(dst, pS[sbk][:, 0:n], 0.0, awi[:, bt, 0:1], ALU.max,
                                                                ALU.mult))
                                pS_free[sbk] = last
                            else:
                                rs = st["ri"] % 2
                                st["ri"] += 1
                                ACT.wait(tS, R_free[rs])
                                tR = ACT.mark(nc.scalar.activation(out=R[rs][:, 0:n], in_=pS[sbk][:, 0:n], func=AF.Relu))
                                pS_free[sbk] = tR
                                DVE.wait(tR, last)
                                last = DVE.mark(V.scalar_tensor_tensor(out=dst, in0=R[rs][:, 0:n],
                                                                       scalar=awi[:, bt, h:h + 1], in1=dst,
                                                                       op0=ALU.mult, op1=ALU.add))
                                R_free[rs] = last
                yield
                am, Aa, mid, cnt, gg, tt = (sm[:, 8:12], sm[:, 12:16], sm[:, 16:20], sm[:, 20:24], sm[:, 24:28],
                                            sm[:, 28:32])
                need = sm[:, 72:76]
                nks = [(4 * G + j + 1) * 128 for j in range(4)]
                DVE.wait(last)
                for j in range(4):
                    a_ = DVE.mark(V.tensor_reduce(out=am[:, j:j + 1], in_=acc4[:, j, 0:nks[j]], axis=AX.X, op=ALU.max,
                                                  apply_absolute_value=True))
                DVE.wait(a_)
                dv(V.tensor_scalar(Aa, am, 1.0001, 1e-30, ALU.mult, ALU.add))
                for j in range(4):
                    DVE.mark(V.tensor_scalar(stp[:, :, j], self.pow2[:], Aa[:, j:j + 1], None, ALU.mult))
                    DVE.mark(V.tensor_tensor(out=acc4[:, j, nks[j] - 128:nks[j]], in0=acc4[:, j, nks[j] - 128:nks[j]],
                                             in1=self.causf[:], op=ALU.add))
                dv(V.memset(mid, 0.0))
                yield
                for k in range(NIT):
                    for j in range(4):
                        yield
                        a_ = DVE.mark(V.tensor_scalar(junk[:, 0:nks[j]], acc4[:, j, 0:nks[j]], mid[:, j:j + 1], 0.0,
                                                      ALU.is_gt, ALU.add, accum_out=cnt[:, j:j + 1]))
                    yield
                    DVE.wait(a_)
                    if k == 0:
                        dv(V.tensor_scalar(need, cnt, -1.0, 256.0, ALU.mult, ALU.add))
                        dv(V.tensor_scalar(need, need, 0.0, None, ALU.max))
                    dv(V.tensor_scalar(gg, cnt, 255.5, 0.5 if k < NIT - 1 else 1.0, ALU.is_ge, ALU.subtract))
                    dv(V.tensor_tensor(out=tt, in0=gg, in1=stp[:, k, :], op=ALU.mult))
                    dv(V.tensor_tensor(out=mid, in0=mid, in1=tt, op=ALU.add))
                DVE.wait(mneg_free[0])
                for j in range(4):
                    n_ = nks[j]
                    yield
                    dv(V.tensor_scalar(junk[:, 0:n_], acc4[:, j, 0:n_], 0.0, None, ALU.is_equal))
                    yield
                    dv(V.tensor_tensor_scan(out=zr[:, 0:n_], data0=ones_t[:, 0:n_], data1=junk[:, 0:n_], initial=0.0,
                                            op0=ALU.mult, op1=ALU.add))
                    yield
                    dv(V.scalar_tensor_tensor(out=junk[:, 0:n_], in0=zr[:, 0:n_], scalar=need[:, j:j + 1],
                                              in1=junk[:, 0:n_], op0=ALU.is_gt, op1=ALU.mult))
                    yield
                    tM = dv(V.scalar_tensor_tensor(out=M_[:, j, 0:n_], in0=acc4[:, j, 0:n_], scalar=mid[:, j:j + 1],
                                                   in1=junk[:, 0:n_], op0=ALU.is_le, op1=ALU.max))
                tmn[G] = tM

            for sq in range(self.nseq):
                cur["sq"] = sq
                zq = self.zq[li, sq]
                zv = self.zv[li, sq].rearrange("(i p) (h d) -> p i h d", p=128, d=65)
                for h in range(4):
                    self.dma(SP, aq[h][0:64, :], zq[C_QA + 64 * h:C_QA + 64 * (h + 1), :], s_a, [a_free])
                    self.dma(SP, aq[h][64:68, :], self.qaug_b[2 * h + 1], s_a)
                    self.dma(SP, aqi[h][0:64, :], self.zqf[li, sq, 64 * h:64 * (h + 1), :], s_a)
                self.dma(SP, ak[0:64, :], zq[C_KA:C_KA + 64, :], s_a)
                self.dma(SP, ak[64:68, :], self.kaug_b, s_a)
                self.dma(SP, aki[0:64, :], self.zqf[li, sq, 256:320, :], s_a)
                self.dma(SP, av[:], zv[:, :, 0, :], s_a)
                t_la = self.dma(SP, awi[:], self.zw[li, sq].rearrange("(i p) h -> p i h", p=128), s_a)

                jobs = [("B", 0), ("B", 1), ("C", 0), ("C", 1), ("B", 2), ("C", 2), ("C", 3), ("C", 4),
                        ("B", 3), ("C", 5), ("C", 6), ("C", 7)]
                grp_units = {0: 8, 1: 16, 2: 20, 3: 20}
                job_tok = {}

                def load_job(ji):
                    kind, h = jobs[ji]
                    sl = ji % 2
                    T = bc[sl]
                    w = [bc_free[sl]]
                    if kind == "B":
                        for m in range(2):
                            c0 = C_QB + h * 64 + m * 32
                            self.dma(SP, T[2 * m][0:32, :], zq[c0:c0 + 32, :], s_bc[sl], w)
                            self.dma(SP, T[2 * m][32:36, :], self.qaug_b[2 * h + 1], s_bc[sl])
                            c0 = C_KB + h * 64 + m * 32
                            self.dma(SP, T[2 * m + 1][0:32, :], zq[c0:c0 + 32, :], s_bc[sl])
                            self.dma(SP, T[2 * m + 1][32:36, :], self.kaug_b, s_bc[sl])
                        job_tok[ji] = self.dma(SP, bcv[sl][:], zv[:, :, 1 + h, :], s_bc[sl])
                    else:
                        self.dma(SP, T[0][0:64, :], zq[C_QC + 64 * h:C_QC + 64 * (h + 1), :], s_bc[sl], w)
                        self.dma(SP, T[0][64:68, :], self.qaug_b[h], s_bc[sl])
                        self.dma(SP, T[2][0:64, :], zq[C_KC + 64 * h:C_KC + 64 * (h + 1), :], s_bc[sl])
                        self.dma(SP, T[2][64:68, :], self.kaug_b, s_bc[sl])
                        job_tok[ji] = self.dma(SP, bcv[sl][:], zv[:, :, 5 + h, :], s_bc[sl])

                load_job(0)
                load_job(1)

                PE.wait(t_la, t_zero)
                DVE.wait(t_la)
                tmn = {}

                def step(gen, n):
                    for _ in range(n):
                        try:
                            next(gen)
                        except StopIteration:
                            return

                gen = iter(())
                curG = None
                nstep = 1

                def finish_group():
                    G = curG
                    step(gen, 1000)
                    flush_pv(0)
                    PE.wait(tmn[G])
                    for hh in range(4):
                        emit_unit(G, aq[hh], ak, 68, av, "A", mneg[0], norm_final(G, 64 * hh))
                    flush_pv(0)
                    mneg_free[0] = PE.last

                for ji, (kind, h) in enumerate(jobs):
                    sl = ji % 2
                    T = bc[sl]
                    if kind == "B":
                        if curG is not None:
                            finish_group()
                        curG = h
                        gen = idx_and_bisect(h)
                        nstep = -(-(NIT * 5 + 20) // grp_units[h])
                        step(gen, 1)
                    PE.wait(job_tok[ji])
                    for G in range(4):
                        if kind == "B":
                            for m in range(2):
                                emit_unit(G, T[2 * m], T[2 * m + 1], 68, bcv[sl], "B", None, b_final(G, h, m))
                                step(gen, nstep)
                        else:
                            emit_unit(G, T[0], T[2], 68, bcv[sl], "C", None, norm_final(G, 512 + 64 * h))
                            step(gen, nstep)
                    flush_pv(0)
                    bc_free[sl] = PE.last
                    if ji + 2 < len(jobs):
                        load_job(ji + 2)
                finish_group()
                a_free = PE.last

    def phase3(self, li, xsrc, ydst):
        nc = self.nc
        PE, ACT, DVE, POOL, SP = self.PE, self.ACT, self.DVE, self.POOL, self.SP
        with ExitStack() as c:
            sb = lambda n, s, d: c.enter_context(nc.sbuf_tensor("L%d_%s" % (li, n), list(s), d))
            ps = lambda n, s, d: c.enter_context(nc.psum_tensor("L%d_%s" % (li, n), list(s), d))
            wo = sb("p3_wo", [128, 8, D], BF16)
            wd = sb("p3_wd", [128, NFF, D], BF16)
            lnc = sb("p3_ln", [128, 4, D], F32)
            wgu = [sb("p3_wgu%d" % i, [128, 2, 1024], BF16) for i in range(3)]
            ot = [sb("p3_ot%d" % i, [128, D], BF16) for i in range(2)]
            xt = [sb("p3_xt%d" % i, [128, D], F32) for i in range(2)]
            oT = sb("p3_oT", [128, 8, 128], BF16)
            x1 = [sb("p3_x1_%d" % i, [128, D], F32) for i in range(4)]
            x1T = sb("p3_x1T", [128, 8, 512], BF16)
            hT = sb("p3_hT", [128, NFF, 512], BF16)
            sg = [sb("p3_sg%d" % i, [128, 512], F32) for i in range(2)]
            yt = [sb("p3_yt%d" % i, [128, D], F32) for i in range(2)]
            stt = sb("p3_stt", [128, 2, 6], F32)
            sm = sb("p3_sm", [128, 16], F32)
            eps = sb("p3_eps", [128, 1], F32)
            pOT = ps("p3_pOT", [128, 1024], BF16)
            pXT = ps("p3_pXT", [128, 1024], F32)
            pM = ps("p3_pM", [128, 1024], F32)
            pG = [ps("p3_pG%d" % i, [128, 512], F32) for i in range(3)]
            s_w = self.dsem("p3w%d" % li)
            s_o = [self.dsem("p3o%d_%d" % (li, i)) for i in range(2)]
            s_x = [self.dsem("p3x%d_%d" % (li, i)) for i in range(2)]
            s_g = [self.dsem("p3g%d_%d" % (li, i)) for i in range(3)]
            s_y = [self.dsem("p3y%d_%d" % (li, i)) for i in range(2)]
            t_w = [self.dma(SP, wo[:], self.wob[li].rearrange("(k p) n -> p k n", p=128), s_w),
                   self.dma(SP, wd[:], self.wdb[li].rearrange("(k p) n -> p k n", p=128), s_w),
                   self.dma(SP, lnc[:], self.ln_r[li].rearrange("a p n -> p a n"), s_w)]
            t_eps = POOL.mark(nc.gpsimd.memset(eps[:], LN_EPS))
            ACT.wait(t_eps)
            V = nc.vector

            def dv(inst):
                t = DVE.mark(inst)
                DVE.wait(t)
                return t

            def layer_norm(src, dst, gi):
                dv(V.bn_stats(out=stt[:, 0, :], in_=src[:, 0:512]))
                dv(V.bn_stats(out=stt[:, 1, :], in_=src[:, 512:1024]))
                a = dv(V.bn_aggr(out=sm[:, 0:2], in_=stt[:].rearrange("p a s -> p (a s)")))
                ACT.wait(a)
                a = ACT.mark(nc.scalar.activation(out=sm[:, 2:3], in_=sm[:, 1:2], func=AF.Sqrt, bias=eps[:, 0:1],
                                                  scale=1.0))
                DVE.wait(a)
                dv(V.reciprocal(out=sm[:, 3:4], in_=sm[:, 2:3]))
                dv(V.tensor_scalar(dst, src, sm[:, 0:1], sm[:, 3:4], ALU.subtract, ALU.mult))
                dv(V.tensor_tensor(out=dst, in0=dst, in1=lnc[:, gi, :], op=ALU.mult))
                return dv(V.tensor_tensor(out=dst, in0=dst, in1=lnc[:, gi + 1, :], op=ALU.add))

            ot_free = [None, None]
            xt_free = [None, None]
            wgu_free = [None] * 3
            pG_free = [None] * 3
            sg_free = [None, None]
            yt_free = [None, None]
            oT_free = None
            pOT_free = None
            pXT_free = None
            pM_free = None
            x1T_free = None
            hT_free = None
            r_free = None
            gi_ = 0
            ci_ = 0
            for sq in range(self.nseq):
                for grp in range(4):
                    x1_tok = []
                    ln_tok = {}

                    def front(tl):
                        nonlocal pOT_free, oT_free, pM_free
                        i = grp * 4 + tl
                        sl = i % 2
                        t_o = self.dma(SP, ot[sl][:], self.om[li, sq, i * 128:(i + 1) * 128, :], s_o[sl], [ot_free[sl]])
                        t_x = self.dma(SP, xt[sl][:], xsrc[sq, i * 128:(i + 1) * 128, :], s_x[sl], [xt_free[sl]])
                        PE.wait(t_o, pOT_free)
                        for k in range(8):
                            ins = nc.tensor.transpose(pOT[:, k * 128:(k + 1) * 128], ot[sl][:, k * 128:(k + 1) * 128],
                                                      self.ident[:])
                        tT = PE.mark(ins)
                        ot_free[sl] = tT
                        ACT.wait(tT, oT_free)
                        tC = ACT.mark(nc.scalar.copy(out=oT[:].rearrange("p k t -> p (k t)"), in_=pOT[:]))
                        pOT_free = tC
                        PE.wait(tC, pM_free, t_w)
                        for half in range(2):
                            for k in range(8):
                                ins = nc.tensor.matmul(pM[:, half * 512:(half + 1) * 512], lhsT=oT[:, k, :],
                                                       rhs=wo[:, k, half * 512:(half + 1) * 512], start=(k == 0),
                                                       stop=(k == 7))
                        tM = PE.mark(ins)
                        oT_free = tM
                        DVE.wait(tM, t_x)
                        a = dv(V.scalar_tensor_tensor(out=x1[tl][:], in0=xt[sl][:], scalar=float(ALPHA), in1=pM[:],
                                                      op0=ALU.mult, op1=ALU.add))
                        pM_free = a
                        xt_free[sl] = a
                        ln_tok[tl] = layer_norm(x1[tl][:], x1[tl][:], 0)

                    def back(tl):
                        nonlocal pXT_free
                        PE.wait(ln_tok[tl], pXT_free)
                        for k in range(8):
                            ins = nc.tensor.transpose(pXT[:, k * 128:(k + 1) * 128], x1[tl][:, k * 128:(k + 1) * 128],
                                                      self.identf[:])
                        tT = PE.mark(ins)
                        ACT.wait(tT, x1T_free)
                        tC = ACT.mark(nc.scalar.copy(out=x1T[:, :, tl * 128:(tl + 1) * 128],
                                                     in_=pXT[:].rearrange("p (k t) -> p k t", k=8)))
                        pXT_free = tC
                        x1_tok.append(tC)

                    front(0)
                    for tl in range(1, 4):
                        front(tl)
                        back(tl - 1)
                    back(3)
                    for cidx in range(NFF):
                        ws = ci_ % 3
                        ci_ += 1
                        t_g = self.dma(SP, wgu[ws][:], self.wgub[li, cidx], s_g[ws], [wgu_free[ws]])
                        bg = gi_ % 3
                        bu = (gi_ + 1) % 3
                        gi_ += 2
                        PE.wait(t_g, x1_tok, pG_free[bg], pG_free[bu])
                        for (bank, j) in ((bg, 0), (bu, 1)):
                            for k in range(8):
                                ins = nc.tensor.matmul(pG[bank][:, :], lhsT=wgu[ws][:, j, k * 128:(k + 1) * 128],
                                                       rhs=x1T[:, k, :], start=(k == 0), stop=(k == 7))
                        tM = PE.mark(ins)
                        wgu_free[ws] = tM
                        ss = cidx % 2
                        ACT.wait(tM, sg_free[ss])
                        tS = ACT.mark(nc.scalar.activation(out=sg[ss][:], in_=pG[bg][:, :], func=AF.Silu))
                        pG_free[bg] = tS
                        DVE.wait(tS, hT_free if cidx == 0 else None)
                        tH = DVE.mark(V.tensor_tensor(out=hT[:, cidx, :], in0=sg[ss][:], in1=pG[bu][:, :], op=ALU.mult))
                        pG_free[bu] = tH
                        sg_free[ss] = tH
                    x1T_free = PE.last
                    for tl in range(4):
                        i = grp * 4 + tl
                        ys = i % 2
                        PE.wait(tH, pM_free)
                        for cidx in range(NFF):
                            for half in range(2):
                                ins = nc.tensor.matmul(pM[:, half * 512:(half + 1) * 512],
                                                       lhsT=hT[:, cidx, tl * 128:(tl + 1) * 128],
                                                       rhs=wd[:, cidx, half * 512:(half + 1) * 512], start=(cidx == 0),
                                                       stop=(cidx == NFF - 1))
                        tM = PE.mark(ins)
                        DVE.wait(tM, yt_free[ys])
                        a = dv(V.scalar_tensor_tensor(out=yt[ys][:], in0=x1[tl][:], scalar=float(ALPHA), in1=pM[:],
                                                      op0=ALU.mult, op1=ALU.add))
                        pM_free = a
                        a = layer_norm(yt[ys][:], yt[ys][:], 2)
                        yt_free[ys] = self.dma(SP, ydst[sq, i * 128:(i + 1) * 128, :], yt[ys][:], s_y[ys], [a])
                    hT_free = PE.last


def make_consts():
    t = np.arange(128)
    lc = np.zeros((16, 128, 128), np.float32)
    for dl in range(16):
        dist = 128 * dl + t[None, :] - t[:, None]
        m = (((dist >= 0) & (dist <= 128)).astype(np.int64)
             + ((dist >= 0) & (dist <= 512) & (dist % 4 == 0))
             + ((dist >= 0) & (dist <= 2048) & (dist % 16 == 0)))
        lc[dl] = np.where(m > 0, np.log(np.maximum(m, 1)), NEG)
    pos = np.arange(S)
    rs, bs = (pos % 128).astype(np.float32), (pos // 128).astype(np.float32)
    kaug = np.stack([rs, np.ones(S, np.float32), bs, np.ones(S, np.float32)])
    qaug = np.zeros((8, 4, S), np.float32)
    for j in range(8):
        sl = 2.0 ** -(j + 1)
        qaug[j] = np.stack([np.full(S, sl, np.float32), -sl * rs, np.full(S, 128 * sl, np.float32), -128 * sl * bs])
    ident = np.eye(128, dtype=np.float32)
    causf = np.where(t[None, :] > t[:, None], -1e30, 0.0).astype(np.float32)
    lcaus = np.where(t[:, None] > t[None, :], NEG, 0.0).astype(np.float32)
    pow2 = np.tile((2.0 ** -np.arange(NIT)).astype(np.float32)[None, :], (128, 1))
    return dict(c_lc=lc, c_kaug=kaug, c_qaug=qaug, c_ident=ident, c_causf=causf, c_lcaus=lcaus, c_pow2=pow2)


_CACHE = {}


def get_nc(nl, nseq, lam_inits, debug=False):
    key = (nl, nseq, tuple(lam_inits), debug)
    if key not in _CACHE:
        _CACHE[key] = K(nl, nseq, lam_inits, debug).build()
    return _CACHE[key]


def layer_inputs(layers, w_in, w_o, lam, subln_g, ln1_g, ln1_b, w_gate, w_up, w_down, ln2_g, ln2_b):
    f = lambda a: np.ascontiguousarray(np.asarray(a, dtype=np.float32))
    L = list(layers)
    rep = lambda v: np.ascontiguousarray(np.broadcast_to(np.asarray(v, np.float32)[None, :], (128, v.shape[-1])))
    d = dict(w_in=f(w_in[L]), w_o=f(w_o[L]), w_gate=f(w_gate[L]), w_up=f(w_up[L]), w_down=f(w_down[L]))
    d["lam_r"] = np.stack([rep(np.asarray(lam[l]).reshape(-1)) for l in L])
    d["sg_r"] = np.stack([rep(np.asarray(subln_g[l])) for l in L])
    d["ln_r"] = np.stack([np.stack([rep(np.asarray(v[l])) for v in (ln1_g, ln1_b, ln2_g, ln2_b)]) for l in L])
    d.update(make_consts())
    return d


def lam_init_of(l):
    return 0.8 - 0.6 * math.exp(-0.3 * l)


def kernel(x, w_in, w_o, lam, subln_g, ln1_g, ln1_b, w_gate, w_up, w_down, ln2_g, ln2_b):
    x = np.ascontiguousarray(np.asarray(x, dtype=np.float32))
    args = (w_in, w_o, lam, subln_g, ln1_g, ln1_b, w_gate, w_up, w_down, ln2_g, ln2_b)
    args = tuple(np.asarray(a) for a in args)
    nl = 2
    nc = get_nc(nl, SEQ_PER_CORE, [lam_init_of(l) for l in range(nl)])
    shared = layer_inputs(range(nl), *args)
    in_maps = []
    for c in range(N_CORES):
        m = dict(shared)
        m["x"] = x[c * SEQ_PER_CORE:(c + 1) * SEQ_PER_CORE]
        in_maps.append(m)
    res = run_bass_kernel_spmd(nc, in_maps, core_ids=list(range(N_CORES)))
    return np.concatenate([np.asarray(r["y"]) for r in res.results], axis=0).astype(np.float32)
```

```python
import math
from contextlib import ExitStack

import numpy as np
import concourse.bass as bass
import concourse.mybir as mybir
from concourse.bass_utils import run_bass_kernel_spmd

F32 = mybir.dt.float32
BF16 = mybir.dt.bfloat16
AF = mybir.ActivationFunctionType
ALU = mybir.AluOpType
AX = mybir.AxisListType

S = 2048
D = 1024
NT = 16
FF = 2816
NFF = 22
INC = 3012
ALPHA = 4.0 ** 0.25
LN_EPS = 1e-5
NIT = 14
NEG = -30000.0
N_CORES = 8
SEQ_PER_CORE = 4

C_QA, C_KA, C_VA, C_QI, C_KI, C_WI, C_QB, C_KB, C_VB, C_QC, C_KC, C_VC = (
    0, 256, 320, 384, 640, 704, 708, 964, 1220, 1476, 1988, 2500)


class Sem:
    def __init__(self, h):
        self.h = h
        self.n = 0


class Eng:
    def __init__(self, k, eng, name):
        self.e = eng
        self.sem = Sem(k.new_sem("e_" + name))
        self.seen = {}
        self.last = None

    def wait(self, *toks):
        for t in toks:
            if t is None:
                continue
            if isinstance(t, list):
                self.wait(*t)
                continue
            s, v = t
            if self.seen.get(s, 0) >= v:
                continue
            self.e.wait_ge(s.h, v)
            self.seen[s] = v

    def mark(self, inst):
        self.sem.n += 1
        inst.then_inc(self.sem.h, 1)
        self.last = (self.sem, self.sem.n)
        return self.last


class K:
    def __init__(self, nl, nseq, lam_inits, debug=False):
        self.nl, self.nseq, self.lam_inits, self.debug = nl, nseq, lam_inits, debug
        self.nc = bass.Bass("TRN2", target_bir_lowering=False)
        self.ctx = ExitStack()
        self.dsems = []
        self.sem_pool = []
        self.in_use = []

    def new_sem(self, name):
        return self.ctx.enter_context(self.nc.semaphore(name))

    def dsem(self, name):
        if self.sem_pool:
            s = self.sem_pool.pop()
        else:
            s = Sem(self.new_sem("d%d" % len(self.dsems)))
            self.dsems.append(s)
        self.in_use.append(s)
        return s

    def dram(self, name, shape, dt, kind="Internal"):
        return self.nc.dram_tensor(name, list(shape), dt, kind=kind).ap()

    def dma(self, q, out, in_, sem, waits=(), **kw):
        q.wait(*waits)
        inst = q.e.dma_start(out=out, in_=in_, **kw)
        sem.n += 16
        inst.then_inc(sem.h, 16)
        return (sem, sem.n)

    def barrier(self):
        sp = self.SP
        for e in self.engs:
            if e is not sp:
                sp.wait(e.last)
        for s in self.dsems:
            if s.n:
                sp.wait((s, s.n))
        tok = self.dma(sp, self.bar_b[:], self.bar_a[:], self.bar_sem)
        for e in self.engs:
            e.wait(tok)
        self.sem_pool.extend(self.in_use)
        self.in_use = []

    def build(self):
        nc, nl, nseq = self.nc, self.nl, self.nseq
        ctx = self.ctx
        dbg = "ExternalOutput" if self.debug else "Internal"
        ein = lambda n, s: self.dram(n, s, F32, "ExternalInput")
        self.x_in = ein("x", [nseq, S, D])
        self.w_in = ein("w_in", [nl, D, INC])
        self.w_o = ein("w_o", [nl, D, D])
        self.w_g = ein("w_gate", [nl, D, FF])
        self.w_u = ein("w_up", [nl, D, FF])
        self.w_d = ein("w_down", [nl, FF, D])
        self.lam_r = ein("lam_r", [nl, 128, 128])
        self.sg_r = ein("sg_r", [nl, 128, 64])
        self.ln_r = ein("ln_r", [nl, 4, 128, D])
        self.c_lc = ein("c_lc", [16, 128, 128])
        self.c_kaug = ein("c_kaug", [4, S])
        self.c_qaug = ein("c_qaug", [8, 4, S])
        self.c_ident = ein("c_ident", [128, 128])
        self.c_causf = ein("c_causf", [128, 128])
        self.c_lcaus = ein("c_lcaus", [128, 128])
        self.c_pow2 = ein("c_pow2", [128, NIT])
        self.y_out = self.dram("y", [nseq, S, D], F32, "ExternalOutput")
        self.w1b = self.dram("w1b", [nl, D, INC], BF16)
        self.wob = self.dram("wob", [nl, D, D], BF16)
        self.wgub = self.dram("wgub", [nl, NFF, 128, 2, 1024], BF16)
        self.wdb = self.dram("wdb", [nl, FF, D], BF16)
        self.kaug_b = self.dram("kaug_b", [4, S], BF16)
        self.qaug_b = self.dram("qaug_b", [8, 4, S], BF16)
        self.zq = self.dram("zq", [nl, nseq, INC, S], BF16, dbg)
        self.zv = self.dram("zv", [nl, nseq, S, 13 * 65], BF16, dbg)
        self.zw = self.dram("zw", [nl, nseq, S, 4], F32, dbg)
        self.zqf = self.dram("zqf", [nl, nseq, 320, S], F32, dbg)
        self.om = self.dram("om", [nl, nseq, S, D], BF16, dbg)
        self.xmid = [self.dram("xmid%d" % i, [nseq, S, D], F32, dbg) for i in range(nl - 1)]

        with ctx:
            self.PE = Eng(self, nc.tensor, "pe")
            self.ACT = Eng(self, nc.scalar, "act")
            self.DVE = Eng(self, nc.vector, "dve")
            self.POOL = Eng(self, nc.gpsimd, "pool")
            self.SP = Eng(self, nc.sync, "sp")
            self.engs = [self.PE, self.ACT, self.DVE, self.POOL, self.SP]
            self.bar_sem = Sem(self.new_sem("bar"))
            sb = lambda n, s, d: ctx.enter_context(nc.sbuf_tensor(n, list(s), d))
            self.bar_a = sb("bar_a", [1, 8], F32)
            self.bar_b = sb("bar_b", [1, 8], F32)
            self.ident = sb("ident", [128, 128], BF16)
            self.identf = sb("identf", [128, 128], F32)
            self.lcaus = sb("lcaus", [128, 128], BF16)
            self.causf = sb("causf", [128, 128], F32)
            self.pow2 = sb("pow2", [128, NIT], F32)
            self.lc = sb("lc", [128, 16, 128], BF16)
            self.phase0()
            self.barrier()
            for li in range(nl):
                xsrc = self.x_in if li == 0 else self.xmid[li - 1]
                ydst = self.y_out if li == nl - 1 else self.xmid[li]
                self.phase1(li, xsrc)
                self.barrier()
                self.phase2(li)
                self.barrier()
                self.phase3(li, xsrc, ydst)
                self.barrier()
        return nc

    def phase0(self):
        nc, nl = self.nc, self.nl
        P, SP = self.POOL, self.SP
        sw = self.dsem("w0")
        ncd = [0]

        def cd(o, i):
            ncd[0] += 1
            t = self.dma(P, o, i, sw, max_dma_last_dim=4096)
            if ncd[0] % 6 == 0:
                P.wait(t)
            return t
        POOLm = lambda inst: P.mark(inst)
        POOLm(nc.gpsimd.memset(self.bar_a[:], 0.0))
        cd(self.ident[:], self.c_ident)
        cd(self.lcaus[:], self.c_lcaus)
        cd(self.lc[:], self.c_lc.rearrange("d s t -> s d t"))
        cd(self.kaug_b, self.c_kaug)
        cd(self.qaug_b, self.c_qaug)
        self.dma(SP, self.identf[:], self.c_ident, sw)
        self.dma(SP, self.causf[:], self.c_causf, sw)
        self.dma(SP, self.pow2[:], self.c_pow2, sw)
        for l in range(nl):
            for kc in range(8):
                cd(self.w1b[l, kc * 128:(kc + 1) * 128, :], self.w_in[l, kc * 128:(kc + 1) * 128, :])
            for h in range(2):
                cd(self.wob[l, h * 512:(h + 1) * 512, :], self.w_o[l, h * 512:(h + 1) * 512, :])
            for c in range(NFF):
                for j, w in enumerate((self.w_g, self.w_u)):
                    cd(self.wgub[l, c, :, j, :].rearrange("p (k f) -> p k f", k=8),
                       w[l, :, c * 128:(c + 1) * 128].rearrange("(k p) f -> p k f", p=128))
            for h in range(NFF):
                cd(self.wdb[l, h * 128:(h + 1) * 128, :], self.w_d[l, h * 128:(h + 1) * 128, :])

    def phase1(self, li, xsrc):
        nc = self.nc
        PE, ACT, DVE, POOL, SP = self.PE, self.ACT, self.DVE, self.POOL, self.SP
        with ExitStack() as c:
            sb = lambda n, s, d: c.enter_context(nc.sbuf_tensor("L%d_%s" % (li, n), list(s), d))
            ps = lambda n, s, d: c.enter_context(nc.psum_tensor("L%d_%s" % (li, n), list(s), d))
            w1 = sb("p1_w1", [128, 8, INC], BF16)
            xT = sb("p1_xT", [128, 8, S], BF16)
            xt = [sb("p1_xt%d" % i, [128, D], F32) for i in range(2)]
            zst = [sb("p1_zst%d" % i, [128, S], BF16) for i in range(2)]
            vst = [sb("p1_vst%d" % i, [128, 13, 65], BF16) for i in range(2)]
            wst4 = [sb("p1_wst4_%d" % i, [128, 4, 4], F32) for i in range(2)]
            xTf = [sb("p1_xTf%d" % i, [128, 8, 512], F32) for i in range(2)]
            w1f = sb("p1_w1f", [128, 8, 388], F32)
            zstf = [sb("p1_zstf%d" % i, [128, 512], F32) for i in range(3)]
            pX = [ps("p1_pX%d" % i, [128, 1024], F32) for i in range(2)]
            pz = [ps("p1_pz%d" % i, [128, 512], F32) for i in range(4)]
            s_w = self.dsem("p1w%d" % li)
            s_x = [self.dsem("p1x%d_%d" % (li, i)) for i in range(2)]
            s_z = [self.dsem("p1z%d_%d" % (li, i)) for i in range(2)]
            s_v = [self.dsem("p1v%d_%d" % (li, i)) for i in range(2)]
            s_f = [self.dsem("p1f%d_%d" % (li, i)) for i in range(3)]
            s_w4 = [self.dsem("p1w4%d_%d" % (li, i)) for i in range(2)]
            t_w = self.dma(SP, w1[:], self.w1b[li].rearrange("(k p) n -> p k n", p=128), s_w)
            t_pad = POOL.mark(nc.gpsimd.memset(w1f[:, :, 320:384], 0.0))
            wsrc = self.w_in[li].rearrange("(k p) n -> p k n", p=128)
            self.dma(SP, w1f[:, :, 0:320], wsrc[:, :, C_QI:C_QI + 320], s_w)
            t_wf = self.dma(SP, w1f[:, :, 384:388], wsrc[:, :, C_WI:C_WI + 4], s_w)
            xTf_free = [None, None]
            zstf_free = [None] * 3
            wst4_free = [None, None]
            fi = 0
            t_ones = [POOL.mark(nc.gpsimd.memset(vst[i][:, :, 64:65], 1.0)) for i in range(2)]
            xt_free = [None, None]
            pX_free = [None, None]
            pz_free = [None] * 4
            zst_free = [None, None]
            vst_free = [None, None]
            xT_free = None
            pzi = 0
            evi = 0

            def ev_copy(out, in_, scale, waits):
                nonlocal evi
                evi += 1
                if evi % 2 == 0:
                    ACT.wait(*waits)
                    if scale == 1.0:
                        return ACT.mark(nc.scalar.copy(out=out, in_=in_))
                    return ACT.mark(nc.scalar.activation(out=out, in_=in_, func=AF.Copy, scale=float(scale)))
                DVE.wait(*waits)
                if scale == 1.0:
                    return DVE.mark(nc.vector.tensor_copy(out=out, in_=in_))
                return DVE.mark(nc.vector.tensor_scalar(out, in_, float(scale), None, ALU.mult))

            fm_groups = ([(C_QA + 128 * i, 128, 0.125) for i in range(2)] + [(C_KA, 64, 1.0)]
                         + [(C_QB + 128 * i, 128, 32.0 ** -0.5) for i in range(2)]
                         + [(C_KB + 128 * i, 128, 1.0) for i in range(2)]
                         + [(C_QC + 128 * i, 128, 0.125) for i in range(4)]
                         + [(C_KC + 128 * i, 128, 1.0) for i in range(4)])
            for sq in range(self.nseq):
                tokX = []
                for i in range(NT):
                    sl = i % 2
                    t_ld = self.dma(SP, xt[sl][:], xsrc[sq, i * 128:(i + 1) * 128, :], s_x[sl], [xt_free[sl]])
                    PE.wait(t_ld, pX_free[sl])
                    for k in range(8):
                        ins = nc.tensor.transpose(pX[sl][:, k * 128:(k + 1) * 128], xt[sl][:, k * 128:(k + 1) * 128],
                                                  self.identf[:])
                    tT = PE.mark(ins)
                    xt_free[sl] = tT
                    ch, jj = i // 4, i % 4
                    xs = ch % 2
                    E1, E2 = (ACT, DVE) if i % 2 == 0 else (DVE, ACT)
                    cpy = lambda E, o, i_: E.mark(nc.scalar.copy(out=o, in_=i_) if E is ACT
                                                  else nc.vector.tensor_copy(out=o, in_=i_))
                    E1.wait(tT, xTf_free[xs])
                    tXf = cpy(E1, xTf[xs][:, :, jj * 128:(jj + 1) * 128], pX[sl][:].rearrange("p (k t) -> p k t", k=8))
                    pX_free[sl] = tXf
                    E2.wait(tXf, xT_free)
                    tX = cpy(E2, xT[:, :, i * 128:(i + 1) * 128], xTf[xs][:, :, jj * 128:(jj + 1) * 128])
                    tokX.append(tX)
                    if jj == 3:
                        PE.wait(t_wf, t_pad, ACT.last, DVE.last)
                        for (c0f, M, scale) in ((0, 128, 0.125), (128, 128, 0.125), (256, 128, 1.0)):
                            Mo = 64 if c0f == 256 else 128
                            b = pzi % 4
                            pzi += 1
                            PE.wait(pz_free[b])
                            for k in range(8):
                                ins = nc.tensor.matmul(pz[b][0:M, :], lhsT=w1f[:, k, c0f:c0f + M], rhs=xTf[xs][:, k, :],
                                                       start=(k == 0), stop=(k == 7))
                            tM = PE.mark(ins)
                            fs = fi % 3
                            fi += 1
                            te = ev_copy(zstf[fs][0:Mo, :], pz[b][0:Mo, :], scale, [tM, zstf_free[fs]])
                            pz_free[b] = te
                            zstf_free[fs] = self.dma(SP, self.zqf[li, sq, c0f:c0f + Mo, ch * 512:(ch + 1) * 512],
                                                     zstf[fs][0:Mo, :], s_f[fs], [te])
                        b = pzi % 4
                        pzi += 1
                        PE.wait(pz_free[b])
                        for j4 in range(4):
                            for k in range(8):
                                ins = nc.tensor.matmul(pz[b][:, j4 * 4:(j4 + 1) * 4],
                                                       lhsT=xTf[xs][:, k, j4 * 128:(j4 + 1) * 128],
                                                       rhs=w1f[:, k, 384:388], start=(k == 0), stop=(k == 7))
                        tM = PE.mark(ins)
                        xTf_free[xs] = tM
                        ws = ch % 2
                        te = ev_copy(wst4[ws][:].rearrange("p j h -> p (j h)"), pz[b][:, 0:16], 0.5,
                                     [tM, wst4_free[ws]])
                        pz_free[b] = te
                        wst4_free[ws] = self.dma(SP, self.zw[li, sq, ch * 512:(ch + 1) * 512, :].rearrange(
                            "(j p) h -> p j h", p=128), wst4[ws][:], s_w4[ws], [te])
                zi = 0
                for (c0, M, scale) in fm_groups:
                    zs = zi % 2
                    zi += 1
                    tE = []
                    for tc in range(4):
                        b = pzi % 4
                        pzi += 1
                        PE.wait(t_w, pz_free[b], tokX[tc * 4:(tc + 1) * 4])
                        for k in range(8):
                            ins = nc.tensor.matmul(pz[b][0:M, :], lhsT=w1[:, k, c0:c0 + M],
                                                   rhs=xT[:, k, tc * 512:(tc + 1) * 512], start=(k == 0), stop=(k == 7))
                        tM = PE.mark(ins)
                        te = ev_copy(zst[zs][0:M, tc * 512:(tc + 1) * 512], pz[b][0:M, :], scale, [tM, zst_free[zs]])
                        pz_free[b] = te
                        tE.append(te)
                    zst_free[zs] = self.dma(SP, self.zq[li, sq, c0:c0 + M, :], zst[zs][0:M, :], s_z[zs], tE)
                for i in range(NT):
                    sl = i % 2
                    bA = pzi % 4
                    bB = (pzi + 1) % 4
                    pzi += 2
                    PE.wait(t_w, pz_free[bA], pz_free[bB], tokX)
                    lhs = lambda k: xT[:, k, i * 128:(i + 1) * 128]
                    for (bank, o0, n, c0) in ((bA, 0, 512, C_VC), (bB, 0, 64, C_VA), (bB, 68, 256, C_VB)):
                        for k in range(8):
                            ins = nc.tensor.matmul(pz[bank][:, o0:o0 + n], lhsT=lhs(k), rhs=w1[:, k, c0:c0 + n],
                                                   start=(k == 0), stop=(k == 7))
                    tM = PE.mark(ins)
                    E = ACT if i % 2 == 0 else DVE
                    E.wait(tM, vst_free[sl], t_ones[sl])
                    if E is ACT:
                        cp = lambda o, i_: ACT.mark(nc.scalar.copy(out=o, in_=i_))
                        wsc = lambda o, i_: ACT.mark(nc.scalar.activation(out=o, in_=i_, func=AF.Copy, scale=0.5))
                    else:
                        cp = lambda o, i_: DVE.mark(nc.vector.tensor_copy(out=o, in_=i_))
                        wsc = lambda o, i_: DVE.mark(nc.vector.tensor_scalar(o, i_, 0.5, None, ALU.mult))
                    cp(vst[sl][:, 5:13, 0:64], pz[bA][:, :].rearrange("p (h d) -> p h d", h=8))
                    cp(vst[sl][:, 0, 0:64], pz[bB][:, 0:64])
                    te = cp(vst[sl][:, 1:5, 0:64], pz[bB][:, 68:324].rearrange("p (h d) -> p h d", h=4))
                    pz_free[bA] = te
                    pz_free[bB] = te
                    vst_free[sl] = self.dma(SP, self.zv[li, sq, i * 128:(i + 1) * 128, :],
                                            vst[sl][:].rearrange("p h d -> p (h d)"), s_v[sl], [te])
                xT_free = PE.last

    def phase2(self, li):
        nc = self.nc
        PE, ACT, DVE, POOL, SP = self.PE, self.ACT, self.DVE, self.POOL, self.SP
        lam_init = self.lam_inits[li]
        V = nc.vector
        with ExitStack() as c:
            sb = lambda n, s, d: c.enter_context(nc.sbuf_tensor("L%d_%s" % (li, n), list(s), d))
            ps = lambda n, s, d: c.enter_context(nc.psum_tensor("L%d_%s" % (li, n), list(s), d))
            aq = [sb("p2_aq%d" % i, [68, S], BF16) for i in range(4)]
            ak = sb("p2_ak", [68, S], BF16)
            aqi = [sb("p2_aqi%d" % i, [68, S], F32) for i in range(4)]
            aki = sb("p2_aki", [68, S], F32)
            av = sb("p2_av", [128, NT, 65], BF16)
            awi = sb("p2_awi", [128, NT, 4], F32)
            bc = [[sb("p2_bc%d_%d" % (s_, i), [68, S], BF16) for i in range(4)] for s_ in range(2)]
            bcv = [sb("p2_bcv%d" % s_, [128, NT, 65], BF16) for s_ in range(2)]
            acc4 = sb("p2_acc4", [128, 4, S], F32)
            mneg = [sb("p2_mneg0", [128, 4, S], BF16)] * 2
            junk = sb("p2_junk", [128, S], BF16)
            ones_t = sb("p2_ones", [128, S], BF16)
            zr = sb("p2_zr", [128, S], F32)
            identN = sb("p2_identN", [128, 128], BF16)
            R = [sb("p2_R%d" % i, [128, 512], F32) for i in range(2)]
            Pt = [sb("p2_P%d" % i, [128, 512], BF16) for i in range(3)]
            oTs = [sb("p2_oTs%d" % i, [65, 512], F32) for i in range(2)]
            ostg = [sb("p2_ostg%d" % i, [128, 4, 64], BF16) for i in range(2)]
            sm = sb("p2_sm", [128, 96], F32)
            stp = sb("p2_stp", [128, NIT, 4], F32)
            lamt = sb("p2_lam", [128, 128], F32)
            lamp = sb("p2_lamp", [128, 64], F32)
            gsc = sb("p2_gsc", [128, 64], F32)
            t1 = sb("p2_t1", [128, 4, 64], F32)
            osb = sb("p2_osb", [128, 4, 64], F32)
            sqj = sb("p2_sqj", [128, 64], F32)
            epst = sb("p2_eps", [128, 1], F32)
            pS = [ps("p2_pS%d" % i, [128, 512], F32) for i in range(3)]
            pO = [ps("p2_pO%d" % i, [128, 512], F32) for i in range(2)]
            pT = [ps("p2_pT%d" % i, [128, 512], F32) for i in range(3)]
            s_c = self.dsem("p2c%d" % li)
            s_a = self.dsem("p2a%d" % li)
            s_bc = [self.dsem("p2bc%d_%d" % (li, i)) for i in range(2)]
            s_o = [self.dsem("p2o%d_%d" % (li, i)) for i in range(2)]

            t_eps = POOL.mark(nc.gpsimd.memset(epst[:], LN_EPS))
            ACT.wait(t_eps)
            t_on = POOL.mark(nc.gpsimd.memset(ones_t[:], 1.0))
            DVE.wait(t_on)
            t_idn = DVE.mark(V.tensor_scalar(identN[:], self.ident[:], NEG, None, ALU.mult))
            PE.wait(t_idn)
            for t_ in aqi + [aki]:
                POOL.mark(nc.gpsimd.memset(t_[64:68, :], 0.0))
            for s_ in range(2):
                for t_ in bc[s_]:
                    t_zero = POOL.mark(nc.gpsimd.memset(t_[:, :], 0.0))
            SP.wait(t_zero)
            t_l = self.dma(SP, lamt[:], self.lam_r[li], s_c)
            t_g = self.dma(SP, gsc[:], self.sg_r[li], s_c)
            DVE.wait(t_l, t_g)
            a = DVE.mark(V.tensor_tensor(out=lamp[:, 0:32], in0=lamt[:, 0:32], in1=lamt[:, 32:64], op=ALU.mult))
            a = DVE.mark(V.tensor_tensor(out=lamp[:, 32:64], in0=lamt[:, 64:96], in1=lamt[:, 96:128], op=ALU.mult))
            DVE.wait(a)
            a = DVE.mark(V.tensor_reduce(out=sm[:, 0:2], in_=lamp[:].rearrange("p (a d) -> p a d", a=2),
                                         axis=AX.X, op=ALU.add))
            ACT.wait(a)
            a = ACT.mark(nc.scalar.activation(out=sm[:, 2:4], in_=sm[:, 0:2], func=AF.Exp))
            DVE.wait(a)
            a = DVE.mark(V.tensor_tensor(out=sm[:, 4:5], in0=sm[:, 3:4], in1=sm[:, 2:3], op=ALU.subtract))
            DVE.wait(a)
            t_nlam = DVE.mark(V.tensor_scalar(sm[:, 5:6], sm[:, 4:5], float(lam_init), None, ALU.subtract))
            t_gsc = DVE.mark(V.tensor_scalar(gsc[:], gsc[:], float(1.0 - lam_init), None, ALU.mult))
            nlam = sm[:, 5:6]

            st = dict(si=0, pi=0, oi=0, ti=0, ei=0, gi=0, ri=0)
            pS_free = [None] * 3
            P_free = [None] * 3
            pO_free = [None] * 2
            pT_free = [None] * 3
            oTs_free = [None] * 2
            ostg_free = [None] * 2
            R_free = [None, None]
            mneg_free = [None]
            a_free = None
            bc_free = [None, None]
            pend = []

            def flush_pv(keep):
                while len(pend) > keep:
                    pend.pop(0)()

            def dv(inst):
                t = DVE.mark(inst)
                DVE.wait(t)
                return t

            def emit_unit(G, qT, kT, Kr, vt, mkind, marg, final_fn):
                bt0 = 4 * G
                nb = bt0 + 4
                ob = st["oi"] % 2
                st["oi"] += 1
                for bs in range(nb):
                    j0 = max(0, bs - bt0)
                    c0 = j0 * 128
                    sbk = st["si"] % 3
                    st["si"] += 1
                    PE.wait(pS_free[sbk])
                    ins = nc.tensor.matmul(pS[sbk][:, c0:512], lhsT=kT[0:Kr, bs * 128:(bs + 1) * 128],
                                           rhs=qT[0:Kr, bt0 * 128 + c0:bt0 * 128 + 512], start=True,
                                           stop=(mkind == "B" and bs < bt0))
                    if mkind == "C":
                        ins = nc.tensor.matmul(pS[sbk][:, c0:512], lhsT=self.ident[:],
                                               rhs=self.lc[:, bt0 + j0 - bs:bt0 + 4 - bs, :].rearrange("p d t -> p (d t)"),
                                               start=False, stop=True)
                    elif mkind == "B":
                        if bs >= bt0:
                            j = bs - bt0
                            ins = nc.tensor.matmul(pS[sbk][:, j * 128:(j + 1) * 128], lhsT=self.ident[:],
                                                   rhs=self.lcaus[:], start=False, stop=True)
                    else:
                        for j in range(j0, 4):
                            ins = nc.tensor.matmul(pS[sbk][:, j * 128:(j + 1) * 128],
                                                   lhsT=marg[:, j, bs * 128:(bs + 1) * 128], rhs=identN[:],
                                                   start=False, stop=True)
                    tS = PE.mark(ins)
                    pi = st["pi"] % 3
                    st["pi"] += 1
                    ACT.wait(tS, P_free[pi])
                    tP = ACT.mark(nc.scalar.activation(out=Pt[pi][:, c0:512], in_=pS[sbk][:, c0:512], func=AF.Exp))
                    pS_free[sbk] = tP

                    def pv(bs=bs, c0=c0, pi=pi, tP=tP):
                        PE.wait(tP)
                        if bs == 0:
                            PE.wait(pO_free[ob])
                        tV = PE.mark(nc.tensor.matmul(pO[ob][0:65, c0:512], lhsT=vt[:, bs, :], rhs=Pt[pi][:, c0:512],
                                                      start=(bs == 0), stop=(bs == nb - 1)))
                        P_free[pi] = tV
                        if bs == nb - 1:
                            es = st["ei"] % 2
                            st["ei"] += 1
                            ACT.wait(tV, oTs_free[es])
                            tE = ACT.mark(nc.scalar.copy(out=oTs[es][:, :], in_=pO[ob][0:65, :]))
                            pO_free[ob] = tE

                            def tr():
                                tb = st["ti"] % 3
                                st["ti"] += 1
                                PE.wait(tE, pT_free[tb])
                                for j in range(4):
                                    ins2 = nc.tensor.transpose(pT[tb][:, j * 65:(j + 1) * 65],
                                                               oTs[es][0:65, j * 128:(j + 1) * 128],
                                                               self.identf[0:65, 0:65])
                                tT = PE.mark(ins2)
                                oTs_free[es] = tT
                                final_fn(tb, tT)
                            pend.append(tr)
                    pend.append(pv)
                    flush_pv(2)

            def store_out(G, col, osl, waits):
                dst = self.om[li, cur["sq"], G * 512:(G + 1) * 512, col:col + 64].rearrange("(j p) c -> p j c", p=128)
                with nc.allow_non_contiguous_dma(reason="64-col head slice"):
                    ostg_free[osl] = self.dma(SP, dst, ostg[osl][:], s_o[osl], waits)

            def norm_final(G, col):
                def f(tb, tT):
                    osl = st["gi"] % 2
                    st["gi"] += 1
                    rec = sm[:, 32 + 4 * tb:36 + 4 * tb]
                    DVE.wait(tT)
                    r = DVE.mark(V.reciprocal(out=rec, in_=pT[tb][:, 0:260].rearrange("p (j d) -> p j d", d=65)[:, :, 64]))
                    ACT.wait(r, ostg_free[osl])
                    for j in range(4):
                        tA = ACT.mark(nc.scalar.activation(out=ostg[osl][:, j, :], in_=pT[tb][:, j * 65:j * 65 + 64],
                                                           func=AF.Copy, scale=rec[:, j:j + 1]))
                    pT_free[tb] = tA
                    store_out(G, col, osl, [tA])
                return f

            bstate = {}

            def b_final(G, h, m):
                def f(tb, tT):
                    bstate[m] = (tb, tT)
                    if m == 0:
                        return
                    (b1, tv1), (b2, tv2) = bstate[0], bstate[1]
                    osl = st["gi"] % 2
                    st["gi"] += 1
                    rec1, rec2, nl2, ss, lnv, rstd = (sm[:, 48:52], sm[:, 52:56], sm[:, 56:60], sm[:, 60:64],
                                                      sm[:, 64:68], sm[:, 68:72])
                    v1 = pT[b1][:, 0:260].rearrange("p (j d) -> p j d", d=65)
                    v2 = pT[b2][:, 0:260].rearrange("p (j d) -> p j d", d=65)
                    DVE.wait(tv1, tv2, t_nlam, t_gsc)
                    DVE.mark(V.reciprocal(out=rec1, in_=v1[:, :, 64]))
                    dv(V.reciprocal(out=rec2, in_=v2[:, :, 64]))
                    dv(V.tensor_scalar(nl2, rec2, nlam, None, ALU.mult))
                    for j in range(4):
                        a_ = DVE.mark(V.tensor_scalar(t1[:, j, :], v1[:, j, 0:64], rec1[:, j:j + 1], None, ALU.mult))
                    DVE.wait(a_)
                    pT_free[b1] = a_
                    for j in range(4):
                        a_ = DVE.mark(V.scalar_tensor_tensor(out=osb[:, j, :], in0=v2[:, j, 0:64], scalar=nl2[:, j:j + 1],
                                                             in1=t1[:, j, :], op0=ALU.mult, op1=ALU.add))
                    DVE.wait(a_)
                    pT_free[b2] = a_
                    for j in range(4):
                        a_ = DVE.mark(V.scalar_tensor_tensor(out=sqj[:], in0=osb[:, j, :], scalar=1.0, in1=osb[:, j, :],
                                                             op0=ALU.mult, op1=ALU.mult, accum_out=ss[:, j:j + 1]))
                    ACT.wait(a_)
                    a6 = ACT.mark(nc.scalar.activation(out=lnv, in_=ss, func=AF.Ln, scale=1.0 / 64.0, bias=epst[:, 0:1]))
                    ACT.wait(a6)
                    a7 = ACT.mark(nc.scalar.activation(out=rstd, in_=lnv, func=AF.Exp, scale=-0.5))
                    DVE.wait(a7, ostg_free[osl])
                    for j in range(4):
                        a_ = DVE.mark(V.scalar_tensor_tensor(out=ostg[osl][:, j, :], in0=osb[:, j, :],
                                                             scalar=rstd[:, j:j + 1], in1=gsc[:], op0=ALU.mult,
                                                             op1=ALU.mult))
                    DVE.wait(a_)
                    store_out(G, 256 + 64 * h, osl, [a_])
                return f

            cur = dict(sq=0)

            def idx_and_bisect(G):
                sl = G % 2
                M_ = mneg[sl]
                last = None
                for j in range(4):
                    bt = 4 * G + j
                    nk = (bt + 1) * 128
                    for cidx in range((nk + 511) // 512):
                        n = min(512, nk - cidx * 512)
                        for h in range(4):
                            sbk = st["si"] % 3
                            st["si"] += 1
                            PE.wait(pS_free[sbk])
                            tS = PE.mark(nc.tensor.matmul(pS[sbk][:, 0:n], lhsT=aqi[h][0:68, bt * 128:(bt + 1) * 128],
                                                          rhs=aki[0:68, cidx * 512:cidx * 512 + n], start=True,
                                                          stop=True))
                            dst = acc4[:, j, cidx * 512:cidx * 512 + n]
                            if h == 0:
                                DVE.wait(tS)
                                last = DVE.mark(V.tensor_scalar(dst, pS[sbk][:, 0:n], 0.0, awi[:, bt, 0:1], ALU.max,
                                                                ALU.mult))
                                pS_free[sbk] = last
                            else:
                                rs = st["ri"] % 2
                                st["ri"] += 1
                                ACT.wait(tS, R_free[rs])
                                tR = ACT.mark(nc.scalar.activation(out=R[rs][:, 0:n], in_=pS[sbk][:, 0:n], func=AF.Relu))
                                pS_free[sbk] = tR
                                DVE.wait(tR, last)
                                last = DVE.mark(V.scalar_tensor_tensor(out=dst, in0=R[rs][:, 0:n],
                                                                       scalar=awi[:, bt, h:h + 1], in1=dst,
                                                                       op0=ALU.mult, op1=ALU.add))
                                R_free[rs] = last
                yield
                am, Aa, mid, cnt, gg, tt = (sm[:, 8:12], sm[:, 12:16], sm[:, 16:20], sm[:, 20:24], sm[:, 24:28],
                                            sm[:, 28:32])
                need = sm[:, 72:76]
                nks = [(4 * G + j + 1) * 128 for j in range(4)]
                DVE.wait(last)
                for j in range(4):
                    a_ = DVE.mark(V.tensor_reduce(out=am[:, j:j + 1], in_=acc4[:, j, 0:nks[j]], axis=AX.X, op=ALU.max,
                                                  apply_absolute_value=True))
                DVE.wait(a_)
                dv(V.tensor_scalar(Aa, am, 1.0001, 1e-30, ALU.mult, ALU.add))
                for j in range(4):
                    DVE.mark(V.tensor_scalar(stp[:, :, j], self.pow2[:], Aa[:, j:j + 1], None, ALU.mult))
                    DVE.mark(V.tensor_tensor(out=acc4[:, j, nks[j] - 128:nks[j]], in0=acc4[:, j, nks[j] - 128:nks[j]],
                                             in1=self.causf[:], op=ALU.add))
                dv(V.memset(mid, 0.0))
                yield
                for k in range(NIT):
                    for j in range(4):
                        yield
                        a_ = DVE.mark(V.tensor_scalar(junk[:, 0:nks[j]], acc4[:, j, 0:nks[j]], mid[:, j:j + 1], 0.0,
                                                      ALU.is_gt, ALU.add, accum_out=cnt[:, j:j + 1]))
                    yield
                    DVE.wait(a_)
                    if k == 0:
                        dv(V.tensor_scalar(need, cnt, -1.0, 256.0, ALU.mult, ALU.add))
                        dv(V.tensor_scalar(need, need, 0.0, None, ALU.max))
                    dv(V.tensor_scalar(gg, cnt, 255.5, 0.5 if k < NIT - 1 else 1.0, ALU.is_ge, ALU.subtract))
                    dv(V.tensor_tensor(out=tt, in0=gg, in1=stp[:, k, :], op=ALU.mult))
                    dv(V.tensor_tensor(out=mid, in0=mid, in1=tt, op=ALU.add))
                DVE.wait(mneg_free[0])
                for j in range(4):
                    n_ = nks[j]
                    yield
                    dv(V.tensor_scalar(junk[:, 0:n_], acc4[:, j, 0:n_], 0.0, None, ALU.is_equal))
                    yield
                    dv(V.tensor_tensor_scan(out=zr[:, 0:n_], data0=ones_t[:, 0:n_], data1=junk[:, 0:n_], initial=0.0,
                                            op0=ALU.mult, op1=ALU.add))
                    yield
                    dv(V.scalar_tensor_tensor(out=junk[:, 0:n_], in0=zr[:, 0:n_], scalar=need[:, j:j + 1],
                                              in1=junk[:, 0:n_], op0=ALU.is_gt, op1=ALU.mult))
                    yield
                    tM = dv(V.scalar_tensor_tensor(out=M_[:, j, 0:n_], in0=acc4[:, j, 0:n_], scalar=mid[:, j:j + 1],
                                                   in1=junk[:, 0:n_], op0=ALU.is_le, op1=ALU.max))
                tmn[G] = tM

            for sq in range(self.nseq):
                cur["sq"] = sq
                zq = self.zq[li, sq]
                zv = self.zv[li, sq].rearrange("(i p) (h d) -> p i h d", p=128, d=65)
                for h in range(4):
                    self.dma(SP, aq[h][0:64, :], zq[C_QA + 64 * h:C_QA + 64 * (h + 1), :], s_a, [a_free])
                    self.dma(SP, aq[h][64:68, :], self.qaug_b[2 * h + 1], s_a)
                    self.dma(SP, aqi[h][0:64, :], self.zqf[li, sq, 64 * h:64 * (h + 1), :], s_a)
                self.dma(SP, ak[0:64, :], zq[C_KA:C_KA + 64, :], s_a)
                self.dma(SP, ak[64:68, :], self.kaug_b, s_a)
                self.dma(SP, aki[0:64, :], self.zqf[li, sq, 256:320, :], s_a)
                self.dma(SP, av[:], zv[:, :, 0, :], s_a)
                t_la = self.dma(SP, awi[:], self.zw[li, sq].rearrange("(i p) h -> p i h", p=128), s_a)

                jobs = [("B", 0), ("B", 1), ("C", 0), ("C", 1), ("B", 2), ("C", 2), ("C", 3), ("C", 4),
                        ("B", 3), ("C", 5), ("C", 6), ("C", 7)]
                grp_units = {0: 8, 1: 16, 2: 20, 3: 20}
                job_tok = {}

                def load_job(ji):
                    kind, h = jobs[ji]
                    sl = ji % 2
                    T = bc[sl]
                    w = [bc_free[sl]]
                    if kind == "B":
                        for m in range(2):
                            c0 = C_QB + h * 64 + m * 32
                            self.dma(SP, T[2 * m][0:32, :], zq[c0:c0 + 32, :], s_bc[sl], w)
                            self.dma(SP, T[2 * m][32:36, :], self.qaug_b[2 * h + 1], s_bc[sl])
                            c0 = C_KB + h * 64 + m * 32
                            self.dma(SP, T[2 * m + 1][0:32, :], zq[c0:c0 + 32, :], s_bc[sl])
                            self.dma(SP, T[2 * m + 1][32:36, :], self.kaug_b, s_bc[sl])
                        job_tok[ji] = self.dma(SP, bcv[sl][:], zv[:, :, 1 + h, :], s_bc[sl])
                    else:
                        self.dma(SP, T[0][0:64, :], zq[C_QC + 64 * h:C_QC + 64 * (h + 1), :], s_bc[sl], w)
                        self.dma(SP, T[0][64:68, :], self.qaug_b[h], s_bc[sl])
                        self.dma(SP, T[2][0:64, :], zq[C_KC + 64 * h:C_KC + 64 * (h + 1), :], s_bc[sl])
                        self.dma(SP, T[2][64:68, :], self.kaug_b, s_bc[sl])
                        job_tok[ji] = self.dma(SP, bcv[sl][:], zv[:, :, 5 + h, :], s_bc[sl])

                load_job(0)
                load_job(1)

                PE.wait(t_la, t_zero)
                DVE.wait(t_la)
                tmn = {}

                def step(gen, n):
                    for _ in range(n):
                        try:
                            next(gen)
                        except StopIteration:
                            return

                gen = iter(())
                curG = None
                nstep = 1

                def finish_group():
                    G = curG
                    step(gen, 1000)
                    flush_pv(0)
                    PE.wait(tmn[G])
                    for hh in range(4):
                        emit_unit(G, aq[hh], ak, 68, av, "A", mneg[0], norm_final(G, 64 * hh))
                    flush_pv(0)
                    mneg_free[0] = PE.last

                for ji, (kind, h) in enumerate(jobs):
                    sl = ji % 2
                    T = bc[sl]
                    if kind == "B":
                        if curG is not None:
                            finish_group()
                        curG = h
                        gen = idx_and_bisect(h)
                        nstep = -(-(NIT * 5 + 20) // grp_units[h])
                        step(gen, 1)
                    PE.wait(job_tok[ji])
                    for G in range(4):
                        if kind == "B":
                            for m in range(2):
                                emit_unit(G, T[2 * m], T[2 * m + 1], 68, bcv[sl], "B", None, b_final(G, h, m))
                                step(gen, nstep)
                        else:
                            emit_unit(G, T[0], T[2], 68, bcv[sl], "C", None, norm_final(G, 512 + 64 * h))
                            step(gen, nstep)
                    flush_pv(0)
                    bc_free[sl] = PE.last
                    if ji + 2 < len(jobs):
                        load_job(ji + 2)
                finish_group()
                a_free = PE.last

    def phase3(self, li, xsrc, ydst):
        nc = self.nc
        PE, ACT, DVE, POOL, SP = self.PE, self.ACT, self.DVE, self.POOL, self.SP
        with ExitStack() as c:
            sb = lambda n, s, d: c.enter_context(nc.sbuf_tensor("L%d_%s" % (li, n), list(s), d))
            ps = lambda n, s, d: c.enter_context(nc.psum_tensor("L%d_%s" % (li, n), list(s), d))
            wo = sb("p3_wo", [128, 8, D], BF16)
            wd = sb("p3_wd", [128, NFF, D], BF16)
            lnc = sb("p3_ln", [128, 4, D], F32)
            wgu = [sb("p3_wgu%d" % i, [128, 2, 1024], BF16) for i in range(3)]
            ot = [sb("p3_ot%d" % i, [128, D], BF16) for i in range(2)]
            xt = [sb("p3_xt%d" % i, [128, D], F32) for i in range(2)]
            oT = sb("p3_oT", [128, 8, 128], BF16)
            x1 = [[sb("p3_x1_%d_%d" % (p_, i), [128, D], F32) for i in range(4)] for p_ in range(2)]
            x1T = [sb("p3_x1T%d" % p_, [128, 8, 512], BF16) for p_ in range(2)]
            hT = sb("p3_hT", [128, NFF, 512], BF16)
            sg = [sb("p3_sg%d" % i, [128, 512], F32) for i in range(2)]
            yt = [sb("p3_yt%d" % i, [128, D], F32) for i in range(2)]
            stt = sb("p3_stt", [128, 2, 6], F32)
            sm = sb("p3_sm", [128, 16], F32)
            eps = sb("p3_eps", [128, 1], F32)
            pOT = ps("p3_pOT", [128, 1024], BF16)
            pXT = ps("p3_pXT", [128, 1024], F32)
            pM = ps("p3_pM", [128, 1024], F32)
            pG = [ps("p3_pG%d" % i, [128, 512], F32) for i in range(3)]
            s_w = self.dsem("p3w%d" % li)
            s_o = [self.dsem("p3o%d_%d" % (li, i)) for i in range(2)]
            s_x = [self.dsem("p3x%d_%d" % (li, i)) for i in range(2)]
            s_g = [self.dsem("p3g%d_%d" % (li, i)) for i in range(3)]
            s_y = [self.dsem("p3y%d_%d" % (li, i)) for i in range(2)]
            t_w = [self.dma(SP, wo[:], self.wob[li].rearrange("(k p) n -> p k n", p=128), s_w),
                   self.dma(SP, wd[:], self.wdb[li].rearrange("(k p) n -> p k n", p=128), s_w),
                   self.dma(SP, lnc[:], self.ln_r[li].rearrange("a p n -> p a n"), s_w)]
            t_eps = POOL.mark(nc.gpsimd.memset(eps[:], LN_EPS))
            ACT.wait(t_eps)
            V = nc.vector

            def dv(inst):
                t = DVE.mark(inst)
                DVE.wait(t)
                return t

            def layer_norm(src, dst, gi):
                dv(V.bn_stats(out=stt[:, 0, :], in_=src[:, 0:512]))
                dv(V.bn_stats(out=stt[:, 1, :], in_=src[:, 512:1024]))
                a = dv(V.bn_aggr(out=sm[:, 0:2], in_=stt[:].rearrange("p a s -> p (a s)")))
                ACT.wait(a)
                a = ACT.mark(nc.scalar.activation(out=sm[:, 2:3], in_=sm[:, 1:2], func=AF.Sqrt, bias=eps[:, 0:1],
                                                  scale=1.0))
                DVE.wait(a)
                dv(V.reciprocal(out=sm[:, 3:4], in_=sm[:, 2:3]))
                dv(V.tensor_scalar(dst, src, sm[:, 0:1], sm[:, 3:4], ALU.subtract, ALU.mult))
                dv(V.tensor_tensor(out=dst, in0=dst, in1=lnc[:, gi, :], op=ALU.mult))
                return dv(V.tensor_tensor(out=dst, in0=dst, in1=lnc[:, gi + 1, :], op=ALU.add))

            ot_free = [None, None]
            xt_free = [None, None]
            wgu_free = [None] * 3
            pG_free = [None] * 3
            sg_free = [None, None]
            yt_free = [None, None]
            oT_free = None
            pOT_free = None
            pXT_free = None
            pM_free = None
            x1T_free = None
            hT_free = None
            r_free = None
            gi_ = 0
            ci_ = 0
            groups = [(sq, grp) for sq in range(self.nseq) for grp in range(4)]
            x1_toks = {}
            x1T_frees = [None, None]
            st3 = dict(pOT_free=None, oT_free=None, pM_free=None, pXT_free=None)

            def a_stage(gidx):
                sq, grp = groups[gidx]
                par = gidx % 2
                toks = []
                ln_tok = {}

                def front(tl):
                    i = grp * 4 + tl
                    sl = i % 2
                    t_o = self.dma(SP, ot[sl][:], self.om[li, sq, i * 128:(i + 1) * 128, :], s_o[sl], [ot_free[sl]])
                    t_x = self.dma(SP, xt[sl][:], xsrc[sq, i * 128:(i + 1) * 128, :], s_x[sl], [xt_free[sl]])
                    PE.wait(t_o, st3["pOT_free"])
                    for k in range(8):
                        ins = nc.tensor.transpose(pOT[:, k * 128:(k + 1) * 128], ot[sl][:, k * 128:(k + 1) * 128],
                                                  self.ident[:])
                    tT = PE.mark(ins)
                    ot_free[sl] = tT
                    ACT.wait(tT, st3["oT_free"])
                    tC = ACT.mark(nc.scalar.copy(out=oT[:].rearrange("p k t -> p (k t)"), in_=pOT[:]))
                    st3["pOT_free"] = tC
                    PE.wait(tC, st3["pM_free"], t_w)
                    for half in range(2):
                        for k in range(8):
                            ins = nc.tensor.matmul(pM[:, half * 512:(half + 1) * 512], lhsT=oT[:, k, :],
                                                   rhs=wo[:, k, half * 512:(half + 1) * 512], start=(k == 0),
                                                   stop=(k == 7))
                    tM = PE.mark(ins)
                    st3["oT_free"] = tM
                    DVE.wait(tM, t_x)
                    a = dv(V.scalar_tensor_tensor(out=x1[par][tl][:], in0=xt[sl][:], scalar=float(ALPHA), in1=pM[:],
                                                  op0=ALU.mult, op1=ALU.add))
                    st3["pM_free"] = a
                    xt_free[sl] = a
                    ln_tok[tl] = layer_norm(x1[par][tl][:], x1[par][tl][:], 0)

                def back(tl):
                    PE.wait(ln_tok[tl], st3["pXT_free"])
                    for k in range(8):
                        ins = nc.tensor.transpose(pXT[:, k * 128:(k + 1) * 128], x1[par][tl][:, k * 128:(k + 1) * 128],
                                                  self.identf[:])
                    tT = PE.mark(ins)
                    ACT.wait(tT, x1T_frees[par])
                    tC = ACT.mark(nc.scalar.copy(out=x1T[par][:, :, tl * 128:(tl + 1) * 128],
                                                 in_=pXT[:].rearrange("p (k t) -> p k t", k=8)))
                    st3["pXT_free"] = tC
                    toks.append(tC)

                front(0)
                yield
                for tl in range(1, 4):
                    front(tl)
                    yield
                    back(tl - 1)
                    yield
                back(3)
                x1_toks[gidx] = toks

            def run_all(gen):
                for _ in gen:
                    pass

            run_all(a_stage(0))
            for gidx, (sq, grp) in enumerate(groups):
                par = gidx % 2
                gen = a_stage(gidx + 1) if gidx + 1 < len(groups) else iter(())
                for cidx in range(NFF):
                    ws = ci_ % 3
                    ci_ += 1
                    t_g = self.dma(SP, wgu[ws][:], self.wgub[li, cidx], s_g[ws], [wgu_free[ws]])
                    bg = gi_ % 3
                    bu = (gi_ + 1) % 3
                    gi_ += 2
                    PE.wait(t_g, x1_toks[gidx], pG_free[bg], pG_free[bu])
                    for (bank, j) in ((bg, 0), (bu, 1)):
                        for k in range(8):
                            ins = nc.tensor.matmul(pG[bank][:, :], lhsT=wgu[ws][:, j, k * 128:(k + 1) * 128],
                                                   rhs=x1T[par][:, k, :], start=(k == 0), stop=(k == 7))
                    tM = PE.mark(ins)
                    wgu_free[ws] = tM
                    ss = cidx % 2
                    ACT.wait(tM, sg_free[ss])
                    tS = ACT.mark(nc.scalar.activation(out=sg[ss][:], in_=pG[bg][:, :], func=AF.Silu))
                    pG_free[bg] = tS
                    DVE.wait(tS, hT_free if cidx == 0 else None)
                    tH = DVE.mark(V.tensor_tensor(out=hT[:, cidx, :], in0=sg[ss][:], in1=pG[bu][:, :], op=ALU.mult))
                    pG_free[bu] = tH
                    sg_free[ss] = tH
                    if cidx % 3 == 1:
                        next(gen, None)
                x1T_frees[par] = PE.last
                run_all(gen)
                for tl in range(4):
                    i = grp * 4 + tl
                    ys = i % 2
                    PE.wait(tH, st3["pM_free"])
                    for cidx in range(NFF):
                        for half in range(2):
                            ins = nc.tensor.matmul(pM[:, half * 512:(half + 1) * 512],
                                                   lhsT=hT[:, cidx, tl * 128:(tl + 1) * 128],
                                                   rhs=wd[:, cidx, half * 512:(half + 1) * 512], start=(cidx == 0),
                                                   stop=(cidx == NFF - 1))
                    tM = PE.mark(ins)
                    DVE.wait(tM, yt_free[ys])
                    a = dv(V.scalar_tensor_tensor(out=yt[ys][:], in0=x1[par][tl][:], scalar=float(ALPHA), in1=pM[:],
                                                  op0=ALU.mult, op1=ALU.add))
                    st3["pM_free"] = a
                    a = layer_norm(yt[ys][:], yt[ys][:], 2)
                    yt_free[ys] = self.dma(SP, ydst[sq, i * 128:(i + 1) * 128, :], yt[ys][:], s_y[ys], [a])
                hT_free = PE.last


def make_consts():
    t = np.arange(128)
    lc = np.zeros((16, 128, 128), np.float32)
    for dl in range(16):
        dist = 128 * dl + t[None, :] - t[:, None]
        m = (((dist >= 0) & (dist <= 128)).astype(np.int64)
             + ((dist >= 0) & (dist <= 512) & (dist % 4 == 0))
             + ((dist >= 0) & (dist <= 2048) & (dist % 16 == 0)))
        lc[dl] = np.where(m > 0, np.log(np.maximum(m, 1)), NEG)
    pos = np.arange(S)
    rs, bs = (pos % 128).astype(np.float32), (pos // 128).astype(np.float32)
    kaug = np.stack([rs, np.ones(S, np.float32), bs, np.ones(S, np.float32)])
    qaug = np.zeros((8, 4, S), np.float32)
    for j in range(8):
        sl = 2.0 ** -(j + 1)
        qaug[j] = np.stack([np.full(S, sl, np.float32), -sl * rs, np.full(S, 128 * sl, np.float32), -128 * sl * bs])
    ident = np.eye(128, dtype=np.float32)
    causf = np.where(t[None, :] > t[:, None], -1e30, 0.0).astype(np.float32)
    lcaus = np.where(t[:, None] > t[None, :], NEG, 0.0).astype(np.float32)
    pow2 = np.tile((2.0 ** -np.arange(NIT)).astype(np.float32)[None, :], (128, 1))
    return dict(c_lc=lc, c_kaug=kaug, c_qaug=qaug, c_ident=ident, c_causf=causf, c_lcaus=lcaus, c_pow2=pow2)


_CACHE = {}


def get_nc(nl, nseq, lam_inits, debug=False):
    key = (nl, nseq, tuple(lam_inits), debug)
    if key not in _CACHE:
        _CACHE[key] = K(nl, nseq, lam_inits, debug).build()
    return _CACHE[key]


def layer_inputs(layers, w_in, w_o, lam, subln_g, ln1_g, ln1_b, w_gate, w_up, w_down, ln2_g, ln2_b):
    f = lambda a: np.ascontiguousarray(np.asarray(a, dtype=np.float32))
    L = list(layers)
    rep = lambda v: np.ascontiguousarray(np.broadcast_to(np.asarray(v, np.float32)[None, :], (128, v.shape[-1])))
    d = dict(w_in=f(w_in[L]), w_o=f(w_o[L]), w_gate=f(w_gate[L]), w_up=f(w_up[L]), w_down=f(w_down[L]))
    d["lam_r"] = np.stack([rep(np.asarray(lam[l]).reshape(-1)) for l in L])
    d["sg_r"] = np.stack([rep(np.asarray(subln_g[l])) for l in L])
    d["ln_r"] = np.stack([np.stack([rep(np.asarray(v[l])) for v in (ln1_g, ln1_b, ln2_g, ln2_b)]) for l in L])
    d.update(make_consts())
    return d


def lam_init_of(l):
    return 0.8 - 0.6 * math.exp(-0.3 * l)


def kernel(x, w_in, w_o, lam, subln_g, ln1_g, ln1_b, w_gate, w_up, w_down, ln2_g, ln2_b):
    x = np.ascontiguousarray(np.asarray(x, dtype=np.float32))
    args = (w_in, w_o, lam, subln_g, ln1_g, ln1_b, w_gate, w_up, w_down, ln2_g, ln2_b)
    args = tuple(np.asarray(a) for a in args)
    nl = 2
    nc = get_nc(nl, SEQ_PER_CORE, [lam_init_of(l) for l in range(nl)])
    shared = layer_inputs(range(nl), *args)
    in_maps = []
    for c in range(N_CORES):
        m = dict(shared)
        m["x"] = x[c * SEQ_PER_CORE:(c + 1) * SEQ_PER_CORE]
        in_maps.append(m)
    res = run_bass_kernel_spmd(nc, in_maps, core_ids=list(range(N_CORES)))
    return np.concatenate([np.asarray(r["y"]) for r in res.results], axis=0).astype(np.float32)
```

```python
import math
from contextlib import ExitStack

import numpy as np
import concourse.bass as bass
import concourse.mybir as mybir
from concourse.bass_utils import run_bass_kernel_spmd

F32 = mybir.dt.float32
BF16 = mybir.dt.bfloat16
AF = mybir.ActivationFunctionType
ALU = mybir.AluOpType
AX = mybir.AxisListType

S = 2048
D = 1024
NT = 16
FF = 2816
NFF = 22
INC = 3012
ALPHA = 4.0 ** 0.25
LN_EPS = 1e-5
NIT = 14
NEG = -30000.0
N_CORES = 8
SEQ_PER_CORE = 4

C_QA, C_KA, C_VA, C_QI, C_KI, C_WI, C_QB, C_KB, C_VB, C_QC, C_KC, C_VC = (
    0, 256, 320, 384, 640, 704, 708, 964, 1220, 1476, 1988, 2500)


class Sem:
    def __init__(self, h):
        self.h = h
        self.n = 0


class Eng:
    def __init__(self, k, eng, name):
        self.e = eng
        self.sem = Sem(k.new_sem("e_" + name))
        self.seen = {}
        self.last = None

    def wait(self, *toks):
        for t in toks:
            if t is None:
                continue
            if isinstance(t, list):
                self.wait(*t)
                continue
            s, v = t
            if self.seen.get(s, 0) >= v:
                continue
            self.e.wait_ge(s.h, v)
            self.seen[s] = v

    def mark(self, inst):
        self.sem.n += 1
        inst.then_inc(self.sem.h, 1)
        self.last = (self.sem, self.sem.n)
        return self.last


class K:
    def __init__(self, nl, nseq, lam_inits, debug=False):
        self.nl, self.nseq, self.lam_inits, self.debug = nl, nseq, lam_inits, debug
        self.nc = bass.Bass("TRN2", target_bir_lowering=False)
        self.ctx = ExitStack()
        self.dsems = []
        self.sem_pool = []
        self.in_use = []

    def new_sem(self, name):
        return self.ctx.enter_context(self.nc.semaphore(name))

    def dsem(self, name):
        if self.sem_pool:
            s = self.sem_pool.pop()
        else:
            s = Sem(self.new_sem("d%d" % len(self.dsems)))
            self.dsems.append(s)
        self.in_use.append(s)
        return s

    def dram(self, name, shape, dt, kind="Internal"):
        return self.nc.dram_tensor(name, list(shape), dt, kind=kind).ap()

    def dma(self, q, out, in_, sem, waits=(), **kw):
        q.wait(*waits)
        inst = q.e.dma_start(out=out, in_=in_, **kw)
        sem.n += 16
        inst.then_inc(sem.h, 16)
        return (sem, sem.n)

    def barrier(self):
        sp = self.SP
        for e in self.engs:
            if e is not sp:
                sp.wait(e.last)
        for s in self.dsems:
            if s.n:
                sp.wait((s, s.n))
        tok = self.dma(sp, self.bar_b[:], self.bar_a[:], self.bar_sem)
        for e in self.engs:
            e.wait(tok)
        self.sem_pool.extend(self.in_use)
        self.in_use = []

    def build(self):
        nc, nl, nseq = self.nc, self.nl, self.nseq
        ctx = self.ctx
        dbg = "ExternalOutput" if self.debug else "Internal"
        ein = lambda n, s: self.dram(n, s, F32, "ExternalInput")
        self.x_in = ein("x", [nseq, S, D])
        self.w_in = ein("w_in", [nl, D, INC])
        self.w_o = ein("w_o", [nl, D, D])
        self.w_g = ein("w_gate", [nl, D, FF])
        self.w_u = ein("w_up", [nl, D, FF])
        self.w_d = ein("w_down", [nl, FF, D])
        self.lam_r = ein("lam_r", [nl, 128, 128])
        self.sg_r = ein("sg_r", [nl, 128, 64])
        self.ln_r = ein("ln_r", [nl, 4, 128, D])
        self.c_lc = ein("c_lc", [16, 128, 128])
        self.c_kaug = ein("c_kaug", [4, S])
        self.c_qaug = ein("c_qaug", [8, 4, S])
        self.c_ident = ein("c_ident", [128, 128])
        self.c_causf = ein("c_causf", [128, 128])
        self.c_lcaus = ein("c_lcaus", [128, 128])
        self.c_pow2 = ein("c_pow2", [128, NIT])
        self.y_out = self.dram("y", [nseq, S, D], F32, "ExternalOutput")
        self.w1b = self.dram("w1b", [nl, D, INC], BF16)
        self.wob = self.dram("wob", [nl, D, D], BF16)
        self.wgub = self.dram("wgub", [nl, NFF, 128, 2, 1024], BF16)
        self.wdb = self.dram("wdb", [nl, FF, D], BF16)
        self.kaug_b = self.dram("kaug_b", [4, S], BF16)
        self.qaug_b = self.dram("qaug_b", [8, 4, S], BF16)
        self.zq = self.dram("zq", [nl, nseq, INC, S], BF16, dbg)
        self.zv = self.dram("zv", [nl, nseq, S, 13 * 65], BF16, dbg)
        self.zw = self.dram("zw", [nl, nseq, S, 4], F32, dbg)
        self.zqf = self.dram("zqf", [nl, nseq, 320, S], F32, dbg)
        self.om = self.dram("om", [nl, nseq, S, D], BF16, dbg)
        self.xmid = [self.dram("xmid%d" % i, [nseq, S, D], F32, dbg) for i in range(nl - 1)]

        with ctx:
            self.PE = Eng(self, nc.tensor, "pe")
            self.ACT = Eng(self, nc.scalar, "act")
            self.DVE = Eng(self, nc.vector, "dve")
            self.POOL = Eng(self, nc.gpsimd, "pool")
            self.SP = Eng(self, nc.sync, "sp")
            self.engs = [self.PE, self.ACT, self.DVE, self.POOL, self.SP]
            self.bar_sem = Sem(self.new_sem("bar"))
            sb = lambda n, s, d: ctx.enter_context(nc.sbuf_tensor(n, list(s), d))
            self.bar_a = sb("bar_a", [1, 8], F32)
            self.bar_b = sb("bar_b", [1, 8], F32)
            self.ident = sb("ident", [128, 128], BF16)
            self.identf = sb("identf", [128, 128], F32)
            self.lcaus = sb("lcaus", [128, 128], BF16)
            self.causf = sb("causf", [128, 128], F32)
            self.pow2 = sb("pow2", [128, NIT], F32)
            self.lc = sb("lc", [128, 16, 128], BF16)
            self.phase0()
            self.barrier()
            for li in range(nl):
                xsrc = self.x_in if li == 0 else self.xmid[li - 1]
                ydst = self.y_out if li == nl - 1 else self.xmid[li]
                self.phase1(li, xsrc)
                self.barrier()
                self.phase2(li)
                self.barrier()
                self.phase3(li, xsrc, ydst)
                self.barrier()
        return nc

    def phase0(self):
        nc = self.nc
        P, SP = self.POOL, self.SP
        sw = self.dsem("w0")
        P.mark(nc.gpsimd.memset(self.bar_a[:], 0.0))
        cd = self.cast_dma_fn(sw)
        cd(self.ident[:], self.c_ident)
        cd(self.lcaus[:], self.c_lcaus)
        cd(self.lc[:], self.c_lc.rearrange("d s t -> s d t"))
        cd(self.kaug_b, self.c_kaug)
        cd(self.qaug_b, self.c_qaug)
        self.dma(SP, self.identf[:], self.c_ident, sw)
        self.dma(SP, self.causf[:], self.c_causf, sw)
        self.dma(SP, self.pow2[:], self.c_pow2, sw)
        self.convert_weights(0, sw)

    def cast_dma_fn(self, sw):
        ncd = [0]

        def cd(o, i):
            ncd[0] += 1
            t = self.dma(self.POOL, o, i, sw, max_dma_last_dim=4096)
            if ncd[0] % 6 == 0:
                self.POOL.wait(t)
            return t
        return cd

    def convert_weights(self, l, sw):
        cd = self.cast_dma_fn(sw)
        for kc in range(8):
            cd(self.w1b[l, kc * 128:(kc + 1) * 128, :], self.w_in[l, kc * 128:(kc + 1) * 128, :])
        for h in range(2):
            cd(self.wob[l, h * 512:(h + 1) * 512, :], self.w_o[l, h * 512:(h + 1) * 512, :])
        for c in range(NFF):
            for j, w in enumerate((self.w_g, self.w_u)):
                cd(self.wgub[l, c, :, j, :].rearrange("p (k f) -> p k f", k=8),
                   w[l, :, c * 128:(c + 1) * 128].rearrange("(k p) f -> p k f", p=128))
        for h in range(NFF):
            cd(self.wdb[l, h * 128:(h + 1) * 128, :], self.w_d[l, h * 128:(h + 1) * 128, :])

    def phase1(self, li, xsrc):
        nc = self.nc
        PE, ACT, DVE, POOL, SP = self.PE, self.ACT, self.DVE, self.POOL, self.SP
        with ExitStack() as c:
            sb = lambda n, s, d: c.enter_context(nc.sbuf_tensor("L%d_%s" % (li, n), list(s), d))
            ps = lambda n, s, d: c.enter_context(nc.psum_tensor("L%d_%s" % (li, n), list(s), d))
            w1 = sb("p1_w1", [128, 8, INC], BF16)
            xT = sb("p1_xT", [128, 8, S], BF16)
            xt = [sb("p1_xt%d" % i, [128, D], F32) for i in range(2)]
            zst = [sb("p1_zst%d" % i, [128, S], BF16) for i in range(2)]
            vst = [sb("p1_vst%d" % i, [128, 13, 65], BF16) for i in range(2)]
            wst4 = [sb("p1_wst4_%d" % i, [128, 4, 4], F32) for i in range(2)]
            xTf = [sb("p1_xTf%d" % i, [128, 8, 512], F32) for i in range(2)]
            w1f = sb("p1_w1f", [128, 8, 388], F32)
            zstf = [sb("p1_zstf%d" % i, [128, 512], F32) for i in range(3)]
            pX = [ps("p1_pX%d" % i, [128, 1024], F32) for i in range(2)]
            pz = [ps("p1_pz%d" % i, [128, 512], F32) for i in range(4)]
            s_w = self.dsem("p1w%d" % li)
            s_x = [self.dsem("p1x%d_%d" % (li, i)) for i in range(2)]
            s_z = [self.dsem("p1z%d_%d" % (li, i)) for i in range(2)]
            s_v = [self.dsem("p1v%d_%d" % (li, i)) for i in range(2)]
            s_f = [self.dsem("p1f%d_%d" % (li, i)) for i in range(3)]
            s_w4 = [self.dsem("p1w4%d_%d" % (li, i)) for i in range(2)]
            t_w = self.dma(SP, w1[:], self.w1b[li].rearrange("(k p) n -> p k n", p=128), s_w)
            t_pad = POOL.mark(nc.gpsimd.memset(w1f[:, :, 320:384], 0.0))
            wsrc = self.w_in[li].rearrange("(k p) n -> p k n", p=128)
            self.dma(SP, w1f[:, :, 0:320], wsrc[:, :, C_QI:C_QI + 320], s_w)
            t_wf = self.dma(SP, w1f[:, :, 384:388], wsrc[:, :, C_WI:C_WI + 4], s_w)
            xTf_free = [None, None]
            zstf_free = [None] * 3
            wst4_free = [None, None]
            fi = 0
            t_ones = [POOL.mark(nc.gpsimd.memset(vst[i][:, :, 64:65], 1.0)) for i in range(2)]
            xt_free = [None, None]
            pX_free = [None, None]
            pz_free = [None] * 4
            zst_free = [None, None]
            vst_free = [None, None]
            xT_free = None
            pzi = 0
            evi = 0

            def ev_copy(out, in_, scale, waits):
                nonlocal evi
                evi += 1
                if evi % 2 == 0:
                    ACT.wait(*waits)
                    if scale == 1.0:
                        return ACT.mark(nc.scalar.copy(out=out, in_=in_))
                    return ACT.mark(nc.scalar.activation(out=out, in_=in_, func=AF.Copy, scale=float(scale)))
                DVE.wait(*waits)
                if scale == 1.0:
                    return DVE.mark(nc.vector.tensor_copy(out=out, in_=in_))
                return DVE.mark(nc.vector.tensor_scalar(out, in_, float(scale), None, ALU.mult))

            fm_groups = ([(C_QA + 128 * i, 128, 0.125) for i in range(2)] + [(C_KA, 64, 1.0)]
                         + [(C_QB + 128 * i, 128, 32.0 ** -0.5) for i in range(2)]
                         + [(C_KB + 128 * i, 128, 1.0) for i in range(2)]
                         + [(C_QC + 128 * i, 128, 0.125) for i in range(4)]
                         + [(C_KC + 128 * i, 128, 1.0) for i in range(4)])
            for sq in range(self.nseq):
                tokX = []
                for i in range(NT):
                    sl = i % 2
                    t_ld = self.dma(SP, xt[sl][:], xsrc[sq, i * 128:(i + 1) * 128, :], s_x[sl], [xt_free[sl]])
                    PE.wait(t_ld, pX_free[sl])
                    for k in range(8):
                        ins = nc.tensor.transpose(pX[sl][:, k * 128:(k + 1) * 128], xt[sl][:, k * 128:(k + 1) * 128],
                                                  self.identf[:])
                    tT = PE.mark(ins)
                    xt_free[sl] = tT
                    ch, jj = i // 4, i % 4
                    xs = ch % 2
                    E1, E2 = (ACT, DVE) if i % 2 == 0 else (DVE, ACT)
                    cpy = lambda E, o, i_: E.mark(nc.scalar.copy(out=o, in_=i_) if E is ACT
                                                  else nc.vector.tensor_copy(out=o, in_=i_))
                    E1.wait(tT, xTf_free[xs])
                    tXf = cpy(E1, xTf[xs][:, :, jj * 128:(jj + 1) * 128], pX[sl][:].rearrange("p (k t) -> p k t", k=8))
                    pX_free[sl] = tXf
                    E2.wait(tXf, xT_free)
                    tX = cpy(E2, xT[:, :, i * 128:(i + 1) * 128], xTf[xs][:, :, jj * 128:(jj + 1) * 128])
                    tokX.append(tX)
                    if jj == 3:
                        PE.wait(t_wf, t_pad, ACT.last, DVE.last)
                        for (c0f, M, scale) in ((0, 128, 0.125), (128, 128, 0.125), (256, 128, 1.0)):
                            Mo = 64 if c0f == 256 else 128
                            b = pzi % 4
                            pzi += 1
                            PE.wait(pz_free[b])
                            for k in range(8):
                                ins = nc.tensor.matmul(pz[b][0:M, :], lhsT=w1f[:, k, c0f:c0f + M], rhs=xTf[xs][:, k, :],
                                                       start=(k == 0), stop=(k == 7))
                            tM = PE.mark(ins)
                            fs = fi % 3
                            fi += 1
                            te = ev_copy(zstf[fs][0:Mo, :], pz[b][0:Mo, :], scale, [tM, zstf_free[fs]])
                            pz_free[b] = te
                            zstf_free[fs] = self.dma(SP, self.zqf[li, sq, c0f:c0f + Mo, ch * 512:(ch + 1) * 512],
                                                     zstf[fs][0:Mo, :], s_f[fs], [te])
                        b = pzi % 4
                        pzi += 1
                        PE.wait(pz_free[b])
                        for j4 in range(4):
                            for k in range(8):
                                ins = nc.tensor.matmul(pz[b][:, j4 * 4:(j4 + 1) * 4],
                                                       lhsT=xTf[xs][:, k, j4 * 128:(j4 + 1) * 128],
                                                       rhs=w1f[:, k, 384:388], start=(k == 0), stop=(k == 7))
                        tM = PE.mark(ins)
                        xTf_free[xs] = tM
                        ws = ch % 2
                        te = ev_copy(wst4[ws][:].rearrange("p j h -> p (j h)"), pz[b][:, 0:16], 0.5,
                                     [tM, wst4_free[ws]])
                        pz_free[b] = te
                        wst4_free[ws] = self.dma(SP, self.zw[li, sq, ch * 512:(ch + 1) * 512, :].rearrange(
                            "(j p) h -> p j h", p=128), wst4[ws][:], s_w4[ws], [te])
                zi = 0
                for (c0, M, scale) in fm_groups:
                    zs = zi % 2
                    zi += 1
                    tE = []
                    for tc in range(4):
                        b = pzi % 4
                        pzi += 1
                        PE.wait(t_w, pz_free[b], tokX[tc * 4:(tc + 1) * 4])
                        for k in range(8):
                            ins = nc.tensor.matmul(pz[b][0:M, :], lhsT=w1[:, k, c0:c0 + M],
                                                   rhs=xT[:, k, tc * 512:(tc + 1) * 512], start=(k == 0), stop=(k == 7))
                        tM = PE.mark(ins)
                        te = ev_copy(zst[zs][0:M, tc * 512:(tc + 1) * 512], pz[b][0:M, :], scale, [tM, zst_free[zs]])
                        pz_free[b] = te
                        tE.append(te)
                    zst_free[zs] = self.dma(SP, self.zq[li, sq, c0:c0 + M, :], zst[zs][0:M, :], s_z[zs], tE)
                for i in range(NT):
                    sl = i % 2
                    bA = pzi % 4
                    bB = (pzi + 1) % 4
                    pzi += 2
                    PE.wait(t_w, pz_free[bA], pz_free[bB], tokX)
                    lhs = lambda k: xT[:, k, i * 128:(i + 1) * 128]
                    for (bank, o0, n, c0) in ((bA, 0, 512, C_VC), (bB, 0, 64, C_VA), (bB, 68, 256, C_VB)):
                        for k in range(8):
                            ins = nc.tensor.matmul(pz[bank][:, o0:o0 + n], lhsT=lhs(k), rhs=w1[:, k, c0:c0 + n],
                                                   start=(k == 0), stop=(k == 7))
                    tM = PE.mark(ins)
                    E = ACT if i % 2 == 0 else DVE
                    E.wait(tM, vst_free[sl], t_ones[sl])
                    if E is ACT:
                        cp = lambda o, i_: ACT.mark(nc.scalar.copy(out=o, in_=i_))
                        wsc = lambda o, i_: ACT.mark(nc.scalar.activation(out=o, in_=i_, func=AF.Copy, scale=0.5))
                    else:
                        cp = lambda o, i_: DVE.mark(nc.vector.tensor_copy(out=o, in_=i_))
                        wsc = lambda o, i_: DVE.mark(nc.vector.tensor_scalar(o, i_, 0.5, None, ALU.mult))
                    cp(vst[sl][:, 5:13, 0:64], pz[bA][:, :].rearrange("p (h d) -> p h d", h=8))
                    cp(vst[sl][:, 0, 0:64], pz[bB][:, 0:64])
                    te = cp(vst[sl][:, 1:5, 0:64], pz[bB][:, 68:324].rearrange("p (h d) -> p h d", h=4))
                    pz_free[bA] = te
                    pz_free[bB] = te
                    vst_free[sl] = self.dma(SP, self.zv[li, sq, i * 128:(i + 1) * 128, :],
                                            vst[sl][:].rearrange("p h d -> p (h d)"), s_v[sl], [te])
                xT_free = PE.last

    def phase2(self, li):
        nc = self.nc
        PE, ACT, DVE, POOL, SP = self.PE, self.ACT, self.DVE, self.POOL, self.SP
        lam_init = self.lam_inits[li]
        V = nc.vector
        with ExitStack() as c:
            sb = lambda n, s, d: c.enter_context(nc.sbuf_tensor("L%d_%s" % (li, n), list(s), d))
            ps = lambda n, s, d: c.enter_context(nc.psum_tensor("L%d_%s" % (li, n), list(s), d))
            aq = [sb("p2_aq%d" % i, [68, S], BF16) for i in range(4)]
            ak = sb("p2_ak", [68, S], BF16)
            aqi = [sb("p2_aqi%d" % i, [68, S], F32) for i in range(4)]
            aki = sb("p2_aki", [68, S], F32)
            av = sb("p2_av", [128, NT, 65], BF16)
            awi = sb("p2_awi", [128, NT, 4], F32)
            bc = [[sb("p2_bc%d_%d" % (s_, i), [68, S], BF16) for i in range(4)] for s_ in range(2)]
            bcv = [sb("p2_bcv%d" % s_, [128, NT, 65], BF16) for s_ in range(2)]
            acc4 = sb("p2_acc4", [128, 4, S], F32)
            mneg = [sb("p2_mneg0", [128, 4, S], BF16)] * 2
            junk = sb("p2_junk", [128, S], BF16)
            ones_t = sb("p2_ones", [128, S], BF16)
            zr = sb("p2_zr", [128, S], F32)
            identN = sb("p2_identN", [128, 128], BF16)
            R = [sb("p2_R%d" % i, [128, 512], F32) for i in range(2)]
            Pt = [sb("p2_P%d" % i, [128, 512], BF16) for i in range(3)]
            oTs = [sb("p2_oTs%d" % i, [65, 512], F32) for i in range(2)]
            ostg = [sb("p2_ostg%d" % i, [128, 4, 64], BF16) for i in range(2)]
            sm = sb("p2_sm", [128, 96], F32)
            stp = sb("p2_stp", [128, NIT, 4], F32)
            lamt = sb("p2_lam", [128, 128], F32)
            lamp = sb("p2_lamp", [128, 64], F32)
            gsc = sb("p2_gsc", [128, 64], F32)
            t1 = sb("p2_t1", [128, 4, 64], F32)
            osb = sb("p2_osb", [128, 4, 64], F32)
            sqj = sb("p2_sqj", [128, 64], F32)
            epst = sb("p2_eps", [128, 1], F32)
            pS = [ps("p2_pS%d" % i, [128, 512], F32) for i in range(3)]
            pO = [ps("p2_pO%d" % i, [128, 512], F32) for i in range(2)]
            pT = [ps("p2_pT%d" % i, [128, 512], F32) for i in range(3)]
            s_c = self.dsem("p2c%d" % li)
            s_a = self.dsem("p2a%d" % li)
            s_bc = [self.dsem("p2bc%d_%d" % (li, i)) for i in range(2)]
            s_o = [self.dsem("p2o%d_%d" % (li, i)) for i in range(2)]

            t_eps = POOL.mark(nc.gpsimd.memset(epst[:], LN_EPS))
            ACT.wait(t_eps)
            t_on = POOL.mark(nc.gpsimd.memset(ones_t[:], 1.0))
            DVE.wait(t_on)
            t_idn = DVE.mark(V.tensor_scalar(identN[:], self.ident[:], NEG, None, ALU.mult))
            PE.wait(t_idn)
            for t_ in aqi + [aki]:
                POOL.mark(nc.gpsimd.memset(t_[64:68, :], 0.0))
            for s_ in range(2):
                for t_ in bc[s_]:
                    t_zero = POOL.mark(nc.gpsimd.memset(t_[:, :], 0.0))
            SP.wait(t_zero)
            if li + 1 < self.nl:
                swn = Sem(self.new_sem("wnext%d" % li))
                self.dsems.append(swn)
                self.convert_weights(li + 1, swn)
            t_l = self.dma(SP, lamt[:], self.lam_r[li], s_c)
            t_g = self.dma(SP, gsc[:], self.sg_r[li], s_c)
            DVE.wait(t_l, t_g)
            a = DVE.mark(V.tensor_tensor(out=lamp[:, 0:32], in0=lamt[:, 0:32], in1=lamt[:, 32:64], op=ALU.mult))
            a = DVE.mark(V.tensor_tensor(out=lamp[:, 32:64], in0=lamt[:, 64:96], in1=lamt[:, 96:128], op=ALU.mult))
            DVE.wait(a)
            a = DVE.mark(V.tensor_reduce(out=sm[:, 0:2], in_=lamp[:].rearrange("p (a d) -> p a d", a=2),
                                         axis=AX.X, op=ALU.add))
            ACT.wait(a)
            a = ACT.mark(nc.scalar.activation(out=sm[:, 2:4], in_=sm[:, 0:2], func=AF.Exp))
            DVE.wait(a)
            a = DVE.mark(V.tensor_tensor(out=sm[:, 4:5], in0=sm[:, 3:4], in1=sm[:, 2:3], op=ALU.subtract))
            DVE.wait(a)
            t_nlam = DVE.mark(V.tensor_scalar(sm[:, 5:6], sm[:, 4:5], float(lam_init), None, ALU.subtract))
            t_gsc = DVE.mark(V.tensor_scalar(gsc[:], gsc[:], float(1.0 - lam_init), None, ALU.mult))
            nlam = sm[:, 5:6]

            st = dict(si=0, pi=0, oi=0, ti=0, ei=0, gi=0, ri=0)
            pS_free = [None] * 3
            P_free = [None] * 3
            pO_free = [None] * 2
            pT_free = [None] * 3
            oTs_free = [None] * 2
            ostg_free = [None] * 2
            R_free = [None, None]
            mneg_free = [None]
            a_free = None
            bc_free = [None, None]
            pend = []

            def flush_pv(keep):
                while len(pend) > keep:
                    pend.pop(0)()

            def dv(inst):
                t = DVE.mark(inst)
                DVE.wait(t)
                return t

            def emit_unit(G, qT, kT, Kr, vt, mkind, marg, final_fn):
                bt0 = 4 * G
                nb = bt0 + 4
                ob = st["oi"] % 2
                st["oi"] += 1
                for bs in range(nb):
                    j0 = max(0, bs - bt0)
                    c0 = j0 * 128
                    sbk = st["si"] % 3
                    st["si"] += 1
                    PE.wait(pS_free[sbk])
                    ins = nc.tensor.matmul(pS[sbk][:, c0:512], lhsT=kT[0:Kr, bs * 128:(bs + 1) * 128],
                                           rhs=qT[0:Kr, bt0 * 128 + c0:bt0 * 128 + 512], start=True,
                                           stop=(mkind == "B" and bs < bt0))
                    if mkind == "C":
                        ins = nc.tensor.matmul(pS[sbk][:, c0:512], lhsT=self.ident[:],
                                               rhs=self.lc[:, bt0 + j0 - bs:bt0 + 4 - bs, :].rearrange("p d t -> p (d t)"),
                                               start=False, stop=True)
                    elif mkind == "B":
                        if bs >= bt0:
                            j = bs - bt0
                            ins = nc.tensor.matmul(pS[sbk][:, j * 128:(j + 1) * 128], lhsT=self.ident[:],
                                                   rhs=self.lcaus[:], start=False, stop=True)
                    else:
                        for j in range(j0, 4):
                            ins = nc.tensor.matmul(pS[sbk][:, j * 128:(j + 1) * 128],
                                                   lhsT=marg[:, j, bs * 128:(bs + 1) * 128], rhs=identN[:],
                                                   start=False, stop=True)
                    tS = PE.mark(ins)
                    pi = st["pi"] % 3
                    st["pi"] += 1
                    ACT.wait(tS, P_free[pi])
                    tP = ACT.mark(nc.scalar.activation(out=Pt[pi][:, c0:512], in_=pS[sbk][:, c0:512], func=AF.Exp))
                    pS_free[sbk] = tP

                    def pv(bs=bs, c0=c0, pi=pi, tP=tP):
                        PE.wait(tP)
                        if bs == 0:
                            PE.wait(pO_free[ob])
                        tV = PE.mark(nc.tensor.matmul(pO[ob][0:65, c0:512], lhsT=vt[:, bs, :], rhs=Pt[pi][:, c0:512],
                                                      start=(bs == 0), stop=(bs == nb - 1)))
                        P_free[pi] = tV
                        if bs == nb - 1:
                            es = st["ei"] % 2
                            st["ei"] += 1
                            ACT.wait(tV, oTs_free[es])
                            tE = ACT.mark(nc.scalar.copy(out=oTs[es][:, :], in_=pO[ob][0:65, :]))
                            pO_free[ob] = tE

                            def tr():
                                tb = st["ti"] % 3
                                st["ti"] += 1
                                PE.wait(tE, pT_free[tb])
                                for j in range(4):
                                    ins2 = nc.tensor.transpose(pT[tb][:, j * 65:(j + 1) * 65],
                                                               oTs[es][0:65, j * 128:(j + 1) * 128],
                                                               self.identf[0:65, 0:65])
                                tT = PE.mark(ins2)
                                oTs_free[es] = tT
                                final_fn(tb, tT)
                            pend.append(tr)
                    pend.append(pv)
                    flush_pv(2)

            def store_out(G, col, osl, waits):
                dst = self.om[li, cur["sq"], G * 512:(G + 1) * 512, col:col + 64].rearrange("(j p) c -> p j c", p=128)
                with nc.allow_non_contiguous_dma(reason="64-col head slice"):
                    ostg_free[osl] = self.dma(SP, dst, ostg[osl][:], s_o[osl], waits)

            def norm_final(G, col):
                def f(tb, tT):
                    osl = st["gi"] % 2
                    st["gi"] += 1
                    rec = sm[:, 32 + 4 * tb:36 + 4 * tb]
                    DVE.wait(tT)
                    r = DVE.mark(V.reciprocal(out=rec, in_=pT[tb][:, 0:260].rearrange("p (j d) -> p j d", d=65)[:, :, 64]))
                    ACT.wait(r, ostg_free[osl])
                    for j in range(4):
                        tA = ACT.mark(nc.scalar.activation(out=ostg[osl][:, j, :], in_=pT[tb][:, j * 65:j * 65 + 64],
                                                           func=AF.Copy, scale=rec[:, j:j + 1]))
                    pT_free[tb] = tA
                    store_out(G, col, osl, [tA])
                return f

            bstate = {}

            def b_final(G, h, m):
                def f(tb, tT):
                    bstate[m] = (tb, tT)
                    if m == 0:
                        return
                    (b1, tv1), (b2, tv2) = bstate[0], bstate[1]
                    osl = st["gi"] % 2
                    st["gi"] += 1
                    rec1, rec2, nl2, ss, lnv, rstd = (sm[:, 48:52], sm[:, 52:56], sm[:, 56:60], sm[:, 60:64],
                                                      sm[:, 64:68], sm[:, 68:72])
                    v1 = pT[b1][:, 0:260].rearrange("p (j d) -> p j d", d=65)
                    v2 = pT[b2][:, 0:260].rearrange("p (j d) -> p j d", d=65)
                    DVE.wait(tv1, tv2, t_nlam, t_gsc)
                    DVE.mark(V.reciprocal(out=rec1, in_=v1[:, :, 64]))
                    dv(V.reciprocal(out=rec2, in_=v2[:, :, 64]))
                    dv(V.tensor_scalar(nl2, rec2, nlam, None, ALU.mult))
                    for j in range(4):
                        a_ = DVE.mark(V.tensor_scalar(t1[:, j, :], v1[:, j, 0:64], rec1[:, j:j + 1], None, ALU.mult))
                    DVE.wait(a_)
                    pT_free[b1] = a_
                    for j in range(4):
                        a_ = DVE.mark(V.scalar_tensor_tensor(out=osb[:, j, :], in0=v2[:, j, 0:64], scalar=nl2[:, j:j + 1],
                                                             in1=t1[:, j, :], op0=ALU.mult, op1=ALU.add))
                    DVE.wait(a_)
                    pT_free[b2] = a_
                    for j in range(4):
                        a_ = DVE.mark(V.scalar_tensor_tensor(out=sqj[:], in0=osb[:, j, :], scalar=1.0, in1=osb[:, j, :],
                                                             op0=ALU.mult, op1=ALU.mult, accum_out=ss[:, j:j + 1]))
                    ACT.wait(a_)
                    a6 = ACT.mark(nc.scalar.activation(out=lnv, in_=ss, func=AF.Ln, scale=1.0 / 64.0, bias=epst[:, 0:1]))
                    ACT.wait(a6)
                    a7 = ACT.mark(nc.scalar.activation(out=rstd, in_=lnv, func=AF.Exp, scale=-0.5))
                    DVE.wait(a7, ostg_free[osl])
                    for j in range(4):
                        a_ = DVE.mark(V.scalar_tensor_tensor(out=ostg[osl][:, j, :], in0=osb[:, j, :],
                                                             scalar=rstd[:, j:j + 1], in1=gsc[:], op0=ALU.mult,
                                                             op1=ALU.mult))
                    DVE.wait(a_)
                    store_out(G, 256 + 64 * h, osl, [a_])
                return f

            cur = dict(sq=0)

            def idx_and_bisect(G):
                sl = G % 2
                M_ = mneg[sl]
                last = None
                for j in range(4):
                    bt = 4 * G + j
                    nk = (bt + 1) * 128
                    for cidx in range((nk + 511) // 512):
                        n = min(512, nk - cidx * 512)
                        for h in range(4):
                            sbk = st["si"] % 3
                            st["si"] += 1
                            PE.wait(pS_free[sbk])
                            tS = PE.mark(nc.tensor.matmul(pS[sbk][:, 0:n], lhsT=aqi[h][0:68, bt * 128:(bt + 1) * 128],
                                                          rhs=aki[0:68, cidx * 512:cidx * 512 + n], start=True,
                                                          stop=True))
                            dst = acc4[:, j, cidx * 512:cidx * 512 + n]
                            if h == 0:
                                DVE.wait(tS)
                                last = DVE.mark(V.tensor_scalar(dst, pS[sbk][:, 0:n], 0.0, awi[:, bt, 0:1], ALU.max,
                                                                ALU.mult))
                                pS_free[sbk] = last
                            else:
                                rs = st["ri"] % 2
                                st["ri"] += 1
                                ACT.wait(tS, R_free[rs])
                                tR = ACT.mark(nc.scalar.activation(out=R[rs][:, 0:n], in_=pS[sbk][:, 0:n], func=AF.Relu))
                                pS_free[sbk] = tR
                                DVE.wait(tR, last)
                                last = DVE.mark(V.scalar_tensor_tensor(out=dst, in0=R[rs][:, 0:n],
                                                                       scalar=awi[:, bt, h:h + 1], in1=dst,
                                                                       op0=ALU.mult, op1=ALU.add))
                                R_free[rs] = last
                yield
                am, Aa, mid, cnt, gg, tt = (sm[:, 8:12], sm[:, 12:16], sm[:, 16:20], sm[:, 20:24], sm[:, 24:28],
                                            sm[:, 28:32])
                need = sm[:, 72:76]
                nks = [(4 * G + j + 1) * 128 for j in range(4)]
                DVE.wait(last)
                for j in range(4):
                    a_ = DVE.mark(V.tensor_reduce(out=am[:, j:j + 1], in_=acc4[:, j, 0:nks[j]], axis=AX.X, op=ALU.max,
                                                  apply_absolute_value=True))
                DVE.wait(a_)
                dv(V.tensor_scalar(Aa, am, 1.0001, 1e-30, ALU.mult, ALU.add))
                for j in range(4):
                    DVE.mark(V.tensor_scalar(stp[:, :, j], self.pow2[:], Aa[:, j:j + 1], None, ALU.mult))
                    DVE.mark(V.tensor_tensor(out=acc4[:, j, nks[j] - 128:nks[j]], in0=acc4[:, j, nks[j] - 128:nks[j]],
                                             in1=self.causf[:], op=ALU.add))
                dv(V.memset(mid, 0.0))
                yield
                for k in range(NIT):
                    for j in range(4):
                        yield
                        a_ = DVE.mark(V.tensor_scalar(junk[:, 0:nks[j]], acc4[:, j, 0:nks[j]], mid[:, j:j + 1], 0.0,
                                                      ALU.is_gt, ALU.add, accum_out=cnt[:, j:j + 1]))
                    yield
                    DVE.wait(a_)
                    if k == 0:
                        dv(V.tensor_scalar(need, cnt, -1.0, 256.0, ALU.mult, ALU.add))
                        dv(V.tensor_scalar(need, need, 0.0, None, ALU.max))
                    dv(V.tensor_scalar(gg, cnt, 255.5, 0.5 if k < NIT - 1 else 1.0, ALU.is_ge, ALU.subtract))
                    dv(V.tensor_tensor(out=tt, in0=gg, in1=stp[:, k, :], op=ALU.mult))
                    dv(V.tensor_tensor(out=mid, in0=mid, in1=tt, op=ALU.add))
                DVE.wait(mneg_free[0])
                for j in range(4):
                    n_ = nks[j]
                    yield
                    dv(V.tensor_scalar(junk[:, 0:n_], acc4[:, j, 0:n_], 0.0, None, ALU.is_equal))
                    yield
                    dv(V.tensor_tensor_scan(out=zr[:, 0:n_], data0=ones_t[:, 0:n_], data1=junk[:, 0:n_], initial=0.0,
                                            op0=ALU.mult, op1=ALU.add))
                    yield
                    dv(V.scalar_tensor_tensor(out=junk[:, 0:n_], in0=zr[:, 0:n_], scalar=need[:, j:j + 1],
                                              in1=junk[:, 0:n_], op0=ALU.is_gt, op1=ALU.mult))
                    yield
                    tM = dv(V.scalar_tensor_tensor(out=M_[:, j, 0:n_], in0=acc4[:, j, 0:n_], scalar=mid[:, j:j + 1],
                                                   in1=junk[:, 0:n_], op0=ALU.is_le, op1=ALU.max))
                tmn[G] = tM

            for sq in range(self.nseq):
                cur["sq"] = sq
                zq = self.zq[li, sq]
                zv = self.zv[li, sq].rearrange("(i p) (h d) -> p i h d", p=128, d=65)
                for h in range(4):
                    self.dma(SP, aq[h][0:64, :], zq[C_QA + 64 * h:C_QA + 64 * (h + 1), :], s_a, [a_free])
                    self.dma(SP, aq[h][64:68, :], self.qaug_b[2 * h + 1], s_a)
                    self.dma(SP, aqi[h][0:64, :], self.zqf[li, sq, 64 * h:64 * (h + 1), :], s_a)
                self.dma(SP, ak[0:64, :], zq[C_KA:C_KA + 64, :], s_a)
                self.dma(SP, ak[64:68, :], self.kaug_b, s_a)
                self.dma(SP, aki[0:64, :], self.zqf[li, sq, 256:320, :], s_a)
                self.dma(SP, av[:], zv[:, :, 0, :], s_a)
                t_la = self.dma(SP, awi[:], self.zw[li, sq].rearrange("(i p) h -> p i h", p=128), s_a)

                jobs = [("B", 0), ("B", 1), ("C", 0), ("C", 1), ("B", 2), ("C", 2), ("C", 3), ("C", 4),
                        ("B", 3), ("C", 5), ("C", 6), ("C", 7)]
                grp_units = {0: 8, 1: 16, 2: 20, 3: 20}
                job_tok = {}

                def load_job(ji):
                    kind, h = jobs[ji]
                    sl = ji % 2
                    T = bc[sl]
                    w = [bc_free[sl]]
                    if kind == "B":
                        for m in range(2):
                            c0 = C_QB + h * 64 + m * 32
                            self.dma(SP, T[2 * m][0:32, :], zq[c0:c0 + 32, :], s_bc[sl], w)
                            self.dma(SP, T[2 * m][32:36, :], self.qaug_b[2 * h + 1], s_bc[sl])
                            c0 = C_KB + h * 64 + m * 32
                            self.dma(SP, T[2 * m + 1][0:32, :], zq[c0:c0 + 32, :], s_bc[sl])
                            self.dma(SP, T[2 * m + 1][32:36, :], self.kaug_b, s_bc[sl])
                        job_tok[ji] = self.dma(SP, bcv[sl][:], zv[:, :, 1 + h, :], s_bc[sl])
                    else:
                        self.dma(SP, T[0][0:64, :], zq[C_QC + 64 * h:C_QC + 64 * (h + 1), :], s_bc[sl], w)
                        self.dma(SP, T[0][64:68, :], self.qaug_b[h], s_bc[sl])
                        self.dma(SP, T[2][0:64, :], zq[C_KC + 64 * h:C_KC + 64 * (h + 1), :], s_bc[sl])
                        self.dma(SP, T[2][64:68, :], self.kaug_b, s_bc[sl])
                        job_tok[ji] = self.dma(SP, bcv[sl][:], zv[:, :, 5 + h, :], s_bc[sl])

                load_job(0)
                load_job(1)

                PE.wait(t_la, t_zero)
                DVE.wait(t_la)
                tmn = {}

                def step(gen, n):
                    for _ in range(n):
                        try:
                            next(gen)
                        except StopIteration:
                            return

                gen = iter(())
                curG = None
                nstep = 1

                def finish_group():
                    G = curG
                    step(gen, 1000)
                    flush_pv(0)
                    PE.wait(tmn[G])
                    for hh in range(4):
                        emit_unit(G, aq[hh], ak, 68, av, "A", mneg[0], norm_final(G, 64 * hh))
                    flush_pv(0)
                    mneg_free[0] = PE.last

                for ji, (kind, h) in enumerate(jobs):
                    sl = ji % 2
                    T = bc[sl]
                    if kind == "B":
                        if curG is not None:
                            finish_group()
                        curG = h
                        gen = idx_and_bisect(h)
                        nstep = -(-(NIT * 5 + 20) // grp_units[h])
                        step(gen, 1)
                    PE.wait(job_tok[ji])
                    for G in range(4):
                        if kind == "B":
                            for m in range(2):
                                emit_unit(G, T[2 * m], T[2 * m + 1], 68, bcv[sl], "B", None, b_final(G, h, m))
                                step(gen, nstep)
                        else:
                            emit_unit(G, T[0], T[2], 68, bcv[sl], "C", None, norm_final(G, 512 + 64 * h))
                            step(gen, nstep)
                    flush_pv(0)
                    bc_free[sl] = PE.last
                    if ji + 2 < len(jobs):
                        load_job(ji + 2)
                finish_group()
                a_free = PE.last

    def phase3(self, li, xsrc, ydst):
        nc = self.nc
        PE, ACT, DVE, POOL, SP = self.PE, self.ACT, self.DVE, self.POOL, self.SP
        with ExitStack() as c:
            sb = lambda n, s, d: c.enter_context(nc.sbuf_tensor("L%d_%s" % (li, n), list(s), d))
            ps = lambda n, s, d: c.enter_context(nc.psum_tensor("L%d_%s" % (li, n), list(s), d))
            wo = sb("p3_wo", [128, 8, D], BF16)
            wd = sb("p3_wd", [128, NFF, D], BF16)
            lnc = sb("p3_ln", [128, 4, D], F32)
            wgu = [sb("p3_wgu%d" % i, [128, 2, 1024], BF16) for i in range(3)]
            ot = [sb("p3_ot%d" % i, [128, D], BF16) for i in range(2)]
            xt = [sb("p3_xt%d" % i, [128, D], F32) for i in range(2)]
            oT = sb("p3_oT", [128, 8, 128], BF16)
            x1 = [[sb("p3_x1_%d_%d" % (p_, i), [128, D], F32) for i in range(4)] for p_ in range(2)]
            x1T = [sb("p3_x1T%d" % p_, [128, 8, 512], BF16) for p_ in range(2)]
            hT = sb("p3_hT", [128, NFF, 512], BF16)
            sg = [sb("p3_sg%d" % i, [128, 512], F32) for i in range(2)]
            yt = [sb("p3_yt%d" % i, [128, D], F32) for i in range(2)]
            stt = sb("p3_stt", [128, 2, 6], F32)
            sm = sb("p3_sm", [128, 16], F32)
            eps = sb("p3_eps", [128, 1], F32)
            pOT = ps("p3_pOT", [128, 1024], BF16)
            pXT = ps("p3_pXT", [128, 512], F32)
            pM = ps("p3_pM", [128, 1024], F32)
            pG = [ps("p3_pG%d" % i, [128, 512], F32) for i in range(4)]
            s_w = self.dsem("p3w%d" % li)
            s_o = [self.dsem("p3o%d_%d" % (li, i)) for i in range(2)]
            s_x = [self.dsem("p3x%d_%d" % (li, i)) for i in range(2)]
            s_g = [self.dsem("p3g%d_%d" % (li, i)) for i in range(3)]
            s_y = [self.dsem("p3y%d_%d" % (li, i)) for i in range(2)]
            t_w = [self.dma(SP, wo[:], self.wob[li].rearrange("(k p) n -> p k n", p=128), s_w),
                   self.dma(SP, wd[:], self.wdb[li].rearrange("(k p) n -> p k n", p=128), s_w),
                   self.dma(SP, lnc[:], self.ln_r[li].rearrange("a p n -> p a n"), s_w)]
            t_eps = POOL.mark(nc.gpsimd.memset(eps[:], LN_EPS))
            ACT.wait(t_eps)
            V = nc.vector

            def dv(inst):
                t = DVE.mark(inst)
                DVE.wait(t)
                return t

            def layer_norm(src, dst, gi):
                dv(V.bn_stats(out=stt[:, 0, :], in_=src[:, 0:512]))
                dv(V.bn_stats(out=stt[:, 1, :], in_=src[:, 512:1024]))
                a = dv(V.bn_aggr(out=sm[:, 0:2], in_=stt[:].rearrange("p a s -> p (a s)")))
                ACT.wait(a)
                a = ACT.mark(nc.scalar.activation(out=sm[:, 2:3], in_=sm[:, 1:2], func=AF.Sqrt, bias=eps[:, 0:1],
                                                  scale=1.0))
                DVE.wait(a)
                dv(V.reciprocal(out=sm[:, 3:4], in_=sm[:, 2:3]))
                dv(V.tensor_scalar(dst, src, sm[:, 0:1], sm[:, 3:4], ALU.subtract, ALU.mult))
                dv(V.tensor_tensor(out=dst, in0=dst, in1=lnc[:, gi, :], op=ALU.mult))
                return dv(V.tensor_tensor(out=dst, in0=dst, in1=lnc[:, gi + 1, :], op=ALU.add))

            ot_free = [None, None]
            xt_free = [None, None]
            wgu_free = [None] * 3
            pG_free = [None] * 4
            sg_free = [None, None]
            yt_free = [None, None]
            oT_free = None
            pOT_free = None
            pXT_free = None
            pM_free = None
            x1T_free = None
            hT_free = None
            r_free = None
            gi_ = 0
            ci_ = 0
            groups = [(sq, grp) for sq in range(self.nseq) for grp in range(4)]
            x1_toks = {}
            x1T_frees = [None, None]
            st3 = dict(pOT_free=None, oT_free=None, pM_free=None, pXT_free=None)

            def a_stage(gidx):
                sq, grp = groups[gidx]
                par = gidx % 2
                toks = []
                ln_tok = {}

                def front(tl):
                    i = grp * 4 + tl
                    sl = i % 2
                    t_o = self.dma(SP, ot[sl][:], self.om[li, sq, i * 128:(i + 1) * 128, :], s_o[sl], [ot_free[sl]])
                    t_x = self.dma(SP, xt[sl][:], xsrc[sq, i * 128:(i + 1) * 128, :], s_x[sl], [xt_free[sl]])
                    PE.wait(t_o, st3["pOT_free"])
                    for k in range(8):
                        ins = nc.tensor.transpose(pOT[:, k * 128:(k + 1) * 128], ot[sl][:, k * 128:(k + 1) * 128],
                                                  self.ident[:])
                    tT = PE.mark(ins)
                    ot_free[sl] = tT
                    ACT.wait(tT, st3["oT_free"])
                    tC = ACT.mark(nc.scalar.copy(out=oT[:].rearrange("p k t -> p (k t)"), in_=pOT[:]))
                    st3["pOT_free"] = tC
                    PE.wait(tC, st3["pM_free"], t_w)
                    for half in range(2):
                        for k in range(8):
                            ins = nc.tensor.matmul(pM[:, half * 512:(half + 1) * 512], lhsT=oT[:, k, :],
                                                   rhs=wo[:, k, half * 512:(half + 1) * 512], start=(k == 0),
                                                   stop=(k == 7))
                    tM = PE.mark(ins)
                    st3["oT_free"] = tM
                    DVE.wait(tM, t_x)
                    a = dv(V.scalar_tensor_tensor(out=x1[par][tl][:], in0=xt[sl][:], scalar=float(ALPHA), in1=pM[:],
                                                  op0=ALU.mult, op1=ALU.add))
                    st3["pM_free"] = a
                    xt_free[sl] = a
                    ln_tok[tl] = layer_norm(x1[par][tl][:], x1[par][tl][:], 0)

                def back(tl):
                    for hf in range(2):
                        PE.wait(ln_tok[tl], st3["pXT_free"])
                        for k in range(4):
                            kk = hf * 4 + k
                            ins = nc.tensor.transpose(pXT[:, k * 128:(k + 1) * 128],
                                                      x1[par][tl][:, kk * 128:(kk + 1) * 128], self.identf[:])
                        tT = PE.mark(ins)
                        ACT.wait(tT, x1T_frees[par])
                        tC = ACT.mark(nc.scalar.copy(out=x1T[par][:, hf * 4:hf * 4 + 4, tl * 128:(tl + 1) * 128],
                                                     in_=pXT[:].rearrange("p (k t) -> p k t", k=4)))
                        st3["pXT_free"] = tC
                    toks.append(tC)

                front(0)
                yield
                for tl in range(1, 4):
                    front(tl)
                    yield
                    back(tl - 1)
                    yield
                back(3)
                x1_toks[gidx] = toks

            def run_all(gen):
                for _ in gen:
                    pass

            run_all(a_stage(0))
            for gidx, (sq, grp) in enumerate(groups):
                par = gidx % 2
                gen = a_stage(gidx + 1) if gidx + 1 < len(groups) else iter(())
                for cidx in range(NFF):
                    ws = ci_ % 3
                    ci_ += 1
                    t_g = self.dma(SP, wgu[ws][:], self.wgub[li, cidx], s_g[ws], [wgu_free[ws]])
                    bg = gi_ % 4
                    bu = (gi_ + 1) % 4
                    gi_ += 2
                    PE.wait(t_g, x1_toks[gidx], pG_free[bg], pG_free[bu])
                    for (bank, j) in ((bg, 0), (bu, 1)):
                        for k in range(8):
                            ins = nc.tensor.matmul(pG[bank][:, :], lhsT=wgu[ws][:, j, k * 128:(k + 1) * 128],
                                                   rhs=x1T[par][:, k, :], start=(k == 0), stop=(k == 7))
                    tM = PE.mark(ins)
                    wgu_free[ws] = tM
                    ss = cidx % 2
                    ACT.wait(tM, sg_free[ss])
                    tS = ACT.mark(nc.scalar.activation(out=sg[ss][:], in_=pG[bg][:, :], func=AF.Silu))
                    pG_free[bg] = tS
                    DVE.wait(tS, hT_free if cidx == 0 else None)
                    tH = DVE.mark(V.tensor_tensor(out=hT[:, cidx, :], in0=sg[ss][:], in1=pG[bu][:, :], op=ALU.mult))
                    pG_free[bu] = tH
                    sg_free[ss] = tH
                    if cidx % 3 == 1:
                        next(gen, None)
                x1T_frees[par] = PE.last
                run_all(gen)
                for tl in range(4):
                    i = grp * 4 + tl
                    ys = i % 2
                    PE.wait(tH, st3["pM_free"])
                    for cidx in range(NFF):
                        for half in range(2):
                            ins = nc.tensor.matmul(pM[:, half * 512:(half + 1) * 512],
                                                   lhsT=hT[:, cidx, tl * 128:(tl + 1) * 128],
                                                   rhs=wd[:, cidx, half * 512:(half + 1) * 512], start=(cidx == 0),
                                                   stop=(cidx == NFF - 1))
                    tM = PE.mark(ins)
                    DVE.wait(tM, yt_free[ys])
                    a = dv(V.scalar_tensor_tensor(out=yt[ys][:], in0=x1[par][tl][:], scalar=float(ALPHA), in1=pM[:],
                                                  op0=ALU.mult, op1=ALU.add))
                    st3["pM_free"] = a
                    a = layer_norm(yt[ys][:], yt[ys][:], 2)
                    yt_free[ys] = self.dma(SP, ydst[sq, i * 128:(i + 1) * 128, :], yt[ys][:], s_y[ys], [a])
                hT_free = PE.last


def make_consts():
    t = np.arange(128)
    lc = np.zeros((16, 128, 128), np.float32)
    for dl in range(16):
        dist = 128 * dl + t[None, :] - t[:, None]
        m = (((dist >= 0) & (dist <= 128)).astype(np.int64)
             + ((dist >= 0) & (dist <= 512) & (dist % 4 == 0))
             + ((dist >= 0) & (dist <= 2048) & (dist % 16 == 0)))
        lc[dl] = np.where(m > 0, np.log(np.maximum(m, 1)), NEG)
    pos = np.arange(S)
    rs, bs = (pos % 128).astype(np.float32), (pos // 128).astype(np.float32)
    kaug = np.stack([rs, np.ones(S, np.float32), bs, np.ones(S, np.float32)])
    qaug = np.zeros((8, 4, S), np.float32)
    for j in range(8):
        sl = 2.0 ** -(j + 1)
        qaug[j] = np.stack([np.full(S, sl, np.float32), -sl * rs, np.full(S, 128 * sl, np.float32), -128 * sl * bs])
    ident = np.eye(128, dtype=np.float32)
    causf = np.where(t[None, :] > t[:, None], -1e30, 0.0).astype(np.float32)
    lcaus = np.where(t[:, None] > t[None, :], NEG, 0.0).astype(np.float32)
    pow2 = np.tile((2.0 ** -np.arange(NIT)).astype(np.float32)[None, :], (128, 1))
    return dict(c_lc=lc, c_kaug=kaug, c_qaug=qaug, c_ident=ident, c_causf=causf, c_lcaus=lcaus, c_pow2=pow2)


_CACHE = {}


def get_nc(nl, nseq, lam_inits, debug=False):
    key = (nl, nseq, tuple(lam_inits), debug)
    if key not in _CACHE:
        _CACHE[key] = K(nl, nseq, lam_inits, debug).build()
    return _CACHE[key]


def layer_inputs(layers, w_in, w_o, lam, subln_g, ln1_g, ln1_b, w_gate, w_up, w_down, ln2_g, ln2_b):
    f = lambda a: np.ascontiguousarray(np.asarray(a, dtype=np.float32))
    L = list(layers)
    rep = lambda v: np.ascontiguousarray(np.broadcast_to(np.asarray(v, np.float32)[None, :], (128, v.shape[-1])))
    d = dict(w_in=f(w_in[L]), w_o=f(w_o[L]), w_gate=f(w_gate[L]), w_up=f(w_up[L]), w_down=f(w_down[L]))
    d["lam_r"] = np.stack([rep(np.asarray(lam[l]).reshape(-1)) for l in L])
    d["sg_r"] = np.stack([rep(np.asarray(subln_g[l])) for l in L])
    d["ln_r"] = np.stack([np.stack([rep(np.asarray(v[l])) for v in (ln1_g, ln1_b, ln2_g, ln2_b)]) for l in L])
    d.update(make_consts())
    return d


def lam_init_of(l):
    return 0.8 - 0.6 * math.exp(-0.3 * l)


def kernel(x, w_in, w_o, lam, subln_g, ln1_g, ln1_b, w_gate, w_up, w_down, ln2_g, ln2_b):
    x = np.ascontiguousarray(np.asarray(x, dtype=np.float32))
    args = (w_in, w_o, lam, subln_g, ln1_g, ln1_b, w_gate, w_up, w_down, ln2_g, ln2_b)
    args = tuple(np.asarray(a) for a in args)
    nl = 2
    nc = get_nc(nl, SEQ_PER_CORE, [lam_init_of(l) for l in range(nl)])
    shared = layer_inputs(range(nl), *args)
    in_maps = []
    for c in range(N_CORES):
        m = dict(shared)
        m["x"] = x[c * SEQ_PER_CORE:(c + 1) * SEQ_PER_CORE]
        in_maps.append(m)
    res = run_bass_kernel_spmd(nc, in_maps, core_ids=list(range(N_CORES)))
    return np.concatenate([np.asarray(r["y"]) for r in res.results], axis=0).astype(np.float32)
```

```python
import math
from contextlib import ExitStack

import numpy as np
import concourse.bass as bass
import concourse.mybir as mybir
from concourse.bass_utils import run_bass_kernel_spmd

F32 = mybir.dt.float32
BF16 = mybir.dt.bfloat16
AF = mybir.ActivationFunctionType
ALU = mybir.AluOpType
AX = mybir.AxisListType

S = 2048
D = 1024
NT = 16
FF = 2816
NFF = 22
INC = 3012
ALPHA = 4.0 ** 0.25
LN_EPS = 1e-5
NIT = 14
NEG = -30000.0
N_CORES = 8
SEQ_PER_CORE = 4

C_QA, C_KA, C_VA, C_QI, C_KI, C_WI, C_QB, C_KB, C_VB, C_QC, C_KC, C_VC = (
    0, 256, 320, 384, 640, 704, 708, 964, 1220, 1476, 1988, 2500)


class Sem:
    def __init__(self, h):
        self.h = h
        self.n = 0


class Eng:
    def __init__(self, k, eng, name):
        self.e = eng
        self.sem = Sem(k.new_sem("e_" + name))
        self.seen = {}
        self.last = None

    def wait(self, *toks):
        for t in toks:
            if t is None:
                continue
            if isinstance(t, list):
                self.wait(*t)
                continue
            s, v = t
            if self.seen.get(s, 0) >= v:
                continue
            self.e.wait_ge(s.h, v)
            self.seen[s] = v

    def mark(self, inst):
        self.sem.n += 1
        inst.then_inc(self.sem.h, 1)
        self.last = (self.sem, self.sem.n)
        return self.last


class K:
    def __init__(self, nl, nseq, lam_inits, debug=False):
        self.nl, self.nseq, self.lam_inits, self.debug = nl, nseq, lam_inits, debug
        self.nc = bass.Bass("TRN2", target_bir_lowering=False)
        self.ctx = ExitStack()
        self.dsems = []
        self.sem_pool = []
        self.in_use = []

    def new_sem(self, name):
        return self.ctx.enter_context(self.nc.semaphore(name))

    def dsem(self, name):
        if self.sem_pool:
            s = self.sem_pool.pop()
        else:
            s = Sem(self.new_sem("d%d" % len(self.dsems)))
            self.dsems.append(s)
        self.in_use.append(s)
        return s

    def dram(self, name, shape, dt, kind="Internal"):
        return self.nc.dram_tensor(name, list(shape), dt, kind=kind).ap()

    def dma(self, q, out, in_, sem, waits=(), **kw):
        q.wait(*waits)
        inst = q.e.dma_start(out=out, in_=in_, **kw)
        sem.n += 16
        inst.then_inc(sem.h, 16)
        return (sem, sem.n)

    def barrier(self):
        sp = self.SP
        for e in self.engs:
            if e is not sp:
                sp.wait(e.last)
        for s in self.dsems:
            if s.n:
                sp.wait((s, s.n))
        tok = self.dma(sp, self.bar_b[:], self.bar_a[:], self.bar_sem)
        for e in self.engs:
            e.wait(tok)
        self.sem_pool.extend(self.in_use)
        self.in_use = []

    def build(self):
        nc, nl, nseq = self.nc, self.nl, self.nseq
        ctx = self.ctx
        dbg = "ExternalOutput" if self.debug else "Internal"
        ein = lambda n, s: self.dram(n, s, F32, "ExternalInput")
        self.x_in = ein("x", [nseq, S, D])
        self.w_in = ein("w_in", [nl, D, INC])
        self.w_o = ein("w_o", [nl, D, D])
        self.w_g = ein("w_gate", [nl, D, FF])
        self.w_u = ein("w_up", [nl, D, FF])
        self.w_d = ein("w_down", [nl, FF, D])
        self.lam_r = ein("lam_r", [nl, 128, 128])
        self.sg_r = ein("sg_r", [nl, 128, 64])
        self.ln_r = ein("ln_r", [nl, 4, 128, D])
        self.c_lc = ein("c_lc", [16, 128, 128])
        self.c_kaug = ein("c_kaug", [4, S])
        self.c_qaug = ein("c_qaug", [8, 4, S])
        self.c_ident = ein("c_ident", [128, 128])
        self.c_causf = ein("c_causf", [128, 128])
        self.c_lcaus = ein("c_lcaus", [128, 128])
        self.c_pow2 = ein("c_pow2", [128, NIT])
        self.y_out = self.dram("y", [nseq, S, D], F32, "ExternalOutput")
        self.w1b = self.dram("w1b", [nl, D, INC], BF16)
        self.wob = self.dram("wob", [nl, D, D], BF16)
        self.wgub = self.dram("wgub", [nl, NFF, 128, 2, 1024], BF16)
        self.wdb = self.dram("wdb", [nl, FF, D], BF16)
        self.kaug_b = self.dram("kaug_b", [4, S], BF16)
        self.qaug_b = self.dram("qaug_b", [8, 4, S], BF16)
        self.zq = self.dram("zq", [nl, nseq, INC, S], BF16, dbg)
        self.zv = self.dram("zv", [nl, nseq, S, 13 * 65], BF16, dbg)
        self.zw = self.dram("zw", [nl, nseq, S, 4], F32, dbg)
        self.zqf = self.dram("zqf", [nl, nseq, 320, S], F32, dbg)
        self.om = self.dram("om", [nl, nseq, S, D], BF16, dbg)
        self.xmid = [self.dram("xmid%d" % i, [nseq, S, D], F32, dbg) for i in range(nl - 1)]

        with ctx:
            self.PE = Eng(self, nc.tensor, "pe")
            self.ACT = Eng(self, nc.scalar, "act")
            self.DVE = Eng(self, nc.vector, "dve")
            self.POOL = Eng(self, nc.gpsimd, "pool")
            self.SP = Eng(self, nc.sync, "sp")
            self.engs = [self.PE, self.ACT, self.DVE, self.POOL, self.SP]
            self.bar_sem = Sem(self.new_sem("bar"))
            sb = lambda n, s, d: ctx.enter_context(nc.sbuf_tensor(n, list(s), d))
            self.bar_a = sb("bar_a", [1, 8], F32)
            self.bar_b = sb("bar_b", [1, 8], F32)
            self.ident = sb("ident", [128, 128], BF16)
            self.identf = sb("identf", [128, 128], F32)
            self.lcaus = sb("lcaus", [128, 128], BF16)
            self.causf = sb("causf", [128, 128], F32)
            self.pow2 = sb("pow2", [128, NIT], F32)
            self.lc = sb("lc", [128, 16, 128], BF16)
            self.phase0()
            self.barrier()
            for li in range(nl):
                xsrc = self.x_in if li == 0 else self.xmid[li - 1]
                ydst = self.y_out if li == nl - 1 else self.xmid[li]
                self.phase1(li, xsrc)
                self.barrier()
                self.phase2(li)
                self.barrier()
                self.phase3(li, xsrc, ydst)
                self.barrier()
        return nc

    def phase0(self):
        nc = self.nc
        P, SP = self.POOL, self.SP
        sw = Sem(self.new_sem("wl0"))
        self.dsems.append(sw)
        sh = self.dsem("c0")
        P.mark(nc.gpsimd.memset(self.bar_a[:], 0.0))
        cd = self.cast_dma_fn(sw)
        cd(self.ident[:], self.c_ident)
        cd(self.lcaus[:], self.c_lcaus)
        cd(self.lc[:], self.c_lc.rearrange("d s t -> s d t"))
        cd(self.kaug_b, self.c_kaug)
        cd(self.qaug_b, self.c_qaug)
        self.dma(SP, self.identf[:], self.c_ident, sh)
        self.dma(SP, self.causf[:], self.c_causf, sh)
        self.dma(SP, self.pow2[:], self.c_pow2, sh)
        self.convert_weights(0, sw)

    def cast_dma_fn(self, sw):
        ncd = [0]

        def cd(o, i):
            ncd[0] += 1
            t = self.dma(self.POOL, o, i, sw, max_dma_last_dim=4096)
            if ncd[0] % 6 == 0:
                self.POOL.wait(t)
            return t
        return cd

    def convert_weights(self, l, sw):
        cd = self.cast_dma_fn(sw)
        for kc in range(8):
            cd(self.w1b[l, kc * 128:(kc + 1) * 128, :], self.w_in[l, kc * 128:(kc + 1) * 128, :])
        for h in range(2):
            cd(self.wob[l, h * 512:(h + 1) * 512, :], self.w_o[l, h * 512:(h + 1) * 512, :])
        for c in range(NFF):
            for j, w in enumerate((self.w_g, self.w_u)):
                cd(self.wgub[l, c, :, j, :].rearrange("p (k f) -> p k f", k=8),
                   w[l, :, c * 128:(c + 1) * 128].rearrange("(k p) f -> p k f", p=128))
        for h in range(NFF):
            cd(self.wdb[l, h * 128:(h + 1) * 128, :], self.w_d[l, h * 128:(h + 1) * 128, :])

    def phase1(self, li, xsrc):
        nc = self.nc
        PE, ACT, DVE, POOL, SP = self.PE, self.ACT, self.DVE, self.POOL, self.SP
        with ExitStack() as c:
            sb = lambda n, s, d: c.enter_context(nc.sbuf_tensor("L%d_%s" % (li, n), list(s), d))
            ps = lambda n, s, d: c.enter_context(nc.psum_tensor("L%d_%s" % (li, n), list(s), d))
            w1 = sb("p1_w1", [128, 8, INC], BF16)
            xT = sb("p1_xT", [128, 8, S], BF16)
            xt = [sb("p1_xt%d" % i, [128, D], F32) for i in range(2)]
            zst = [sb("p1_zst%d" % i, [128, S], BF16) for i in range(2)]
            vst = [sb("p1_vst%d" % i, [128, 13, 65], BF16) for i in range(2)]
            wst4 = [sb("p1_wst4_%d" % i, [128, 4, 4], F32) for i in range(2)]
            xTf = [sb("p1_xTf%d" % i, [128, 8, 512], F32) for i in range(2)]
            w1f = sb("p1_w1f", [128, 8, 388], F32)
            zstf = [sb("p1_zstf%d" % i, [128, 512], F32) for i in range(3)]
            pX = [ps("p1_pX%d" % i, [128, 1024], F32) for i in range(2)]
            pz = [ps("p1_pz%d" % i, [128, 512], F32) for i in range(4)]
            s_w = self.dsem("p1w%d" % li)
            s_x = [self.dsem("p1x%d_%d" % (li, i)) for i in range(2)]
            s_z = [self.dsem("p1z%d_%d" % (li, i)) for i in range(2)]
            s_v = [self.dsem("p1v%d_%d" % (li, i)) for i in range(2)]
            s_f = [self.dsem("p1f%d_%d" % (li, i)) for i in range(3)]
            s_w4 = [self.dsem("p1w4%d_%d" % (li, i)) for i in range(2)]
            t_w = self.dma(SP, w1[:], self.w1b[li].rearrange("(k p) n -> p k n", p=128), s_w)
            t_pad = POOL.mark(nc.gpsimd.memset(w1f[:, :, 320:384], 0.0))
            wsrc = self.w_in[li].rearrange("(k p) n -> p k n", p=128)
            self.dma(SP, w1f[:, :, 0:320], wsrc[:, :, C_QI:C_QI + 320], s_w)
            t_wf = self.dma(SP, w1f[:, :, 384:388], wsrc[:, :, C_WI:C_WI + 4], s_w)
            xTf_free = [None, None]
            zstf_free = [None] * 3
            wst4_free = [None, None]
            fi = 0
            t_ones = [POOL.mark(nc.gpsimd.memset(vst[i][:, :, 64:65], 1.0)) for i in range(2)]
            xt_free = [None, None]
            pX_free = [None, None]
            pz_free = [None] * 4
            zst_free = [None, None]
            vst_free = [None, None]
            xT_free = None
            pzi = 0
            evi = 0

            def ev_copy(out, in_, scale, waits):
                nonlocal evi
                evi += 1
                if evi % 2 == 0:
                    ACT.wait(*waits)
                    if scale == 1.0:
                        return ACT.mark(nc.scalar.copy(out=out, in_=in_))
                    return ACT.mark(nc.scalar.activation(out=out, in_=in_, func=AF.Copy, scale=float(scale)))
                DVE.wait(*waits)
                if scale == 1.0:
                    return DVE.mark(nc.vector.tensor_copy(out=out, in_=in_))
                return DVE.mark(nc.vector.tensor_scalar(out, in_, float(scale), None, ALU.mult))

            fm_groups = ([(C_QA + 128 * i, 128, 0.125) for i in range(2)] + [(C_KA, 64, 1.0)]
                         + [(C_QB + 128 * i, 128, 32.0 ** -0.5) for i in range(2)]
                         + [(C_KB + 128 * i, 128, 1.0) for i in range(2)]
                         + [(C_QC + 128 * i, 128, 0.125) for i in range(4)]
                         + [(C_KC + 128 * i, 128, 1.0) for i in range(4)])
            for sq in range(self.nseq):
                tokX = []
                for i in range(NT):
                    sl = i % 2
                    t_ld = self.dma(SP, xt[sl][:], xsrc[sq, i * 128:(i + 1) * 128, :], s_x[sl], [xt_free[sl]])
                    PE.wait(t_ld, pX_free[sl])
                    for k in range(8):
                        ins = nc.tensor.transpose(pX[sl][:, k * 128:(k + 1) * 128], xt[sl][:, k * 128:(k + 1) * 128],
                                                  self.identf[:])
                    tT = PE.mark(ins)
                    xt_free[sl] = tT
                    ch, jj = i // 4, i % 4
                    xs = ch % 2
                    E1, E2 = (ACT, DVE) if i % 2 == 0 else (DVE, ACT)
                    cpy = lambda E, o, i_: E.mark(nc.scalar.copy(out=o, in_=i_) if E is ACT
                                                  else nc.vector.tensor_copy(out=o, in_=i_))
                    E1.wait(tT, xTf_free[xs])
                    tXf = cpy(E1, xTf[xs][:, :, jj * 128:(jj + 1) * 128], pX[sl][:].rearrange("p (k t) -> p k t", k=8))
                    pX_free[sl] = tXf
                    E2.wait(tXf, xT_free)
                    tX = cpy(E2, xT[:, :, i * 128:(i + 1) * 128], xTf[xs][:, :, jj * 128:(jj + 1) * 128])
                    tokX.append(tX)
                    if jj == 3:
                        PE.wait(t_wf, t_pad, ACT.last, DVE.last)
                        for (c0f, M, scale) in ((0, 128, 0.125), (128, 128, 0.125), (256, 128, 1.0)):
                            Mo = 64 if c0f == 256 else 128
                            b = pzi % 4
                            pzi += 1
                            PE.wait(pz_free[b])
                            for k in range(8):
                                ins = nc.tensor.matmul(pz[b][0:M, :], lhsT=w1f[:, k, c0f:c0f + M], rhs=xTf[xs][:, k, :],
                                                       start=(k == 0), stop=(k == 7))
                            tM = PE.mark(ins)
                            fs = fi % 3
                            fi += 1
                            te = ev_copy(zstf[fs][0:Mo, :], pz[b][0:Mo, :], scale, [tM, zstf_free[fs]])
                            pz_free[b] = te
                            zstf_free[fs] = self.dma(SP, self.zqf[li, sq, c0f:c0f + Mo, ch * 512:(ch + 1) * 512],
                                                     zstf[fs][0:Mo, :], s_f[fs], [te])
                        b = pzi % 4
                        pzi += 1
                        PE.wait(pz_free[b])
                        for j4 in range(4):
                            for k in range(8):
                                ins = nc.tensor.matmul(pz[b][:, j4 * 4:(j4 + 1) * 4],
                                                       lhsT=xTf[xs][:, k, j4 * 128:(j4 + 1) * 128],
                                                       rhs=w1f[:, k, 384:388], start=(k == 0), stop=(k == 7))
                        tM = PE.mark(ins)
                        xTf_free[xs] = tM
                        ws = ch % 2
                        te = ev_copy(wst4[ws][:].rearrange("p j h -> p (j h)"), pz[b][:, 0:16], 0.5,
                                     [tM, wst4_free[ws]])
                        pz_free[b] = te
                        wst4_free[ws] = self.dma(SP, self.zw[li, sq, ch * 512:(ch + 1) * 512, :].rearrange(
                            "(j p) h -> p j h", p=128), wst4[ws][:], s_w4[ws], [te])
                zi = 0
                for (c0, M, scale) in fm_groups:
                    zs = zi % 2
                    zi += 1
                    tE = []
                    for tc in range(4):
                        b = pzi % 4
                        pzi += 1
                        PE.wait(t_w, pz_free[b], tokX[tc * 4:(tc + 1) * 4])
                        for k in range(8):
                            ins = nc.tensor.matmul(pz[b][0:M, :], lhsT=w1[:, k, c0:c0 + M],
                                                   rhs=xT[:, k, tc * 512:(tc + 1) * 512], start=(k == 0), stop=(k == 7))
                        tM = PE.mark(ins)
                        te = ev_copy(zst[zs][0:M, tc * 512:(tc + 1) * 512], pz[b][0:M, :], scale, [tM, zst_free[zs]])
                        pz_free[b] = te
                        tE.append(te)
                    zst_free[zs] = self.dma(SP, self.zq[li, sq, c0:c0 + M, :], zst[zs][0:M, :], s_z[zs], tE)
                for i in range(NT):
                    sl = i % 2
                    bA = pzi % 4
                    bB = (pzi + 1) % 4
                    pzi += 2
                    PE.wait(t_w, pz_free[bA], pz_free[bB], tokX)
                    lhs = lambda k: xT[:, k, i * 128:(i + 1) * 128]
                    for (bank, o0, n, c0) in ((bA, 0, 512, C_VC), (bB, 0, 64, C_VA), (bB, 68, 256, C_VB)):
                        for k in range(8):
                            ins = nc.tensor.matmul(pz[bank][:, o0:o0 + n], lhsT=lhs(k), rhs=w1[:, k, c0:c0 + n],
                                                   start=(k == 0), stop=(k == 7))
                    tM = PE.mark(ins)
                    E = ACT if i % 2 == 0 else DVE
                    E.wait(tM, vst_free[sl], t_ones[sl])
                    if E is ACT:
                        cp = lambda o, i_: ACT.mark(nc.scalar.copy(out=o, in_=i_))
                        wsc = lambda o, i_: ACT.mark(nc.scalar.activation(out=o, in_=i_, func=AF.Copy, scale=0.5))
                    else:
                        cp = lambda o, i_: DVE.mark(nc.vector.tensor_copy(out=o, in_=i_))
                        wsc = lambda o, i_: DVE.mark(nc.vector.tensor_scalar(o, i_, 0.5, None, ALU.mult))
                    cp(vst[sl][:, 5:13, 0:64], pz[bA][:, :].rearrange("p (h d) -> p h d", h=8))
                    cp(vst[sl][:, 0, 0:64], pz[bB][:, 0:64])
                    te = cp(vst[sl][:, 1:5, 0:64], pz[bB][:, 68:324].rearrange("p (h d) -> p h d", h=4))
                    pz_free[bA] = te
                    pz_free[bB] = te
                    vst_free[sl] = self.dma(SP, self.zv[li, sq, i * 128:(i + 1) * 128, :],
                                            vst[sl][:].rearrange("p h d -> p (h d)"), s_v[sl], [te])
                xT_free = PE.last

    def phase2(self, li):
        nc = self.nc
        PE, ACT, DVE, POOL, SP = self.PE, self.ACT, self.DVE, self.POOL, self.SP
        lam_init = self.lam_inits[li]
        V = nc.vector
        with ExitStack() as c:
            sb = lambda n, s, d: c.enter_context(nc.sbuf_tensor("L%d_%s" % (li, n), list(s), d))
            ps = lambda n, s, d: c.enter_context(nc.psum_tensor("L%d_%s" % (li, n), list(s), d))
            aq = [sb("p2_aq%d" % i, [68, S], BF16) for i in range(4)]
            ak = sb("p2_ak", [68, S], BF16)
            aqi = [sb("p2_aqi%d" % i, [68, S], F32) for i in range(4)]
            aki = sb("p2_aki", [68, S], F32)
            av = sb("p2_av", [128, NT, 65], BF16)
            awi = sb("p2_awi", [128, NT, 4], F32)
            bc = [[sb("p2_bc%d_%d" % (s_, i), [68, S], BF16) for i in range(4)] for s_ in range(2)]
            bcv = [sb("p2_bcv%d" % s_, [128, NT, 65], BF16) for s_ in range(2)]
            acc4 = sb("p2_acc4", [128, 4, S], F32)
            mneg = [sb("p2_mneg0", [128, 4, S], BF16)] * 2
            junk = sb("p2_junk", [128, S], BF16)
            ones_t = sb("p2_ones", [128, S], BF16)
            zr = sb("p2_zr", [128, S], F32)
            identN = sb("p2_identN", [128, 128], BF16)
            R = [sb("p2_R%d" % i, [128, 512], F32) for i in range(2)]
            Pt = [sb("p2_P%d" % i, [128, 512], BF16) for i in range(3)]
            oTs = [sb("p2_oTs%d" % i, [65, 512], F32) for i in range(2)]
            ostg = [sb("p2_ostg%d" % i, [128, 4, 64], BF16) for i in range(2)]
            sm = sb("p2_sm", [128, 96], F32)
            stp = sb("p2_stp", [128, NIT, 4], F32)
            lamt = sb("p2_lam", [128, 128], F32)
            lamp = sb("p2_lamp", [128, 64], F32)
            gsc = sb("p2_gsc", [128, 64], F32)
            t1 = sb("p2_t1", [128, 4, 64], F32)
            osb = sb("p2_osb", [128, 4, 64], F32)
            sqj = sb("p2_sqj", [128, 64], F32)
            epst = sb("p2_eps", [128, 1], F32)
            pS = [ps("p2_pS%d" % i, [128, 512], F32) for i in range(3)]
            pO = [ps("p2_pO%d" % i, [128, 512], F32) for i in range(2)]
            pT = [ps("p2_pT%d" % i, [128, 512], F32) for i in range(3)]
            s_c = self.dsem("p2c%d" % li)
            s_a = self.dsem("p2a%d" % li)
            s_bc = [self.dsem("p2bc%d_%d" % (li, i)) for i in range(2)]
            s_o = [self.dsem("p2o%d_%d" % (li, i)) for i in range(2)]

            t_eps = POOL.mark(nc.gpsimd.memset(epst[:], LN_EPS))
            ACT.wait(t_eps)
            t_on = POOL.mark(nc.gpsimd.memset(ones_t[:], 1.0))
            DVE.wait(t_on)
            t_idn = DVE.mark(V.tensor_scalar(identN[:], self.ident[:], NEG, None, ALU.mult))
            PE.wait(t_idn)
            for t_ in aqi + [aki]:
                POOL.mark(nc.gpsimd.memset(t_[64:68, :], 0.0))
            for s_ in range(2):
                for t_ in bc[s_]:
                    t_zero = POOL.mark(nc.gpsimd.memset(t_[:, :], 0.0))
            SP.wait(t_zero)
            if li + 1 < self.nl:
                swn = Sem(self.new_sem("wnext%d" % li))
                self.dsems.append(swn)
                self.convert_weights(li + 1, swn)
            t_l = self.dma(SP, lamt[:], self.lam_r[li], s_c)
            t_g = self.dma(SP, gsc[:], self.sg_r[li], s_c)
            DVE.wait(t_l, t_g)
            a = DVE.mark(V.tensor_tensor(out=lamp[:, 0:32], in0=lamt[:, 0:32], in1=lamt[:, 32:64], op=ALU.mult))
            a = DVE.mark(V.tensor_tensor(out=lamp[:, 32:64], in0=lamt[:, 64:96], in1=lamt[:, 96:128], op=ALU.mult))
            DVE.wait(a)
            a = DVE.mark(V.tensor_reduce(out=sm[:, 0:2], in_=lamp[:].rearrange("p (a d) -> p a d", a=2),
                                         axis=AX.X, op=ALU.add))
            ACT.wait(a)
            a = ACT.mark(nc.scalar.activation(out=sm[:, 2:4], in_=sm[:, 0:2], func=AF.Exp))
            DVE.wait(a)
            a = DVE.mark(V.tensor_tensor(out=sm[:, 4:5], in0=sm[:, 3:4], in1=sm[:, 2:3], op=ALU.subtract))
            DVE.wait(a)
            t_nlam = DVE.mark(V.tensor_scalar(sm[:, 5:6], sm[:, 4:5], float(lam_init), None, ALU.subtract))
            t_gsc = DVE.mark(V.tensor_scalar(gsc[:], gsc[:], float(1.0 - lam_init), None, ALU.mult))
            nlam = sm[:, 5:6]

            st = dict(si=0, pi=0, oi=0, ti=0, ei=0, gi=0, ri=0)
            pS_free = [None] * 3
            P_free = [None] * 3
            pO_free = [None] * 2
            pT_free = [None] * 3
            oTs_free = [None] * 2
            ostg_free = [None] * 2
            R_free = [None, None]
            mneg_free = [None]
            a_free = None
            bc_free = [None, None]
            pend = []

            def flush_pv(keep):
                while len(pend) > keep:
                    pend.pop(0)()

            def dv(inst):
                t = DVE.mark(inst)
                DVE.wait(t)
                return t

            def emit_unit(G, qT, kT, Kr, vt, mkind, marg, final_fn):
                bt0 = 4 * G
                nb = bt0 + 4
                ob = st["oi"] % 2
                st["oi"] += 1
                for bs in range(nb):
                    j0 = max(0, bs - bt0)
                    c0 = j0 * 128
                    sbk = st["si"] % 3
                    st["si"] += 1
                    PE.wait(pS_free[sbk])
                    ins = nc.tensor.matmul(pS[sbk][:, c0:512], lhsT=kT[0:Kr, bs * 128:(bs + 1) * 128],
                                           rhs=qT[0:Kr, bt0 * 128 + c0:bt0 * 128 + 512], start=True,
                                           stop=(mkind == "B" and bs < bt0))
                    if mkind == "C":
                        ins = nc.tensor.matmul(pS[sbk][:, c0:512], lhsT=self.ident[:],
                                               rhs=self.lc[:, bt0 + j0 - bs:bt0 + 4 - bs, :].rearrange("p d t -> p (d t)"),
                                               start=False, stop=True)
                    elif mkind == "B":
                        if bs >= bt0:
                            j = bs - bt0
                            ins = nc.tensor.matmul(pS[sbk][:, j * 128:(j + 1) * 128], lhsT=self.ident[:],
                                                   rhs=self.lcaus[:], start=False, stop=True)
                    else:
                        for j in range(j0, 4):
                            ins = nc.tensor.matmul(pS[sbk][:, j * 128:(j + 1) * 128],
                                                   lhsT=marg[:, j, bs * 128:(bs + 1) * 128], rhs=identN[:],
                                                   start=False, stop=True)
                    tS = PE.mark(ins)
                    pi = st["pi"] % 3
                    st["pi"] += 1
                    ACT.wait(tS, P_free[pi])
                    tP = ACT.mark(nc.scalar.activation(out=Pt[pi][:, c0:512], in_=pS[sbk][:, c0:512], func=AF.Exp))
                    pS_free[sbk] = tP

                    def pv(bs=bs, c0=c0, pi=pi, tP=tP):
                        PE.wait(tP)
                        if bs == 0:
                            PE.wait(pO_free[ob])
                        tV = PE.mark(nc.tensor.matmul(pO[ob][0:65, c0:512], lhsT=vt[:, bs, :], rhs=Pt[pi][:, c0:512],
                                                      start=(bs == 0), stop=(bs == nb - 1)))
                        P_free[pi] = tV
                        if bs == nb - 1:
                            es = st["ei"] % 2
                            st["ei"] += 1
                            ACT.wait(tV, oTs_free[es])
                            tE = ACT.mark(nc.scalar.copy(out=oTs[es][:, :], in_=pO[ob][0:65, :]))
                            pO_free[ob] = tE

                            def tr():
                                tb = st["ti"] % 3
                                st["ti"] += 1
                                PE.wait(tE, pT_free[tb])
                                for j in range(4):
                                    ins2 = nc.tensor.transpose(pT[tb][:, j * 65:(j + 1) * 65],
                                                               oTs[es][0:65, j * 128:(j + 1) * 128],
                                                               self.identf[0:65, 0:65])
                                tT = PE.mark(ins2)
                                oTs_free[es] = tT
                                final_fn(tb, tT)
                            pend.append(tr)
                    pend.append(pv)
                    flush_pv(2)

            def store_out(G, col, osl, waits):
                dst = self.om[li, cur["sq"], G * 512:(G + 1) * 512, col:col + 64].rearrange("(j p) c -> p j c", p=128)
                with nc.allow_non_contiguous_dma(reason="64-col head slice"):
                    ostg_free[osl] = self.dma(SP, dst, ostg[osl][:], s_o[osl], waits)

            def norm_final(G, col):
                def f(tb, tT):
                    osl = st["gi"] % 2
                    st["gi"] += 1
                    rec = sm[:, 32 + 4 * tb:36 + 4 * tb]
                    DVE.wait(tT)
                    r = DVE.mark(V.reciprocal(out=rec, in_=pT[tb][:, 0:260].rearrange("p (j d) -> p j d", d=65)[:, :, 64]))
                    ACT.wait(r, ostg_free[osl])
                    for j in range(4):
                        tA = ACT.mark(nc.scalar.activation(out=ostg[osl][:, j, :], in_=pT[tb][:, j * 65:j * 65 + 64],
                                                           func=AF.Copy, scale=rec[:, j:j + 1]))
                    pT_free[tb] = tA
                    store_out(G, col, osl, [tA])
                return f

            bstate = {}

            def b_final(G, h, m):
                def f(tb, tT):
                    bstate[m] = (tb, tT)
                    if m == 0:
                        return
                    (b1, tv1), (b2, tv2) = bstate[0], bstate[1]
                    osl = st["gi"] % 2
                    st["gi"] += 1
                    rec1, rec2, nl2, ss, lnv, rstd = (sm[:, 48:52], sm[:, 52:56], sm[:, 56:60], sm[:, 60:64],
                                                      sm[:, 64:68], sm[:, 68:72])
                    v1 = pT[b1][:, 0:260].rearrange("p (j d) -> p j d", d=65)
                    v2 = pT[b2][:, 0:260].rearrange("p (j d) -> p j d", d=65)
                    DVE.wait(tv1, tv2, t_nlam, t_gsc)
                    DVE.mark(V.reciprocal(out=rec1, in_=v1[:, :, 64]))
                    dv(V.reciprocal(out=rec2, in_=v2[:, :, 64]))
                    dv(V.tensor_scalar(nl2, rec2, nlam, None, ALU.mult))
                    for j in range(4):
                        a_ = DVE.mark(V.tensor_scalar(t1[:, j, :], v1[:, j, 0:64], rec1[:, j:j + 1], None, ALU.mult))
                    DVE.wait(a_)
                    pT_free[b1] = a_
                    for j in range(4):
                        a_ = DVE.mark(V.scalar_tensor_tensor(out=osb[:, j, :], in0=v2[:, j, 0:64], scalar=nl2[:, j:j + 1],
                                                             in1=t1[:, j, :], op0=ALU.mult, op1=ALU.add))
                    DVE.wait(a_)
                    pT_free[b2] = a_
                    for j in range(4):
                        a_ = DVE.mark(V.scalar_tensor_tensor(out=sqj[:], in0=osb[:, j, :], scalar=1.0, in1=osb[:, j, :],
                                                             op0=ALU.mult, op1=ALU.mult, accum_out=ss[:, j:j + 1]))
                    ACT.wait(a_)
                    a6 = ACT.mark(nc.scalar.activation(out=lnv, in_=ss, func=AF.Ln, scale=1.0 / 64.0, bias=epst[:, 0:1]))
                    ACT.wait(a6)
                    a7 = ACT.mark(nc.scalar.activation(out=rstd, in_=lnv, func=AF.Exp, scale=-0.5))
                    DVE.wait(a7, ostg_free[osl])
                    for j in range(4):
                        a_ = DVE.mark(V.scalar_tensor_tensor(out=ostg[osl][:, j, :], in0=osb[:, j, :],
                                                             scalar=rstd[:, j:j + 1], in1=gsc[:], op0=ALU.mult,
                                                             op1=ALU.mult))
                    DVE.wait(a_)
                    store_out(G, 256 + 64 * h, osl, [a_])
                return f

            cur = dict(sq=0)

            def idx_and_bisect(G):
                sl = G % 2
                M_ = mneg[sl]
                last = None
                for j in range(4):
                    bt = 4 * G + j
                    nk = (bt + 1) * 128
                    for cidx in range((nk + 511) // 512):
                        n = min(512, nk - cidx * 512)
                        for h in range(4):
                            sbk = st["si"] % 3
                            st["si"] += 1
                            PE.wait(pS_free[sbk])
                            tS = PE.mark(nc.tensor.matmul(pS[sbk][:, 0:n], lhsT=aqi[h][0:68, bt * 128:(bt + 1) * 128],
                                                          rhs=aki[0:68, cidx * 512:cidx * 512 + n], start=True,
                                                          stop=True))
                            dst = acc4[:, j, cidx * 512:cidx * 512 + n]
                            if h == 0:
                                DVE.wait(tS)
                                last = DVE.mark(V.tensor_scalar(dst, pS[sbk][:, 0:n], 0.0, awi[:, bt, 0:1], ALU.max,
                                                                ALU.mult))
                                pS_free[sbk] = last
                            else:
                                rs = st["ri"] % 2
                                st["ri"] += 1
                                ACT.wait(tS, R_free[rs])
                                tR = ACT.mark(nc.scalar.activation(out=R[rs][:, 0:n], in_=pS[sbk][:, 0:n], func=AF.Relu))
                                pS_free[sbk] = tR
                                DVE.wait(tR, last)
                                last = DVE.mark(V.scalar_tensor_tensor(out=dst, in0=R[rs][:, 0:n],
                                                                       scalar=awi[:, bt, h:h + 1], in1=dst,
                                                                       op0=ALU.mult, op1=ALU.add))
                                R_free[rs] = last
                yield
                am, Aa, mid, cnt, gg, tt = (sm[:, 8:12], sm[:, 12:16], sm[:, 16:20], sm[:, 20:24], sm[:, 24:28],
                                            sm[:, 28:32])
                need = sm[:, 72:76]
                nks = [(4 * G + j + 1) * 128 for j in range(4)]
                DVE.wait(last)
                for j in range(4):
                    a_ = DVE.mark(V.tensor_reduce(out=am[:, j:j + 1], in_=acc4[:, j, 0:nks[j]], axis=AX.X, op=ALU.max,
                                                  apply_absolute_value=True))
                DVE.wait(a_)
                dv(V.tensor_scalar(Aa, am, 1.0001, 1e-30, ALU.mult, ALU.add))
                for j in range(4):
                    DVE.mark(V.tensor_scalar(stp[:, :, j], self.pow2[:], Aa[:, j:j + 1], None, ALU.mult))
                    DVE.mark(V.tensor_tensor(out=acc4[:, j, nks[j] - 128:nks[j]], in0=acc4[:, j, nks[j] - 128:nks[j]],
                                             in1=self.causf[:], op=ALU.add))
                dv(V.memset(mid, 0.0))
                yield
                for k in range(NIT):
                    for j in range(4):
                        yield
                        a_ = DVE.mark(V.tensor_scalar(junk[:, 0:nks[j]], acc4[:, j, 0:nks[j]], mid[:, j:j + 1], 0.0,
                                                      ALU.is_gt, ALU.add, accum_out=cnt[:, j:j + 1]))
                    yield
                    DVE.wait(a_)
                    if k == 0:
                        dv(V.tensor_scalar(need, cnt, -1.0, 256.0, ALU.mult, ALU.add))
                        dv(V.tensor_scalar(need, need, 0.0, None, ALU.max))
                    dv(V.tensor_scalar(gg, cnt, 255.5, 0.5 if k < NIT - 1 else 1.0, ALU.is_ge, ALU.subtract))
                    dv(V.tensor_tensor(out=tt, in0=gg, in1=stp[:, k, :], op=ALU.mult))
                    dv(V.tensor_tensor(out=mid, in0=mid, in1=tt, op=ALU.add))
                DVE.wait(mneg_free[0])
                for j in range(4):
                    n_ = nks[j]
                    yield
                    dv(V.tensor_scalar(junk[:, 0:n_], acc4[:, j, 0:n_], 0.0, None, ALU.is_equal))
                    yield
                    dv(V.tensor_tensor_scan(out=zr[:, 0:n_], data0=ones_t[:, 0:n_], data1=junk[:, 0:n_], initial=0.0,
                                            op0=ALU.mult, op1=ALU.add))
                    yield
                    dv(V.scalar_tensor_tensor(out=junk[:, 0:n_], in0=zr[:, 0:n_], scalar=need[:, j:j + 1],
                                              in1=junk[:, 0:n_], op0=ALU.is_gt, op1=ALU.mult))
                    yield
                    tM = dv(V.scalar_tensor_tensor(out=M_[:, j, 0:n_], in0=acc4[:, j, 0:n_], scalar=mid[:, j:j + 1],
                                                   in1=junk[:, 0:n_], op0=ALU.is_le, op1=ALU.max))
                tmn[G] = tM

            for sq in range(self.nseq):
                cur["sq"] = sq
                zq = self.zq[li, sq]
                zv = self.zv[li, sq].rearrange("(i p) (h d) -> p i h d", p=128, d=65)
                for h in range(4):
                    self.dma(SP, aq[h][0:64, :], zq[C_QA + 64 * h:C_QA + 64 * (h + 1), :], s_a, [a_free])
                    self.dma(SP, aq[h][64:68, :], self.qaug_b[2 * h + 1], s_a)
                    self.dma(SP, aqi[h][0:64, :], self.zqf[li, sq, 64 * h:64 * (h + 1), :], s_a)
                self.dma(SP, ak[0:64, :], zq[C_KA:C_KA + 64, :], s_a)
                self.dma(SP, ak[64:68, :], self.kaug_b, s_a)
                self.dma(SP, aki[0:64, :], self.zqf[li, sq, 256:320, :], s_a)
                self.dma(SP, av[:], zv[:, :, 0, :], s_a)
                t_la = self.dma(SP, awi[:], self.zw[li, sq].rearrange("(i p) h -> p i h", p=128), s_a)

                jobs = [("B", 0), ("B", 1), ("C", 0), ("C", 1), ("B", 2), ("C", 2), ("C", 3), ("C", 4),
                        ("B", 3), ("C", 5), ("C", 6), ("C", 7)]
                grp_units = {0: 8, 1: 16, 2: 20, 3: 20}
                job_tok = {}

                def load_job(ji):
                    kind, h = jobs[ji]
                    sl = ji % 2
                    T = bc[sl]
                    w = [bc_free[sl]]
                    if kind == "B":
                        for m in range(2):
                            c0 = C_QB + h * 64 + m * 32
                            self.dma(SP, T[2 * m][0:32, :], zq[c0:c0 + 32, :], s_bc[sl], w)
                            self.dma(SP, T[2 * m][32:36, :], self.qaug_b[2 * h + 1], s_bc[sl])
                            c0 = C_KB + h * 64 + m * 32
                            self.dma(SP, T[2 * m + 1][0:32, :], zq[c0:c0 + 32, :], s_bc[sl])
                            self.dma(SP, T[2 * m + 1][32:36, :], self.kaug_b, s_bc[sl])
                        job_tok[ji] = self.dma(SP, bcv[sl][:], zv[:, :, 1 + h, :], s_bc[sl])
                    else:
                        self.dma(SP, T[0][0:64, :], zq[C_QC + 64 * h:C_QC + 64 * (h + 1), :], s_bc[sl], w)
                        self.dma(SP, T[0][64:68, :], self.qaug_b[h], s_bc[sl])
                        self.dma(SP, T[2][0:64, :], zq[C_KC + 64 * h:C_KC + 64 * (h + 1), :], s_bc[sl])
                        self.dma(SP, T[2][64:68, :], self.kaug_b, s_bc[sl])
                        job_tok[ji] = self.dma(SP, bcv[sl][:], zv[:, :, 5 + h, :], s_bc[sl])

                load_job(0)
                load_job(1)

                PE.wait(t_la, t_zero)
                DVE.wait(t_la)
                tmn = {}

                def step(gen, n):
                    for _ in range(n):
                        try:
                            next(gen)
                        except StopIteration:
                            return

                gen = iter(())
                curG = None
                nstep = 1

                def finish_group():
                    G = curG
                    step(gen, 1000)
                    flush_pv(0)
                    PE.wait(tmn[G])
                    for hh in range(4):
                        emit_unit(G, aq[hh], ak, 68, av, "A", mneg[0], norm_final(G, 64 * hh))
                    flush_pv(0)
                    mneg_free[0] = PE.last

                for ji, (kind, h) in enumerate(jobs):
                    sl = ji % 2
                    T = bc[sl]
                    if kind == "B":
                        if curG is not None:
                            finish_group()
                        curG = h
                        gen = idx_and_bisect(h)
                        nstep = -(-(NIT * 5 + 20) // grp_units[h])
                        step(gen, 1)
                    PE.wait(job_tok[ji])
                    for G in range(4):
                        if kind == "B":
                            for m in range(2):
                                emit_unit(G, T[2 * m], T[2 * m + 1], 68, bcv[sl], "B", None, b_final(G, h, m))
                                step(gen, nstep)
                        else:
                            emit_unit(G, T[0], T[2], 68, bcv[sl], "C", None, norm_final(G, 512 + 64 * h))
                            step(gen, nstep)
                    flush_pv(0)
                    bc_free[sl] = PE.last
                    if ji + 2 < len(jobs):
                        load_job(ji + 2)
                finish_group()
                a_free = PE.last

    def phase3(self, li, xsrc, ydst):
        nc = self.nc
        PE, ACT, DVE, POOL, SP = self.PE, self.ACT, self.DVE, self.POOL, self.SP
        with ExitStack() as c:
            sb = lambda n, s, d: c.enter_context(nc.sbuf_tensor("L%d_%s" % (li, n), list(s), d))
            ps = lambda n, s, d: c.enter_context(nc.psum_tensor("L%d_%s" % (li, n), list(s), d))
            wo = sb("p3_wo", [128, 8, D], BF16)
            wd = sb("p3_wd", [128, NFF, D], BF16)
            lnc = sb("p3_ln", [128, 4, D], F32)
            wgu = [sb("p3_wgu%d" % i, [128, 2, 1024], BF16) for i in range(3)]
            ot = [sb("p3_ot%d" % i, [128, D], BF16) for i in range(2)]
            xt = [sb("p3_xt%d" % i, [128, D], F32) for i in range(2)]
            oT = sb("p3_oT", [128, 8, 128], BF16)
            x1 = [[sb("p3_x1_%d_%d" % (p_, i), [128, D], F32) for i in range(4)] for p_ in range(2)]
            x1T = [sb("p3_x1T%d" % p_, [128, 8, 512], BF16) for p_ in range(2)]
            hT = sb("p3_hT", [128, NFF, 512], BF16)
            sg = [sb("p3_sg%d" % i, [128, 512], F32) for i in range(2)]
            yt = [sb("p3_yt%d" % i, [128, D], F32) for i in range(2)]
            stt = sb("p3_stt", [128, 2, 6], F32)
            sm = sb("p3_sm", [128, 16], F32)
            eps = sb("p3_eps", [128, 1], F32)
            pOT = ps("p3_pOT", [128, 1024], BF16)
            pXT = ps("p3_pXT", [128, 512], F32)
            pM = ps("p3_pM", [128, 1024], F32)
            pG = [ps("p3_pG%d" % i, [128, 512], F32) for i in range(4)]
            s_w = self.dsem("p3w%d" % li)
            s_o = [self.dsem("p3o%d_%d" % (li, i)) for i in range(2)]
            s_x = [self.dsem("p3x%d_%d" % (li, i)) for i in range(2)]
            s_g = [self.dsem("p3g%d_%d" % (li, i)) for i in range(3)]
            s_y = [self.dsem("p3y%d_%d" % (li, i)) for i in range(2)]
            t_w = [self.dma(SP, wo[:], self.wob[li].rearrange("(k p) n -> p k n", p=128), s_w),
                   self.dma(SP, wd[:], self.wdb[li].rearrange("(k p) n -> p k n", p=128), s_w),
                   self.dma(SP, lnc[:], self.ln_r[li].rearrange("a p n -> p a n"), s_w)]
            t_eps = POOL.mark(nc.gpsimd.memset(eps[:], LN_EPS))
            ACT.wait(t_eps)
            V = nc.vector

            def dv(inst):
                t = DVE.mark(inst)
                DVE.wait(t)
                return t

            def layer_norm(src, dst, gi):
                dv(V.bn_stats(out=stt[:, 0, :], in_=src[:, 0:512]))
                dv(V.bn_stats(out=stt[:, 1, :], in_=src[:, 512:1024]))
                a = dv(V.bn_aggr(out=sm[:, 0:2], in_=stt[:].rearrange("p a s -> p (a s)")))
                ACT.wait(a)
                a = ACT.mark(nc.scalar.activation(out=sm[:, 2:3], in_=sm[:, 1:2], func=AF.Sqrt, bias=eps[:, 0:1],
                                                  scale=1.0))
                DVE.wait(a)
                dv(V.reciprocal(out=sm[:, 3:4], in_=sm[:, 2:3]))
                dv(V.tensor_scalar(dst, src, sm[:, 0:1], sm[:, 3:4], ALU.subtract, ALU.mult))
                dv(V.tensor_tensor(out=dst, in0=dst, in1=lnc[:, gi, :], op=ALU.mult))
                return dv(V.tensor_tensor(out=dst, in0=dst, in1=lnc[:, gi + 1, :], op=ALU.add))

            ot_free = [None, None]
            xt_free = [None, None]
            wgu_free = [None] * 3
            pG_free = [None] * 4
            sg_free = [None, None]
            yt_free = [None, None]
            oT_free = None
            pOT_free = None
            pXT_free = None
            pM_free = None
            x1T_free = None
            hT_free = None
            r_free = None
            gi_ = 0
            ci_ = 0
            groups = [(sq, grp) for sq in range(self.nseq) for grp in range(4)]
            x1_toks = {}
            x1T_frees = [None, None]
            st3 = dict(pOT_free=None, oT_free=None, pM_free=None, pXT_free=None)

            def a_stage(gidx):
                sq, grp = groups[gidx]
                par = gidx % 2
                toks = []
                ln_tok = {}

                def front(tl):
                    i = grp * 4 + tl
                    sl = i % 2
                    t_o = self.dma(SP, ot[sl][:], self.om[li, sq, i * 128:(i + 1) * 128, :], s_o[sl], [ot_free[sl]])
                    t_x = self.dma(SP, xt[sl][:], xsrc[sq, i * 128:(i + 1) * 128, :], s_x[sl], [xt_free[sl]])
                    PE.wait(t_o, st3["pOT_free"])
                    for k in range(8):
                        ins = nc.tensor.transpose(pOT[:, k * 128:(k + 1) * 128], ot[sl][:, k * 128:(k + 1) * 128],
                                                  self.ident[:])
                    tT = PE.mark(ins)
                    ot_free[sl] = tT
                    ACT.wait(tT, st3["oT_free"])
                    tC = ACT.mark(nc.scalar.copy(out=oT[:].rearrange("p k t -> p (k t)"), in_=pOT[:]))
                    st3["pOT_free"] = tC
                    PE.wait(tC, st3["pM_free"], t_w)
                    for half in range(2):
                        for k in range(8):
                            ins = nc.tensor.matmul(pM[:, half * 512:(half + 1) * 512], lhsT=oT[:, k, :],
                                                   rhs=wo[:, k, half * 512:(half + 1) * 512], start=(k == 0),
                                                   stop=(k == 7))
                    tM = PE.mark(ins)
                    st3["oT_free"] = tM
                    DVE.wait(tM, t_x)
                    a = dv(V.scalar_tensor_tensor(out=x1[par][tl][:], in0=xt[sl][:], scalar=float(ALPHA), in1=pM[:],
                                                  op0=ALU.mult, op1=ALU.add))
                    st3["pM_free"] = a
                    xt_free[sl] = a
                    ln_tok[tl] = layer_norm(x1[par][tl][:], x1[par][tl][:], 0)

                def back(tl):
                    for hf in range(2):
                        PE.wait(ln_tok[tl], st3["pXT_free"])
                        for k in range(4):
                            kk = hf * 4 + k
                            ins = nc.tensor.transpose(pXT[:, k * 128:(k + 1) * 128],
                                                      x1[par][tl][:, kk * 128:(kk + 1) * 128], self.identf[:])
                        tT = PE.mark(ins)
                        ACT.wait(tT, x1T_frees[par])
                        tC = ACT.mark(nc.scalar.copy(out=x1T[par][:, hf * 4:hf * 4 + 4, tl * 128:(tl + 1) * 128],
                                                     in_=pXT[:].rearrange("p (k t) -> p k t", k=4)))
                        st3["pXT_free"] = tC
                    toks.append(tC)

                front(0)
                yield
                for tl in range(1, 4):
                    front(tl)
                    yield
                    back(tl - 1)
                    yield
                back(3)
                x1_toks[gidx] = toks

            def run_all(gen):
                for _ in gen:
                    pass

            run_all(a_stage(0))
            for gidx, (sq, grp) in enumerate(groups):
                par = gidx % 2
                gen = a_stage(gidx + 1) if gidx + 1 < len(groups) else iter(())
                for cidx in range(NFF):
                    ws = ci_ % 3
                    ci_ += 1
                    t_g = self.dma(SP, wgu[ws][:], self.wgub[li, cidx], s_g[ws], [wgu_free[ws]])
                    bg = gi_ % 4
                    bu = (gi_ + 1) % 4
                    gi_ += 2
                    PE.wait(t_g, x1_toks[gidx], pG_free[bg], pG_free[bu])
                    for (bank, j) in ((bg, 0), (bu, 1)):
                        for k in range(8):
                            ins = nc.tensor.matmul(pG[bank][:, :], lhsT=wgu[ws][:, j, k * 128:(k + 1) * 128],
                                                   rhs=x1T[par][:, k, :], start=(k == 0), stop=(k == 7))
                    tM = PE.mark(ins)
                    wgu_free[ws] = tM
                    ss = cidx % 2
                    ACT.wait(tM, sg_free[ss])
                    tS = ACT.mark(nc.scalar.activation(out=sg[ss][:], in_=pG[bg][:, :], func=AF.Silu))
                    pG_free[bg] = tS
                    DVE.wait(tS, hT_free if cidx == 0 else None)
                    tH = DVE.mark(V.tensor_tensor(out=hT[:, cidx, :], in0=sg[ss][:], in1=pG[bu][:, :], op=ALU.mult))
                    pG_free[bu] = tH
                    sg_free[ss] = tH
                    if cidx % 3 == 1:
                        next(gen, None)
                x1T_frees[par] = PE.last
                run_all(gen)
                for tl in range(4):
                    i = grp * 4 + tl
                    ys = i % 2
                    PE.wait(tH, st3["pM_free"])
                    for cidx in range(NFF):
                        for half in range(2):
                            ins = nc.tensor.matmul(pM[:, half * 512:(half + 1) * 512],
                                                   lhsT=hT[:, cidx, tl * 128:(tl + 1) * 128],
                                                   rhs=wd[:, cidx, half * 512:(half + 1) * 512], start=(cidx == 0),
                                                   stop=(cidx == NFF - 1))
                    tM = PE.mark(ins)
                    DVE.wait(tM, yt_free[ys])
                    a = dv(V.scalar_tensor_tensor(out=yt[ys][:], in0=x1[par][tl][:], scalar=float(ALPHA), in1=pM[:],
                                                  op0=ALU.mult, op1=ALU.add))
                    st3["pM_free"] = a
                    a = layer_norm(yt[ys][:], yt[ys][:], 2)
                    yt_free[ys] = self.dma(SP, ydst[sq, i * 128:(i + 1) * 128, :], yt[ys][:], s_y[ys], [a])
                hT_free = PE.last


def make_consts():
    t = np.arange(128)
    lc = np.zeros((16, 128, 128), np.float32)
    for dl in range(16):
        dist = 128 * dl + t[None, :] - t[:, None]
        m = (((dist >= 0) & (dist <= 128)).astype(np.int64)
             + ((dist >= 0) & (dist <= 512) & (dist % 4 == 0))
             + ((dist >= 0) & (dist <= 2048) & (dist % 16 == 0)))
        lc[dl] = np.where(m > 0, np.log(np.maximum(m, 1)), NEG)
    pos = np.arange(S)
    rs, bs = (pos % 128).astype(np.float32), (pos // 128).astype(np.float32)
    kaug = np.stack([rs, np.ones(S, np.float32), bs, np.ones(S, np.float32)])
    qaug = np.zeros((8, 4, S), np.float32)
    for j in range(8):
        sl = 2.0 ** -(j + 1)
        qaug[j] = np.stack([np.full(S, sl, np.float32), -sl * rs, np.full(S, 128 * sl, np.float32), -128 * sl * bs])
    ident = np.eye(128, dtype=np.float32)
    causf = np.where(t[None, :] > t[:, None], -1e30, 0.0).astype(np.float32)
    lcaus = np.where(t[:, None] > t[None, :], NEG, 0.0).astype(np.float32)
    pow2 = np.tile((2.0 ** -np.arange(NIT)).astype(np.float32)[None, :], (128, 1))
    return dict(c_lc=lc, c_kaug=kaug, c_qaug=qaug, c_ident=ident, c_causf=causf, c_lcaus=lcaus, c_pow2=pow2)


_CACHE = {}


def get_nc(nl, nseq, lam_inits, debug=False):
    key = (nl, nseq, tuple(lam_inits), debug)
    if key not in _CACHE:
        _CACHE[key] = K(nl, nseq, lam_inits, debug).build()
    return _CACHE[key]


def layer_inputs(layers, w_in, w_o, lam, subln_g, ln1_g, ln1_b, w_gate, w_up, w_down, ln2_g, ln2_b):
    f = lambda a: np.ascontiguousarray(np.asarray(a, dtype=np.float32))
    L = list(layers)
    rep = lambda v: np.ascontiguousarray(np.broadcast_to(np.asarray(v, np.float32)[None, :], (128, v.shape[-1])))
    d = dict(w_in=f(w_in[L]), w_o=f(w_o[L]), w_gate=f(w_gate[L]), w_up=f(w_up[L]), w_down=f(w_down[L]))
    d["lam_r"] = np.stack([rep(np.asarray(lam[l]).reshape(-1)) for l in L])
    d["sg_r"] = np.stack([rep(np.asarray(subln_g[l])) for l in L])
    d["ln_r"] = np.stack([np.stack([rep(np.asarray(v[l])) for v in (ln1_g, ln1_b, ln2_g, ln2_b)]) for l in L])
    d.update(make_consts())
    return d


def lam_init_of(l):
    return 0.8 - 0.6 * math.exp(-0.3 * l)


def kernel(x, w_in, w_o, lam, subln_g, ln1_g, ln1_b, w_gate, w_up, w_down, ln2_g, ln2_b):
    x = np.ascontiguousarray(np.asarray(x, dtype=np.float32))
    args = (w_in, w_o, lam, subln_g, ln1_g, ln1_b, w_gate, w_up, w_down, ln2_g, ln2_b)
    args = tuple(np.asarray(a) for a in args)
    nl = 2
    nc = get_nc(nl, SEQ_PER_CORE, [lam_init_of(l) for l in range(nl)])
    shared = layer_inputs(range(nl), *args)
    in_maps = []
    for c in range(N_CORES):
        m = dict(shared)
        m["x"] = x[c * SEQ_PER_CORE:(c + 1) * SEQ_PER_CORE]
        in_maps.append(m)
    res = run_bass_kernel_spmd(nc, in_maps, core_ids=list(range(N_CORES)))
    return np.concatenate([np.asarray(r["y"]) for r in res.results], axis=0).astype(np.float32)
```
